# Optimizing a Trainium2 kernel written in Bass

```python
import math
import jax, jax.numpy as jnp
from jax import lax
import numpy as np

D_MODEL = 1024
BATCH = 4
SEQ = 4096
DEPTH = 1

M_HEADS = 4
M_HEAD_DIM = D_MODEL // (2 * M_HEADS)
M_CHUNK = 64
CONV_W = 4
A_HEADS = 8
A_KV_GROUPS = 2
A_HPG = A_HEADS // A_KV_GROUPS
A_HEAD_DIM = D_MODEL // (2 * A_HEADS)
CMP_BLOCK = 32
CMP_STRIDE = 16
SEL_BLOCK = 64
SEL_TOP_N = 16
WINDOW = 512
Q_BLOCK = 128
FORCED_BONUS = 1e3
NUM_BUCKETS = 32
MAX_DISTANCE = 128
N_EXPERTS = 32
TOP_K = 4
D_EXPERT = D_MODEL
SWIGLU_LIMIT = 7.0
SWIGLU_ALPHA = 1.702
EXPERT_ROWS = 128
EPS = 1e-6
D_MIX = M_HEADS * M_HEAD_DIM + A_HEADS * A_HEAD_DIM
SPLIT_SIZES = (
    M_HEADS * M_HEAD_DIM,
    M_HEADS * M_HEAD_DIM,
    M_HEADS * M_HEAD_DIM,
    M_HEADS * M_HEAD_DIM,
    M_HEADS,
    M_HEADS,
    A_HEADS * A_HEAD_DIM,
    A_KV_GROUPS * A_HEAD_DIM,
    A_KV_GROUPS * A_HEAD_DIM,
    A_KV_GROUPS * A_HEAD_DIM,
    A_KV_GROUPS * A_HEAD_DIM,
    A_KV_GROUPS * A_HEAD_DIM,
    A_KV_GROUPS * A_HEAD_DIM,
    A_HEADS * 3,
)
D_IN = sum(SPLIT_SIZES)

kernel_name = "hybrid_mlstm_nsa_moe_block"


def rmsnorm(x, g):
    xf = x.astype(jnp.float32)
    y = xf * lax.rsqrt(jnp.mean(xf * xf, axis=-1, keepdims=True) + EPS)
    return (y * g.astype(jnp.float32)).astype(x.dtype)


def t5_bucket(dist):
    n = jnp.maximum(dist, 0)
    max_exact = NUM_BUCKETS // 2
    nf = jnp.maximum(n, 1).astype(jnp.float32)
    large = max_exact + (jnp.log(nf / max_exact) / math.log(MAX_DISTANCE / max_exact)
                         * (NUM_BUCKETS - max_exact)).astype(jnp.int32)
    large = jnp.minimum(large, NUM_BUCKETS - 1)
    return jnp.where(n < max_exact, n, large)


def masked_softmax(logits, valid):
    l = jnp.where(valid, logits, -jnp.inf)
    m = jnp.max(l, axis=-1, keepdims=True)
    m = jnp.where(jnp.isfinite(m), m, 0.0)
    e = jnp.where(valid, jnp.exp(l - m), 0.0)
    s = jnp.sum(e, axis=-1, keepdims=True)
    return e / jnp.where(s > 0, s, 1.0)


def causal_depthwise_conv(x, w):
    return lax.conv_general_dilated(
        x, w[:, None, :].astype(x.dtype), window_strides=(1,), padding=[(CONV_W - 1, 0)],
        dimension_numbers=('NWC', 'WIO', 'NWC'), feature_group_count=x.shape[-1])


def mlstm(q, k, v, i_pre, f_pre):
    dtype = q.dtype
    B_, S_, H_, Dh = q.shape
    nc = S_ // M_CHUNK
    f32 = jnp.float32

    def chunks(a):
        return a.astype(f32).reshape(B_, nc, M_CHUNK, H_, -1).transpose(0, 3, 1, 2, 4)

    qc = chunks(q)
    kc = chunks(k) * (Dh ** -0.5)
    vc = chunks(v)
    ig = i_pre.astype(f32).reshape(B_, nc, M_CHUNK, H_).transpose(0, 3, 1, 2)
    lf = jax.nn.log_sigmoid(f_pre.astype(f32)).reshape(B_, nc, M_CHUNK, H_).transpose(0, 3, 1, 2)
    g = jnp.cumsum(lf, axis=-1)
    G = g[..., -1]
    w_state = G[..., None] - g + ig

    def step(carry, xs):
        C, n, m = carry
        k_c, v_c, w_c, G_c = xs
        m_new = jnp.maximum(G_c + m, jnp.max(w_c, axis=-1))
        decay = jnp.exp(G_c + m - m_new)
        wexp = jnp.exp(w_c - m_new[..., None])
        C_new = decay[..., None, None] * C + jnp.einsum('bhl,bhld,bhle->bhde', wexp, k_c, v_c)
        n_new = decay[..., None] * n + jnp.einsum('bhl,bhld->bhd', wexp, k_c)
        return (C_new, n_new, m_new), (C, n, m)

    init = (jnp.zeros((B_, H_, Dh, Dh), f32), jnp.zeros((B_, H_, Dh), f32),
            jnp.full((B_, H_), -jnp.inf, f32))
    xs = (kc.transpose(2, 0, 1, 3, 4), vc.transpose(2, 0, 1, 3, 4),
          w_state.transpose(2, 0, 1, 3), G.transpose(2, 0, 1))
    _, (Cp, Np, Mp) = lax.scan(step, init, xs)
    Cp = Cp.transpose(1, 2, 0, 3, 4)
    Np = Np.transpose(1, 2, 0, 3)
    Mp = Mp.transpose(1, 2, 0)

    causal = jnp.tril(jnp.ones((M_CHUNK, M_CHUNK), bool))
    Dm = jnp.where(causal, g[..., :, None] - g[..., None, :] + ig[..., None, :], -jnp.inf)
    inter = g + Mp[..., None]
    m_tau = jnp.maximum(inter, jnp.max(Dm, axis=-1))
    Wm = jnp.exp(Dm - m_tau[..., None]) * jnp.einsum('bhcld,bhcsd->bhcls', qc, kc)
    inter_w = jnp.exp(inter - m_tau)
    num = (jnp.einsum('bhcls,bhcse->bhcle', Wm, vc)
           + inter_w[..., None] * jnp.einsum('bhcld,bhcde->bhcle', qc, Cp))
    den = jnp.sum(Wm, axis=-1) + inter_w * jnp.einsum('bhcld,bhcd->bhcl', qc, Np)
    h = num / jnp.maximum(jnp.abs(den), jnp.exp(-m_tau))[..., None]
    return h.transpose(0, 2, 3, 1, 4).reshape(B_, S_, H_ * Dh).astype(dtype)


def compress(k, pos_emb, w):
    S_ = k.shape[1]
    n_cmp = (S_ - CMP_BLOCK) // CMP_STRIDE + 1
    idx = jnp.arange(n_cmp)[:, None] * CMP_STRIDE + jnp.arange(CMP_BLOCK)[None, :]
    blocks = k[:, idx] + pos_emb[None, None, :, None, :]
    return jnp.einsum('bilgd,lde->bige', blocks, w)


def nsa(q, kc, vc, ks, vs, kw, vw, gates, rel_bias):
    B_, S_, G_, HPG_, Dh = q.shape
    scale = Dh ** -0.5
    f32 = jnp.float32
    n_cmp = kc.shape[1]
    n_sel = S_ // SEL_BLOCK
    top_n = min(SEL_TOP_N, n_sel)
    n_tok = top_n * SEL_BLOCK
    cmp_end = jnp.arange(n_cmp) * CMP_STRIDE + CMP_BLOCK - 1
    cmp_pos = jnp.arange(n_cmp)[:, None] * CMP_STRIDE + jnp.arange(CMP_BLOCK)[None, :]
    overlap = jnp.sum(jax.nn.one_hot(cmp_pos // SEL_BLOCK, n_sel, dtype=f32), axis=1) / CMP_BLOCK
    ks_blk = ks.reshape(B_, n_sel, SEL_BLOCK, G_, Dh).transpose(0, 3, 1, 2, 4).reshape(B_, G_, n_sel, SEL_BLOCK * Dh)
    vs_blk = vs.reshape(B_, n_sel, SEL_BLOCK, G_, Dh).transpose(0, 3, 1, 2, 4).reshape(B_, G_, n_sel, SEL_BLOCK * Dh)
    kw_pad = jnp.pad(kw, ((0, 0), (WINDOW, 0), (0, 0), (0, 0)))
    vw_pad = jnp.pad(vw, ((0, 0), (WINDOW, 0), (0, 0), (0, 0)))
    rb = rel_bias.reshape(NUM_BUCKETS, G_, HPG_)
    g_idx = jnp.arange(G_)[None, None, :, None]
    blk = jnp.arange(n_sel)
    sel_off = jnp.arange(SEL_BLOCK)
    win_off = jnp.arange(Q_BLOCK + WINDOW) - WINDOW

    def block(qb):
        q0 = qb * Q_BLOCK
        t = q0 + jnp.arange(Q_BLOCK)
        qq = lax.dynamic_slice_in_dim(q, q0, Q_BLOCK, axis=1)
        lc = jnp.einsum('bqghd,bigd->bqghi', qq, kc, preferred_element_type=f32) * scale
        lc = lc + rb[t5_bucket(t[:, None] - cmp_end[None, :])].transpose(0, 2, 3, 1)[None]
        valid_c = (cmp_end[None, :] <= t[:, None])[None, :, None, None, :]
        pc = masked_softmax(lc, valid_c)
        oc = jnp.einsum('bqghi,bigd->bqghd', pc, vc)
        ps = jnp.einsum('bqghi,ij->bqgj', pc, overlap)
        cur = t // SEL_BLOCK
        forced = (blk[None, :] == 0) | (blk[None, :] == cur[:, None]) | (blk[None, :] == cur[:, None] - 1)
        valid_b = blk[None, :] * SEL_BLOCK <= t[:, None]
        score = jnp.where(valid_b[None, :, None, :],
                          ps + jnp.where(forced, FORCED_BONUS, 0.0)[None, :, None, :], -jnp.inf)
        _, sel = lax.top_k(score, top_n)
        sel_t = sel.transpose(0, 2, 1, 3).reshape(B_, G_, Q_BLOCK * top_n)[..., None]
        kg = jnp.take_along_axis(ks_blk, sel_t, axis=2).reshape(B_, G_, Q_BLOCK, n_tok, Dh)
        vg = jnp.take_along_axis(vs_blk, sel_t, axis=2).reshape(B_, G_, Q_BLOCK, n_tok, Dh)
        tok = (sel[..., None] * SEL_BLOCK + sel_off).reshape(B_, Q_BLOCK, G_, n_tok)
        ls = jnp.einsum('bqghd,bgqnd->bqghn', qq, kg, preferred_element_type=f32) * scale
        bs = rb[t5_bucket(t[None, :, None, None] - tok), g_idx]
        ls = ls + bs.transpose(0, 1, 2, 4, 3)
        pss = masked_softmax(ls, (tok <= t[None, :, None, None])[:, :, :, None, :])
        osel = jnp.einsum('bqghn,bgqnd->bqghd', pss, vg)
        kwb = lax.dynamic_slice_in_dim(kw_pad, q0, Q_BLOCK + WINDOW, axis=1)
        vwb = lax.dynamic_slice_in_dim(vw_pad, q0, Q_BLOCK + WINDOW, axis=1)
        j = q0 + win_off
        dist = t[:, None] - j[None, :]
        valid_w = (j[None, :] >= 0) & (dist >= 0) & (dist < WINDOW)
        lw = jnp.einsum('bqghd,bkgd->bqghk', qq, kwb, preferred_element_type=f32) * scale
        lw = lw + rb[t5_bucket(dist)].transpose(0, 2, 3, 1)[None]
        pw = masked_softmax(lw, valid_w[None, :, None, None, :])
        ow = jnp.einsum('bqghk,bkgd->bqghd', pw, vwb)
        gg = lax.dynamic_slice_in_dim(gates, q0, Q_BLOCK, axis=1).astype(f32)
        o = gg[..., 0:1] * oc + gg[..., 1:2] * osel + gg[..., 2:3] * ow
        return o.reshape(B_, Q_BLOCK, G_ * HPG_ * Dh).astype(q.dtype)

    out = lax.map(block, jnp.arange(S_ // Q_BLOCK))
    return out.transpose(1, 0, 2, 3).reshape(B_, S_, G_ * HPG_ * Dh)


def moe(h, w_router, b_router, w_up, b_up, w_down, b_down):
    B_, S_, D_ = h.shape
    N = B_ * S_
    NK = N * TOP_K
    xf = h.reshape(N, D_)
    logits = jnp.dot(xf, w_router, preferred_element_type=jnp.float32) + b_router.astype(jnp.float32)
    vals, idx = lax.top_k(logits, TOP_K)
    gate = jax.nn.softmax(vals, axis=-1)
    e_flat = idx.reshape(NK)
    tok_flat = jnp.arange(NK) // TOP_K
    gate_flat = gate.reshape(NK)
    order = jnp.argsort(e_flat)
    sorted_e = e_flat[order]
    counts = jnp.bincount(e_flat, length=N_EXPERTS)
    starts = jnp.cumsum(counts) - counts
    padded = (counts + EXPERT_ROWS - 1) // EXPERT_ROWS * EXPERT_ROWS
    pends = jnp.cumsum(padded)
    pstarts = pends - padded
    dest = pstarts[sorted_e] + (jnp.arange(NK) - starts[sorted_e])
    R = NK + N_EXPERTS * EXPERT_ROWS
    n_blk = R // EXPERT_ROWS
    row_tok = jnp.full((R,), N, jnp.int32).at[dest].set(tok_flat[order])
    row_gate = jnp.zeros((R,), jnp.float32).at[dest].set(gate_flat[order])
    blk_e = jnp.minimum(jnp.sum(jnp.arange(n_blk)[:, None] * EXPERT_ROWS >= pends[None, :], axis=1),
                        N_EXPERTS - 1)
    x_pad = jnp.concatenate([xf, jnp.zeros((1, D_), xf.dtype)], axis=0)
    xin = x_pad[row_tok].reshape(n_blk, EXPERT_ROWS, D_)

    def expert_block(args):
        xb, eb = args
        up = jnp.dot(xb, w_up[eb]) + b_up[eb]
        glu, lin = jnp.split(up, 2, axis=-1)
        glu = jnp.minimum(glu, SWIGLU_LIMIT)
        lin = jnp.clip(lin, -SWIGLU_LIMIT, SWIGLU_LIMIT)
        act = glu * jax.nn.sigmoid(SWIGLU_ALPHA * glu) * (lin + 1.0)
        return jnp.dot(act, w_down[eb]) + b_down[eb]

    yb = lax.map(expert_block, (xin, blk_e)).reshape(R, D_)
    out = jnp.zeros((N + 1, D_), h.dtype).at[row_tok].add(yb * row_gate[:, None].astype(h.dtype))
    return out[:N].reshape(B_, S_, D_)


def setup_inputs(seed: int = 0) -> dict:
    key = jax.random.key(seed)
    ks = jax.random.split(key, 24)
    nrm = lambda k, shape, s: jax.random.normal(k, shape, jnp.float32) * s
    L = DEPTH
    gate_base = jnp.concatenate([jnp.zeros((M_HEADS,), jnp.float32),
                                 jnp.linspace(3.0, 6.0, M_HEADS, dtype=jnp.float32)])
    return {
        "x": nrm(ks[0], (BATCH, SEQ, D_MODEL), 1.0),
        "c": nrm(ks[1], (BATCH, D_MODEL), 1.0),
        "w_ada": nrm(ks[2], (L, D_MODEL, 6 * D_MODEL), 0.5 * D_MODEL ** -0.5),
        "b_ada": nrm(ks[3], (L, 6 * D_MODEL), 0.02),
        "g_pre_mix": 1.0 + nrm(ks[4], (L, D_MODEL), 0.05),
        "g_post_mix": 1.0 + nrm(ks[5], (L, D_MODEL), 0.05),
        "g_pre_ffn": 1.0 + nrm(ks[6], (L, D_MODEL), 0.05),
        "g_post_ffn": 1.0 + nrm(ks[7], (L, D_MODEL), 0.05),
        "w_in": nrm(ks[8], (L, D_MODEL, D_IN), D_MODEL ** -0.5),
        "b_gates": gate_base + nrm(ks[9], (L, 2 * M_HEADS), 0.1),
        "conv_qk": nrm(ks[10], (L, CONV_W, 2 * M_HEADS * M_HEAD_DIM), CONV_W ** -0.5),
        "w_cmp_k": nrm(ks[11], (L, CMP_BLOCK, A_HEAD_DIM, A_HEAD_DIM), (CMP_BLOCK * A_HEAD_DIM) ** -0.5),
        "w_cmp_v": nrm(ks[12], (L, CMP_BLOCK, A_HEAD_DIM, A_HEAD_DIM), (CMP_BLOCK * A_HEAD_DIM) ** -0.5),
        "pos_cmp_k": nrm(ks[13], (L, CMP_BLOCK, A_HEAD_DIM), 0.1),
        "pos_cmp_v": nrm(ks[14], (L, CMP_BLOCK, A_HEAD_DIM), 0.1),
        "rel_bias": nrm(ks[15], (NUM_BUCKETS, A_HEADS), 0.5),
        "w_out": nrm(ks[16], (L, D_MIX, D_MODEL), D_MIX ** -0.5),
        "w_router": nrm(ks[17], (L, D_MODEL, N_EXPERTS), D_MODEL ** -0.5),
        "b_router": nrm(ks[18], (L, N_EXPERTS), 0.01),
        "w_up": nrm(ks[19], (L, N_EXPERTS, D_MODEL, 2 * D_EXPERT), D_MODEL ** -0.5),
        "b_up": nrm(ks[20], (L, N_EXPERTS, 2 * D_EXPERT), 0.01),
        "w_down": nrm(ks[21], (L, N_EXPERTS, D_EXPERT, D_MODEL), D_EXPERT ** -0.5),
        "b_down": nrm(ks[22], (L, N_EXPERTS, D_MODEL), 0.01),
    }


def reference(x, c, w_ada, b_ada, g_pre_mix, g_post_mix, g_pre_ffn, g_post_ffn, w_in, b_gates,
              conv_qk, w_cmp_k, w_cmp_v, pos_cmp_k, pos_cmp_v, rel_bias, w_out, w_router, b_router,
              w_up, b_up, w_down, b_down):
    B_, S_, _ = x.shape
    split_at = np.cumsum(SPLIT_SIZES)[:-1].tolist()
    for l in range(DEPTH):
        mod = jnp.dot(jax.nn.silu(c), w_ada[l]) + b_ada[l]
        sh1, sc1, gt1, sh2, sc2, gt2 = [m[:, None, :] for m in jnp.split(mod, 6, axis=-1)]
        h = rmsnorm(x, g_pre_mix[l]) * (1.0 + sc1) + sh1
        z = jnp.dot(h, w_in[l])
        (q_m, k_m, v_m, o_m, i_m, f_m, q_a, kc_a, vc_a, ks_a, vs_a, kw_a, vw_a, g_a) = \
            jnp.split(z, split_at, axis=-1)
        qk = jax.nn.silu(causal_depthwise_conv(jnp.concatenate([q_m, k_m], axis=-1), conv_qk[l]))
        q_m, k_m = jnp.split(qk, 2, axis=-1)
        mh = lambda a: a.reshape(B_, S_, M_HEADS, M_HEAD_DIM)
        y_m = mlstm(mh(q_m), mh(k_m), mh(v_m),
                    i_m + b_gates[l, :M_HEADS], f_m + b_gates[l, M_HEADS:])
        y_m = y_m * jax.nn.sigmoid(o_m)
        kv = lambda a: a.reshape(B_, S_, A_KV_GROUPS, A_HEAD_DIM)
        kc = compress(kv(kc_a), pos_cmp_k[l], w_cmp_k[l])
        vc = compress(kv(vc_a), pos_cmp_v[l], w_cmp_v[l])
        y_a = nsa(q_a.reshape(B_, S_, A_KV_GROUPS, A_HPG, A_HEAD_DIM), kc, vc,
                  kv(ks_a), kv(vs_a), kv(kw_a), kv(vw_a),
                  jax.nn.sigmoid(g_a).reshape(B_, S_, A_KV_GROUPS, A_HPG, 3), rel_bias)
        y = jnp.dot(jnp.concatenate([y_m, y_a], axis=-1), w_out[l])
        x = x + gt1 * rmsnorm(y, g_post_mix[l])
        h2 = rmsnorm(x, g_pre_ffn[l]) * (1.0 + sc2) + sh2
        y2 = moe(h2, w_router[l], b_router[l], w_up[l], b_up[l], w_down[l], b_down[l])
        x = x + gt2 * rmsnorm(y2, g_post_ffn[l])
    return x
```

```python
import numpy as np
from contextlib import ExitStack
import concourse.bass as bass
import concourse.mybir as mybir
from concourse.bass_utils import run_bass_kernel_spmd

F32 = mybir.dt.float32
BF16 = mybir.dt.bfloat16
AF = mybir.ActivationFunctionType
ALU = mybir.AluOpType
AX = mybir.AxisListType

COMPUTE = ("pe", "act", "dve", "pool")
NDMASEM = 24
NEG = -30000.0


class _Op:
    __slots__ = ("id", "eng", "fn", "deps", "dma", "needs_inc", "inc_idx", "dsem", "dval", "dprev")


class Sched:
    def __init__(self, nc, stack, same_engine_sync=True):
        self.nc = nc
        self.same = same_engine_sync
        self.sem = {e: stack.enter_context(nc.semaphore("sem_" + e)) for e in COMPUTE}
        self.dsem = [stack.enter_context(nc.semaphore("dsem%d" % i)) for i in range(NDMASEM)]
        self.bar = stack.enter_context(nc.semaphore("sem_bar"))
        self.cnt = {e: 0 for e in COMPUTE}
        self.ndma = 0
        self.dfinal = {}
        self.phase = 0
        self.scr = stack.enter_context(nc.sbuf_tensor("sched_scr", [128, 8], F32))
        self._reset()

    def _reset(self):
        self.ops = []
        self.lastw = {}
        self.readers = {}
        self.dma_hist = {}

    def _add(self, eng, fn, reads, writes, dma):
        deps = {}

        def add_dep(p):
            o = self.ops[p]
            if o.dma:
                deps[("d", p)] = p
            else:
                k = o.eng
                if k not in deps or deps[k] < p:
                    deps[k] = p

        for k in reads:
            for p in self.lastw.get(k, ()):
                add_dep(p)
        for k in writes:
            lw = self.lastw.get(k, ())
            if not (dma and lw and all(self.ops[p].dma for p in lw) and not self.readers.get(k)):
                for p in lw:
                    add_dep(p)
            for r in self.readers.get(k, ()):
                add_dep(r)
        op = _Op()
        op.id = len(self.ops)
        op.eng = eng
        op.fn = fn
        op.deps = sorted(set(deps.values()))
        op.dma = dma
        op.needs_inc = False
        op.inc_idx = 0
        op.dsem = None
        op.dval = 0
        op.dprev = None
        if dma:
            op.dsem = self.ndma % NDMASEM
            op.dval = 16 * (self.ndma // NDMASEM + 1)
            op.dprev = self.dma_hist.get(op.dsem)
            self.dma_hist[op.dsem] = op.id
            self.dfinal[op.dsem] = op.dval
            self.ndma += 1
        self.ops.append(op)
        for k in writes:
            lw = self.lastw.get(k, ())
            if dma and lw and all(self.ops[p].dma for p in lw) and not self.readers.get(k):
                self.lastw[k] = list(lw) + [op.id]
            else:
                self.lastw[k] = [op.id]
            self.readers[k] = []
        for k in reads:
            self.readers.setdefault(k, []).append(op.id)
        return op.id

    def op(self, eng, fn, reads=(), writes=()):
        return self._add(eng, fn, tuple(reads), tuple(writes), False)

    def dma(self, eng, fn, reads=(), writes=()):
        return self._add(eng, fn, tuple(reads), tuple(writes), True)

    def flush(self):
        nc = self.nc
        ops = self.ops
        for o in ops:
            for p in o.deps:
                po = ops[p]
                if po.dma:
                    continue
                if po.eng == o.eng and (po.eng == "pe" or not self.same):
                    continue
                po.needs_inc = True
        for o in ops:
            if not o.dma and o.needs_inc:
                self.cnt[o.eng] += 1
                o.inc_idx = self.cnt[o.eng]
        per = {e: [] for e in ("pe", "act", "dve", "pool", "sp")}
        for o in ops:
            per[o.eng].append(o)
        self.phase += 1
        phase = self.phase
        dfinal = dict(self.dfinal)
        sem, dsem, bar, scr = self.sem, self.dsem, self.bar, self.scr
        same = self.same

        def run(ename, e):
            waited_e = {f: 0 for f in COMPUTE}
            waited_d = {}
            for o in per[ename]:
                for p in o.deps:
                    po = ops[p]
                    if po.dma:
                        if waited_d.get(po.dsem, 0) < po.dval:
                            e.wait_ge(dsem[po.dsem], po.dval)
                            waited_d[po.dsem] = po.dval
                    else:
                        if not po.needs_inc:
                            continue
                        if po.eng == ename and (ename == "pe" or not same):
                            continue
                        if waited_e[po.eng] < po.inc_idx:
                            e.wait_ge(sem[po.eng], po.inc_idx)
                            waited_e[po.eng] = po.inc_idx
                if o.dma and o.dprev is not None:
                    po = ops[o.dprev]
                    if waited_d.get(po.dsem, 0) < po.dval:
                        e.wait_ge(dsem[po.dsem], po.dval)
                        waited_d[po.dsem] = po.dval
                inst = o.fn(e)
                if o.dma:
                    inst.then_inc(dsem[o.dsem], 16)
                elif o.needs_inc:
                    inst.then_inc(sem[o.eng], 1)
            if ename == "act":
                e.memzero(scr[0:1, 0:1]).then_inc(bar, 1)
            elif ename == "dve":
                e.memset(scr[0:1, 1:2], 0.0).then_inc(bar, 1)
            elif ename == "pool":
                e.memset(scr[0:1, 2:3], 0.0).then_inc(bar, 1)
            e.wait_ge(bar, 3 * phase)
            for s, v in dfinal.items():
                e.wait_ge(dsem[s], v)

        with nc.Block() as block:
            @block.tensor
            def _(e):
                run("pe", e)

            @block.scalar
            def _(e):
                run("act", e)

            @block.vector
            def _(e):
                run("dve", e)

            @block.gpsimd
            def _(e):
                run("pool", e)

            @block.sync
            def _(e):
                run("sp", e)
        self._reset()


D = 1024
T = 4096
TO = 2048
NT = 32
NTO = 16
D_IN = 3360
NE = 32
def KM(h): return 512 + 128 * h
def QM(h): return 128 * h
def VM(h): return 1024 + 128 * h
def OM(h): return 1536 + 128 * h
GIF = 2048
def QA(h8): return 2056 + 64 * h8
def KC(g): return 2568 + 64 * g
def VC(g): return 2696 + 64 * g
def KS(g): return 2824 + 64 * g
def KW(g): return 2952 + 64 * g
def VSW(g): return 3080 + 128 * g
GA = 3336


def _perm():
    p = list(range(D_IN))
    new = list(range(2568, 2952))
    new += list(range(3080, 3208))
    for g in range(2):
        new += list(range(2952 + 64 * g, 2952 + 64 * g + 64))
        new += list(range(3208 + 64 * g, 3208 + 64 * g + 64))
    p[2568:3336] = new
    return np.array(p)


def _t5_bucket(dist):
    n = np.maximum(dist, 0)
    nf = np.maximum(n, 1).astype(np.float32)
    large = 16 + (np.log(nf / np.float32(16)) / np.float32(np.log(128 / 16)) * np.float32(16)).astype(np.int32)
    large = np.minimum(large, 31)
    return np.where(n < 16, n, large)


def build_nc(stage=99, same=True):
    nc = bass.Bass("TRN2", target_bir_lowering=False)
    din = lambda name, shape: nc.dram_tensor(name, list(shape), F32, kind="ExternalInput").ap()
    xw = din("xw", [T, D])
    c_col = din("c_col", [128, 8])
    w_ada = din("w_ada", [D, 6 * D])
    b_ada_col = din("b_ada_col", [128, 48])
    b_ada_row = din("b_ada_row", [1, 6 * D])
    gpm_col = din("gpm_col", [128, 8])
    gpf_col = din("gpf_col", [128, 8])
    gpostm = din("gpostm", [1, D])
    gpostf = din("gpostf", [1, D])
    w_in = din("w_in", [D, D_IN])
    b_gates = din("b_gates", [1, 8])
    conv_col = din("conv_col", [128, 8, 4])
    wck = din("wck", [64, 32, 64])
    wcv = din("wcv", [64, 32, 64])
    posk = din("posk", [64, 32])
    posv = din("posv", [64, 32])
    w_out = din("w_out", [D, D])
    w_router = din("w_router", [D, NE])
    b_router = din("b_router", [1, NE])
    w_up = din("w_up", [NE, D, 2 * D])
    b_up_col = din("b_up_col", [128, NE, 16])
    w_down = din("w_down", [NE, D, D])
    b_down = din("b_down", [NE, D])
    flag = din("flag", [128, 1])
    t_sb = din("t_sb", [2, 2, 128, 512])
    t_cst = din("t_cst", [2, 1, 512])
    t_wb = din("t_wb", [2, 128, 10, 512])
    t_cb = din("t_cb", [2, 16, 128, 2, 512])
    t_add = din("t_add", [128, 16, 64])
    t_E = din("t_E", [65, 32, 128])
    t_ovl = din("t_ovl", [128, 2, 64])
    t_tri = din("t_tri", [128, 128])
    out = nc.dram_tensor("out", [TO, D], F32, kind="ExternalOutput").ap()
    dbg = None
    if stage < 99:
        dbg = nc.dram_tensor("dbg", [128, 8 * TO], F32, kind="ExternalOutput").ap()

    xw_t = xw.rearrange("(n p) d -> n p d", p=128)
    out_t = out.rearrange("(n p) d -> n p d", p=128)

    with ExitStack() as gs:
        S = Sched(nc, gs, same_engine_sync=same)
        _uid = [0]

        def _nm(name):
            _uid[0] += 1
            return "%s_u%d" % (name, _uid[0])

        SB = lambda st, name, shape, dt: st.enter_context(nc.sbuf_tensor(_nm(name), list(shape), dt))
        PS = lambda st, name, shape, dt: st.enter_context(nc.psum_tensor(_nm(name), list(shape), dt))

        def mm(o, l, r, start, stop, rd=(), wr=(), skip=False):
            if skip:
                S.op("pe", lambda e: e.matmul(o, lhsT=l, rhs=r, start=start, stop=stop, skip_group_check=True), rd, wr)
            else:
                S.op("pe", lambda e: e.matmul(o, lhsT=l, rhs=r, start=start, stop=stop), rd, wr)

        def tr(o, i, idn, rd=(), wr=()):
            S.op("pe", lambda e: e.transpose(o, i, idn), rd, wr)

        def act(o, i, func, rd=(), wr=(), **kw):
            S.op("act", lambda e: e.activation(out=o, in_=i, func=func, **kw), rd, wr)

        def ts(eng, o, i, s1, s2, op0, op1=None, rd=(), wr=()):
            if op1 is None:
                S.op(eng, lambda e: e.tensor_scalar(out=o, in0=i, scalar1=s1, scalar2=None, op0=op0), rd, wr)
            else:
                S.op(eng, lambda e: e.tensor_scalar(out=o, in0=i, scalar1=s1, scalar2=s2, op0=op0, op1=op1), rd, wr)

        def tt(eng, o, a, b, op, rd=(), wr=()):
            S.op(eng, lambda e: e.tensor_tensor(out=o, in0=a, in1=b, op=op), rd, wr)

        def stt(eng, o, a, sc, b, op0, op1, rd=(), wr=()):
            S.op(eng, lambda e: e.scalar_tensor_tensor(out=o, in0=a, scalar=sc, in1=b, op0=op0, op1=op1), rd, wr)

        def cp(eng, o, i, rd=(), wr=()):
            S.op(eng, lambda e: e.tensor_copy(out=o, in_=i), rd, wr)

        def ms(eng, o, v, rd=(), wr=()):
            S.op(eng, lambda e: e.memset(o, v), rd, wr)

        def dma(q, o, i, rd=(), wr=()):
            S.dma(q, lambda e: e.dma_start(out=o, in_=i), rd, wr)

        def rcp(o, i, rd=(), wr=()):
            S.op("dve", lambda e: e.reciprocal(out=o, in_=i), rd, wr)

        identb = SB(gs, "identb", [128, 128], BF16)
        identf = SB(gs, "identf", [128, 128], F32)
        tri_f = SB(gs, "tri_f", [128, 128], F32)
        tri_b = SB(gs, "tri_b", [128, 128], BF16)
        ones_f = SB(gs, "ones_f", [128, 128], F32)
        ones_b = SB(gs, "ones_b", [128, 128], BF16)
        A1 = SB(gs, "A1", [128, 8], F32)
        B1 = SB(gs, "B1", [128, 8], F32)
        A2 = SB(gs, "A2", [128, 8], F32)
        B2 = SB(gs, "B2", [128, 8], F32)
        GT1 = SB(gs, "GT1", [128, D], F32)
        GT2 = SB(gs, "GT2", [128, D], F32)
        flg = SB(gs, "flg", [128, 1], F32)
        junk = SB(gs, "junk", [128, D], BF16)
        epsc = SB(gs, "epsc", [128, 1], F32)
        xn = [SB(gs, "xng%d" % i, [128, D], BF16) for i in range(2)]
        junkf = SB(gs, "junkf", [128, D], F32)

        def sumsq(src, srckeys, dst, dkey):
            S.op("act", lambda e: e.activation(out=junkf[:], in_=src, func=AF.Square), list(srckeys), ["junkf"])
            S.op("dve", lambda e: e.tensor_reduce(out=dst, in_=junkf[:], axis=AX.X, op=ALU.add), ["junkf", dkey], [dkey])
        hT = SB(gs, "hT", [128, 8, T], BF16)
        h2T = hT

        with ExitStack() as st:
            wada = [SB(st, "wada%d" % i, [128, 8, 1536], BF16) for i in range(2)]
            ccol = SB(st, "ccol", [128, 8], F32)
            scb = SB(st, "scb", [128, 8], BF16)
            scB = SB(st, "scB", [128, 8, 128], BF16)
            bcol = SB(st, "bcol", [128, 48], F32)
            modc = SB(st, "modc", [128, 48], F32)
            gpm = SB(st, "gpm", [128, 8], F32)
            gpf = SB(st, "gpf", [128, 8], F32)
            brow = SB(st, "brow", [128, 2, D], F32)
            grow = SB(st, "grow", [128, 2, D], F32)
            psmod = PS(st, "psmod", [128, 48], F32)
            psg = [PS(st, "psg%d" % i, [128, 512], F32) for i in range(4)]

            ms("pool", identf[:], 1.0, rd=[], wr=["tri_tmp"])
            S.op("pool", lambda e: e.affine_select(out=identf[:], in_=identf[:], pattern=[[-1, 128]],
                                                   compare_op=ALU.is_equal, fill=0.0, base=0, channel_multiplier=1),
                 ["tri_tmp"], ["tri_tmp"])
            cp("dve", identb[:], identf[:], rd=["tri_tmp"], wr=["identb"])
            dma("sp", tri_f[:], t_tri, wr=["tri_f"])
            cp("dve", tri_b[:], tri_f[:], rd=["tri_f"], wr=["tri_b"])
            ms("pool", epsc[:], 1e-6, wr=["epsc"])
            ms("pool", ones_f[:], 1.0, wr=["ones_f"])
            ms("pool", ones_b[:], 1.0, wr=["ones_b"])
            dma("sp", flg[:], flag, wr=["flg"])
            dma("sp", ccol[:], c_col, wr=["ccol"])
            dma("sp", bcol[:], b_ada_col, wr=["bcol"])
            dma("sp", gpm[:], gpm_col, wr=["gpm"])
            dma("sp", gpf[:], gpf_col, wr=["gpf"])
            dma("sp", brow[:, 0, :], b_ada_row[:, 2048:3072].partition_broadcast(128), wr=["brow0"])
            dma("sp", brow[:, 1, :], b_ada_row[:, 5120:6144].partition_broadcast(128), wr=["brow1"])
            dma("sp", grow[:, 0, :], gpostm.partition_broadcast(128), wr=["grow0"])
            dma("sp", grow[:, 1, :], gpostf.partition_broadcast(128), wr=["grow1"])
            act(scb[:], ccol[:], AF.Silu, rd=["ccol"], wr=["scb"])
            for kc in range(8):
                cp("dve", scB[:, kc, :], scb[:, kc:kc + 1].to_broadcast([128, 128]), rd=["scb"], wr=["scB"])
            wada_v = w_ada.rearrange("(k p) f -> p k f", p=128)
            for pc in range(4):
                wb = wada[pc % 2]
                key = "wada%d" % (pc % 2)
                for kc in range(8):
                    dma("pool", wb[:, kc, :], wada_v[:, kc, pc * 1536:(pc + 1) * 1536], wr=[key])
                for fl in range(12):
                    fc = pc * 12 + fl
                    for kc in range(8):
                        mm(psmod[:, fc:fc + 1], wb[:, kc, fl * 128:(fl + 1) * 128], scb[:, kc:kc + 1],
                           kc == 0, kc == 7, rd=[key, "scb"], wr=["psmod"])
                if pc in (1, 3):
                    j = 0 if pc == 1 else 1
                    for hb in range(2):
                        pt = psg[j * 2 + hb]
                        for kc in range(8):
                            mm(pt[:], scB[:, kc, :], wb[:, kc, 512 + hb * 512:512 + (hb + 1) * 512],
                               kc == 0, kc == 7, rd=[key, "scB"], wr=["psg%d" % (j * 2 + hb)])
                        dst = (GT1 if j == 0 else GT2)
                        tt("dve", dst[:, hb * 512:(hb + 1) * 512], pt[:], brow[:, j, hb * 512:(hb + 1) * 512], ALU.add,
                           rd=["psg%d" % (j * 2 + hb), "brow%d" % j], wr=["GT%d%d" % (j, hb)])
                        tt("dve", dst[:, hb * 512:(hb + 1) * 512], dst[:, hb * 512:(hb + 1) * 512],
                           grow[:, j, hb * 512:(hb + 1) * 512], ALU.mult,
                           rd=["GT%d%d" % (j, hb), "grow%d" % j], wr=["GT%d%d" % (j, hb)])
            tt("dve", modc[:], psmod[:], bcol[:], ALU.add, rd=["psmod", "bcol"], wr=["modc"])
            stt("dve", A1[:], modc[:, 8:16], 1.0, gpm[:], ALU.add, ALU.mult, rd=["modc", "gpm"], wr=["A1"])
            cp("dve", B1[:], modc[:, 0:8], rd=["modc"], wr=["B1"])
            stt("dve", A2[:], modc[:, 32:40], 1.0, gpf[:], ALU.add, ALU.mult, rd=["modc", "gpf"], wr=["A2"])
            cp("dve", B2[:], modc[:, 24:32], rd=["modc"], wr=["B2"])
            S.flush()

        def norm_transpose(src_f32, srckey, dstT, col0, Acol, Bcol, xn, pT, ss, idx, tag):
            import os
            STEP = int(os.environ.get("DBG_STEP", 99))
            if os.environ.get("DBG_XNJ"):
                xn = junk
            k = "%s%d" % (tag, idx % 2)
            if STEP < 2:
                return
            sumsq(src_f32, [srckey], ss[:, 0:1], "ss" + k)
            if STEP < 3:
                return
            act(ss[:, 1:2], ss[:, 0:1], AF.Sqrt, rd=["ss" + k], wr=["ss" + k], scale=1.0 / D, bias=epsc[:, 0:1])
            rcp(ss[:, 2:3], ss[:, 1:2], rd=["ss" + k], wr=["ss" + k])
            if STEP < 4:
                return
            act(xn[:], src_f32, AF.Identity, rd=[srckey, "ss" + k], wr=["xn" + k], scale=ss[:, 2:3])
            if STEP < 5:
                return
            for kc in range(8):
                tr(pT[:, kc * 128:(kc + 1) * 128], xn[:, kc * 128:(kc + 1) * 128], identb[:], rd=["xn" + k], wr=["pT" + k])
            if STEP < 6:
                return
            for kc in range(8):
                o = dstT[:, kc, col0:col0 + 128]
                i = pT[:, kc * 128:(kc + 1) * 128]
                if idx % 2 == 0:
                    act(o, i, AF.Identity, rd=["pT" + k], wr=[], scale=Acol[:, kc:kc + 1], bias=Bcol[:, kc:kc + 1])
                else:
                    ts("dve", o, i, Acol[:, kc:kc + 1], Bcol[:, kc:kc + 1], ALU.mult, ALU.add, rd=["pT" + k], wr=[])

        if stage == 0:
            dma("sp", dbg[:, 0:1024], GT1[:])
            dma("sp", dbg[:, 1024:2048], GT2[:])
            dma("sp", dbg[:, 2048:2056], A1[:])
            dma("sp", dbg[:, 2056:2064], B1[:])
            dma("sp", dbg[:, 2064:2072], A2[:])
            dma("sp", dbg[:, 2072:2080], B2[:])
            S.flush()
            return nc
        with ExitStack() as mix:
            yT = SB(mix, "yT", [128, 8, TO], BF16)
            with ExitStack() as st:
                xt = [SB(st, "xt%d" % i, [128, D], F32) for i in range(3)]
                ssb = [SB(st, "ssb%d" % i, [128, 4], F32) for i in range(2)]
                pT = [PS(st, "pT%d" % i, [128, D], BF16) for i in range(2)]
                import os
                for wt in range(int(os.environ.get("DBG_NT", NT))):
                    dma("sp", xt[wt % 3][:], xw_t[wt], wr=["xt%d" % (wt % 3)])
                    norm_transpose(xt[wt % 3][:], "xt%d" % (wt % 3), hT, wt * 128, A1, B1, xn[wt % 2], pT[wt % 2],
                                   ssb[wt % 2], wt, "b")
                S.flush()
            if stage == 1:
                with ExitStack() as st:
                    tmp = SB(st, "dbgt", [128, 8, TO], F32)
                    for kc in range(8):
                        cp("dve", tmp[:, kc, :], hT[:, kc, TO:T], wr=["t%d" % kc])
                        dma("sp", dbg[:, kc * TO:(kc + 1) * TO], tmp[:, kc, :], rd=["t%d" % kc])
                    S.flush()

            if stage >= 2:
              with ExitStack() as st:
                wing = SB(st, "win_g", [128, 8, 8], BF16)
                w_in_v = w_in.rearrange("(k p) f -> p k f", p=128)
                dma("pool", wing[:], w_in_v[:, :, GIF:GIF + 8], wr=["win"])
                winh = [SB(st, "win_h%d" % h_, [128, 8, 512], BF16) for h_ in range(2)]

                def load_winh(h_):
                    for jj, c0 in enumerate((QM(h_), KM(h_), VM(h_), OM(h_))):
                        dma("pool", winh[h_ % 2][:, :, jj * 128:(jj + 1) * 128], w_in_v[:, :, c0:c0 + 128], wr=["winh%d" % (h_ % 2)])

                load_winh(0)
                bg = SB(st, "bg", [128, 8], F32)
                dma("sp", bg[:], b_gates.partition_broadcast(128), wr=["bg"])
                convw = SB(st, "convw", [128, 8, 4], F32)
                dma("sp", convw[:], conv_col, wr=["convw"])
                gpre = SB(st, "gpre", [128, NT, 8], F32)
                lf = SB(st, "lf", [128, NT, 4], F32)
                gcs = SB(st, "gcs", [128, NT, 4], F32)
                ea = SB(st, "ea", [128, NT, 4], F32)
                qs = SB(st, "qs", [128, NT, 4], F32)
                eG = SB(st, "eG", [128, NT, 4], F32)
                psgt = PS(st, "psgt", [128, NT, 8], F32)
                for wt in range(NT):
                    for kc in range(8):
                        mm(psgt[:, wt, :], hT[:, kc, wt * 128:(wt + 1) * 128], wing[:, kc, :], kc == 0, kc == 7,
                           rd=["win"], wr=["psgt"])
                tt("dve", gpre[:], psgt[:], bg[:].unsqueeze(1).to_broadcast([128, NT, 8]), ALU.add, rd=["psgt", "bg"], wr=["gpre"])
                act(lf[:], gpre[:, :, 4:8], AF.Exp, rd=["gpre"], wr=["lf"], scale=-1.0)
                act(lf[:], lf[:], AF.Ln, rd=["lf"], wr=["lf"], bias=1.0)
                ts("dve", lf[:], lf[:], -1.0, None, ALU.mult, rd=["lf"], wr=["lf"])
                lf2 = lf[:].rearrange("p n h -> p (n h)")
                mm(psgt[:].rearrange("p n h -> p (n h)")[:, 0:128], tri_f[:], lf2, True, True, rd=["lf", "tri_f", "gpre"], wr=["psgt"])
                mm(psgt[:].rearrange("p n h -> p (n h)")[:, 128:256], ones_f[:], lf2, True, True, rd=["lf", "ones_f"], wr=["psgt"])
                psv = psgt[:].rearrange("p n h -> p (n h)")
                act(gcs[:].rearrange("p n h -> p (n h)"), psv[:, 0:128], AF.Copy, rd=["psgt"], wr=["gcs"])
                act(eG[:].rearrange("p n h -> p (n h)"), psv[:, 128:256], AF.Exp, rd=["psgt"], wr=["eG"])
                tt("dve", ea[:], gpre[:, :, 0:4], gcs[:], ALU.subtract, rd=["gpre", "gcs"], wr=["ea"])
                act(ea[:], ea[:], AF.Exp, rd=["ea"], wr=["ea"])
                act(qs[:], gcs[:], AF.Exp, rd=["gcs"], wr=["qs"])
                ts("dve", qs[:], qs[:], float(128 ** -0.5), None, ALU.mult, rd=["qs"], wr=["qs"])
                ts("dve", eG[:, 15, :], eG[:, 15, :], flg[:, 0:1], None, ALU.mult, rd=["eG", "flg"], wr=["eG"])
                S.flush()

                for hd in range(4):
                    with ExitStack() as hs:
                        win = winh[hd % 2]
                        if hd + 1 < 4:
                            load_winh(hd + 1)
                        kraw = SB(hs, "kraw", [128, T + 3], BF16)
                        qraw = SB(hs, "qraw", [128, TO + 3], BF16)
                        ctmp = [SB(hs, "ctmp%d" % i, [128, 1024], F32) for i in range(2)]
                        kT = SB(hs, "kT", [128, T], BF16)
                        qT = SB(hs, "qT", [128, TO], BF16)
                        vt = SB(hs, "vt", [128, NT, 129], BF16)
                        ktm = SB(hs, "ktm", [128, NT, 128], BF16)
                        og = SB(hs, "og", [128, NTO, 128], BF16)
                        Cst = SB(hs, "Cst", [128, 129], F32)
                        Ct = SB(hs, "Ct", [128, 129], F32)
                        Cbf = [SB(hs, "Cbf%d" % i, [128, 129], BF16) for i in range(2)]
                        Sm = [SB(hs, "Sm%d" % i, [128, 128], BF16) for i in range(2)]
                        sm4 = [SB(hs, "sm4%d" % i, [128, 4], F32) for i in range(2)]
                        ytm = [SB(hs, "ytm%d" % i, [128, 128], BF16) for i in range(2)]
                        pp = [PS(hs, "pp%d" % i, [128, 512], F32) for i in range(3)]
                        pCf = [PS(hs, "pCf%d" % i, [128, 512], F32) for i in range(2)]
                        pC = [t_[:, 0:129] for t_ in pCf]
                        pYf = [PS(hs, "pYf%d" % i, [128, 1024], BF16) for i in range(2)]
                        pY = [t_[:, 0:128] for t_ in pYf]
                        npp = [0]

                        def nextpp():
                            i = npp[0] % 3
                            npp[0] += 1
                            return pp[i], "pp%d" % i

                        ms("dve", kraw[:, 0:3], 0.0, wr=["kraw_h"])
                        for blk in range(8):
                            p, pk = nextpp()
                            for kc in range(8):
                                mm(p[:], win[:, kc, 128:256], hT[:, kc, blk * 512:(blk + 1) * 512], kc == 0, kc == 7, rd=["win"], wr=[pk])
                            act(kraw[:, 3 + blk * 512:3 + (blk + 1) * 512], p[:], AF.Copy, rd=[pk], wr=["kraw%d" % blk])
                        ts("dve", kraw[:, 3 + 2045:3 + 2048], kraw[:, 3 + 2045:3 + 2048], flg[:, 0:1], None, ALU.mult,
                           rd=["kraw3"], wr=["kraw3"])
                        p, pk = nextpp()
                        for kc in range(8):
                            mm(p[:, 0:128], win[:, kc, 0:128], hT[:, kc, 1920:2048], kc == 0, kc == 7, rd=["win"], wr=[pk])
                        ts("dve", qraw[:, 0:3], p[:, 125:128], flg[:, 0:1], None, ALU.mult, rd=[pk], wr=["qraw_h"])
                        for blk in range(4):
                            p, pk = nextpp()
                            for kc in range(8):
                                mm(p[:], win[:, kc, 0:128], hT[:, kc, TO + blk * 512:TO + (blk + 1) * 512],
                                   kc == 0, kc == 7, rd=["win"], wr=[pk])
                            act(qraw[:, 3 + blk * 512:3 + (blk + 1) * 512], p[:], AF.Copy, rd=[pk], wr=["qraw%d" % blk])
                        nct = [0]

                        def conv(raw, rawkeys, dstT, nblk, wch, eng):
                            for b in range(nblk):
                                ci = nct[0] % 2
                                nct[0] += 1
                                ck = "ctmp%d" % ci
                                c_ = ctmp[ci]
                                ts(eng, c_[:], raw[:, b * 1024:b * 1024 + 1024], convw[:, wch, 0:1], None, ALU.mult,
                                   rd=rawkeys, wr=[ck])
                                for j in range(1, 4):
                                    stt(eng, c_[:], raw[:, b * 1024 + j:b * 1024 + j + 1024], convw[:, wch, j:j + 1], c_[:],
                                        ALU.mult, ALU.add, rd=rawkeys + [ck], wr=[ck])
                                act(dstT[:, b * 1024:(b + 1) * 1024], c_[:], AF.Silu, rd=[ck], wr=["cv%d_%d" % (wch, b)])

                        conv(kraw, ["kraw%d" % b for b in range(8)] + ["kraw_h"], kT, 4, 4 + hd, "dve")
                        conv(qraw, ["qraw%d" % b for b in range(4)] + ["qraw_h"], qT, 2, hd, "dve")
                        kTkeys = ["cv%d_%d" % (4 + hd, b) for b in range(4)]
                        qTkeys = ["cv%d_%d" % (hd, b) for b in range(2)]
                        for wt in range(NT):
                            if wt % 4 == 0:
                                p, pk = nextpp()
                            o = p[:, (wt % 4) * 128:(wt % 4 + 1) * 128]
                            for kc in range(8):
                                mm(o, hT[:, kc, wt * 128:(wt + 1) * 128], win[:, kc, 256:384], kc == 0, kc == 7, rd=["win"], wr=[pk])
                            ts("dve", vt[:, wt, 0:128], o, ea[:, wt, hd:hd + 1], None, ALU.mult, rd=[pk], wr=["vt%d" % wt])
                            cp("pool", vt[:, wt, 128:129], ea[:, wt, hd:hd + 1], wr=["vt1_%d" % wt])
                        for i in range(NTO):
                            if i % 4 == 0:
                                p, pk = nextpp()
                            o = p[:, (i % 4) * 128:(i % 4 + 1) * 128]
                            for kc in range(8):
                                mm(o, hT[:, kc, TO + i * 128:TO + (i + 1) * 128], win[:, kc, 384:512], kc == 0, kc == 7, rd=["win"], wr=[pk])
                            act(og[:, i, :], o, AF.Sigmoid, rd=[pk], wr=["og%d" % i])
                        for wt in range(NT - 1):
                            py = pY[wt % 2]
                            tr(py, kT[:, wt * 128:(wt + 1) * 128], identb[:], rd=[kTkeys[wt // 8]], wr=["pY%d" % (wt % 2)])
                            act(ktm[:, wt, :], py, AF.Copy, rd=["pY%d" % (wt % 2)], wr=["ktm%d" % wt])
                        ms("dve", Cst[:], 0.0, wr=["Cst"])
                        ms("dve", Cbf[0][:], 0.0, wr=["Cbf0"])
                        for c in range(NT):
                            cb = Cbf[c % 2]
                            cbk = "Cbf%d" % (c % 2)
                            if c >= 16:
                                i = c - 16
                                p, pk = nextpp()
                                mm(p[:, 0:128], kT[:, c * 128:(c + 1) * 128], qT[:, i * 128:(i + 1) * 128], True, True,
                                   rd=[kTkeys[c // 8], qTkeys[i // 8]], wr=[pk])
                                sm = Sm[i % 2]
                                smk = "Sm%d" % (i % 2)
                                tt("dve", sm[:], p[:, 0:128], tri_f[:], ALU.mult, rd=[pk], wr=[smk])
                                pn = p[:, 256:385]
                                mm(pn, sm[:], vt[:, c, :], True, False, rd=[smk, "vt%d" % c, "vt1_%d" % c], wr=[pk])
                                mm(pn, qT[:, i * 128:(i + 1) * 128], cb[:], False, True, rd=[cbk, qTkeys[i // 8]], wr=[pk])
                                s4 = sm4[i % 2]
                                s4k = "sm4%d" % (i % 2)
                                ts("dve", s4[:, 0:1], p[:, 384:385], qs[:, c, hd:hd + 1], None, ALU.mult, rd=[pk], wr=[s4k])
                                ts("dve", s4[:, 3:4], s4[:, 0:1], -1.0, None, ALU.mult, rd=[s4k], wr=[s4k])
                                tt("dve", s4[:, 0:1], s4[:, 0:1], s4[:, 3:4], ALU.max, rd=[s4k], wr=[s4k])
                                ts("dve", s4[:, 0:1], s4[:, 0:1], 1.0, None, ALU.max, rd=[s4k], wr=[s4k])
                                rcp(s4[:, 1:2], s4[:, 0:1], rd=[s4k], wr=[s4k])
                                tt("dve", s4[:, 2:3], s4[:, 1:2], qs[:, c, hd:hd + 1], ALU.mult, rd=[s4k], wr=[s4k])
                                yt_ = ytm[i % 2]
                                ytk = "ytm%d" % (i % 2)
                                stt("dve", yt_[:], p[:, 256:384], s4[:, 2:3], og[:, i, :], ALU.mult, ALU.mult,
                                    rd=[pk, s4k, "og%d" % i], wr=[ytk])
                                py = pY[i % 2]
                                tr(py, yt_[:], identb[:], rd=[ytk], wr=["pY%d" % (i % 2)])
                                act(yT[:, hd, i * 128:(i + 1) * 128], py, AF.Copy, rd=["pY%d" % (i % 2)], wr=[])
                            if c < NT - 1:
                                pc_ = pC[c % 2]
                                pck = "pC%d" % (c % 2)
                                mm(pc_, ktm[:, c, :], vt[:, c, :], True, True, rd=["ktm%d" % c, "vt%d" % c, "vt1_%d" % c], wr=[pck])
                                tt("dve", Ct[:], pc_, Cst[:], ALU.add, rd=[pck, "Cst"], wr=["Ct"])
                                ts("dve", Cst[:], Ct[:], eG[:, c, hd:hd + 1], None, ALU.mult, rd=["Ct"], wr=["Cst"])
                                nb = Cbf[(c + 1) % 2]
                                act(nb[:], Ct[:], AF.Identity, rd=["Ct"], wr=["Cbf%d" % ((c + 1) % 2)], scale=eG[:, c, hd:hd + 1])
                        S.flush()
            if stage == 2:
                with ExitStack() as st:
                    tmp = SB(st, "dbgt", [128, 8, TO], F32)
                    for kc in range(8):
                        if kc < 4:
                            cp("dve", tmp[:, kc, :], yT[:, kc, :], wr=["t%d" % kc])
                        else:
                            ms("dve", tmp[:, kc, :], 0.0, wr=["t%d" % kc])
                        dma("sp", dbg[:, kc * TO:(kc + 1) * TO], tmp[:, kc, :], rd=["t%d" % kc])
                    S.flush()

            if stage >= 3:
              w_in_v = w_in.rearrange("(k p) f -> p k f", p=128)
              for g in range(2):
                with ExitStack() as gsx:
                    qaT = SB(gsx, "qaT", [64, 4, TO], BF16)
                    ksT = SB(gsx, "ksT", [64, T], BF16)
                    kwT = SB(gsx, "kwT", [64, T], BF16)
                    vsa = SB(gsx, "vsa", [128, NT, 65], BF16)
                    vwa = SB(gsx, "vwa", [128, NT, 65], BF16)
                    kcT = SB(gsx, "kcT", [64, 256], BF16)
                    vca = SB(gsx, "vca", [128, 2, 65], BF16)
                    gat = SB(gsx, "gat", [128, NTO, 12], F32)
                    with ExitStack() as st:
                        win = SB(st, "win_a", [128, 8, 652], BF16)
                        dma("pool", win[:, :, 0:256], w_in_v[:, :, QA(4 * g):QA(4 * g) + 256], wr=["win"])
                        dma("pool", win[:, :, 256:320], w_in_v[:, :, KC(g):KC(g) + 64], wr=["win"])
                        dma("pool", win[:, :, 320:384], w_in_v[:, :, VC(g):VC(g) + 64], wr=["win"])
                        dma("pool", win[:, :, 384:448], w_in_v[:, :, KS(g):KS(g) + 64], wr=["win"])
                        dma("pool", win[:, :, 448:512], w_in_v[:, :, KW(g):KW(g) + 64], wr=["win"])
                        dma("pool", win[:, :, 512:640], w_in_v[:, :, VSW(g):VSW(g) + 128], wr=["win"])
                        dma("pool", win[:, :, 640:652], w_in_v[:, :, GA + 12 * g:GA + 12 * g + 12], wr=["win"])
                        kcr = SB(st, "kcr", [64, T + 32], BF16)
                        vcr = SB(st, "vcr", [64, T + 32], BF16)
                        wk = SB(st, "wk", [64, 32, 64], BF16)
                        wv = SB(st, "wv", [64, 32, 64], BF16)
                        pk_ = SB(st, "pk_", [64, 32], BF16)
                        pv_ = SB(st, "pv_", [64, 32], BF16)
                        kcb = SB(st, "kcb", [64, 1], F32)
                        cvr = SB(st, "cvr", [1, 64], BF16)
                        dma("pool", wk[:], wck, wr=["wk"])
                        dma("pool", wv[:], wcv, wr=["wv"])
                        dma("pool", pk_[:], posk, wr=["pk_"])
                        dma("pool", pv_[:], posv, wr=["pv_"])
                        ms("dve", kcr[:, T:T + 32], 0.0, wr=["kcr_t"])
                        ms("dve", vcr[:, T:T + 32], 0.0, wr=["vcr_t"])
                        ms("pool", vsa[:, :, 64:65], 1.0, wr=["vsa1"])
                        ms("pool", vwa[:, :, 64:65], 1.0, wr=["vwa1"])
                        ms("pool", vca[:, :, 64:65], 1.0, wr=["vca1"])
                        ms("pool", kcT[:, 255:256], 0.0, wr=["kcT1"])
                        pp = [PS(st, "pp%d" % i, [128, 512], F32) for i in range(6)]
                        npp = [0]

                        def nextpp():
                            i = npp[0] % 6
                            npp[0] += 1
                            return pp[i], "pp%d" % i

                        ne = [0]

                        def evac(o, i, rd, wr, scale=None):
                            if int(rd[0][2:]) % 2 == 0:
                                if scale is None:
                                    act(o, i, AF.Copy, rd=rd, wr=wr)
                                else:
                                    act(o, i, AF.Identity, rd=rd, wr=wr, scale=scale)
                            else:
                                if scale is None:
                                    cp("dve", o, i, rd=rd, wr=wr)
                                else:
                                    ts("dve", o, i, scale, None, ALU.mult, rd=rd, wr=wr)

                        for h in range(4):
                            for blk in range(4):
                                p, pk = nextpp()
                                for kc in range(8):
                                    mm(p[0:64, :], win[:, kc, h * 64:(h + 1) * 64], hT[:, kc, TO + blk * 512:TO + (blk + 1) * 512],
                                       kc == 0, kc == 7, rd=["win"], wr=[pk])
                                evac(qaT[:, h, blk * 512:(blk + 1) * 512], p[0:64, :], [pk], [], scale=0.125)
                        for (dst, c0, nm) in ((kcr, 256, "kcr"), (vcr, 320, "vcr"), (ksT, 384, "ksT"), (kwT, 448, "kwT")):
                            for blk in range(8):
                                p, pk = nextpp()
                                for kc in range(8):
                                    mm(p[0:64, :], win[:, kc, c0:c0 + 64], hT[:, kc, blk * 512:(blk + 1) * 512], kc == 0, kc == 7,
                                       rd=["win"], wr=[pk])
                                evac(dst[:, blk * 512:(blk + 1) * 512], p[0:64, :], [pk], [nm])
                        for wt in range(NT):
                            if wt % 4 == 0:
                                p, pk = nextpp()
                            o = p[:, (wt % 4) * 128:(wt % 4 + 1) * 128]
                            for kc in range(8):
                                mm(o, hT[:, kc, wt * 128:(wt + 1) * 128], win[:, kc, 512:640], kc == 0, kc == 7, rd=["win"], wr=[pk])
                            evac(vsa[:, wt, 0:64], o[:, 0:64], [pk], [])
                            evac(vwa[:, wt, 0:64], o[:, 64:128], [pk], [])
                        for i in range(NTO):
                            if i % 4 == 0:
                                p, pk = nextpp()
                            o = p[:, (i % 4) * 16:(i % 4) * 16 + 12]
                            for kc in range(8):
                                mm(o, hT[:, kc, TO + i * 128:TO + (i + 1) * 128], win[:, kc, 640:652], kc == 0, kc == 7, rd=["win"], wr=[pk])
                            act(gat[:, i, :], o, AF.Sigmoid, rd=[pk], wr=[])
                        p, pk = nextpp()
                        for l in range(32):
                            mm(p[0:64, 0:255], wk[:, l, :], kcr[:, l:l + 4065:16], l == 0, l == 31,
                               rd=["wk", "kcr", "kcr_t"], wr=[pk])
                        p2, pk2 = nextpp()
                        for l in range(32):
                            mm(p2[0:64, 0:1], wk[:, l, :], pk_[:, l:l + 1], l == 0, l == 31, rd=["wk", "pk_"], wr=[pk2])
                        cp("dve", kcb[:], p2[0:64, 0:1], rd=[pk2], wr=["kcb"])
                        ts("dve", kcT[:, 0:255], p[0:64, 0:255], kcb[:, 0:1], None, ALU.add, rd=[pk, "kcb"], wr=[])
                        p3, pk3 = nextpp()
                        for l in range(32):
                            mm(p3[0:1, 0:64], pv_[:, l:l + 1], wv[:, l, :], l == 0, l == 31, rd=["wv", "pv_"], wr=[pk3])
                        cp("dve", cvr[:], p3[0:1, 0:64], rd=[pk3], wr=["cvr"])
                        for it in range(2):
                            p, pk = nextpp()
                            for l in range(32):
                                mm(p[:, 0:64], vcr[:, it * 2048 + l:it * 2048 + l + 2033:16], wv[:, l, :], l == 0, False,
                                   rd=["wv", "vcr", "vcr_t"], wr=[pk])
                            mm(p[:, 0:64], ones_b[0:1, :], cvr[:], False, True, rd=["cvr"], wr=[pk])
                            cp("dve", vca[:, it, 0:64], p[:, 0:64], rd=[pk], wr=[])
                        S.flush()

                    with ExitStack() as st:
                        E = SB(st, "E", [65, NT, 128], BF16)
                        dma("pool", E[:], t_E, wr=["E"])
                        ovl = SB(st, "ovl", [128, 2, 64], BF16)
                        dma("pool", ovl[:], t_ovl, wr=["ovl"])
                        wbt = SB(st, "wbt", [128, 10, 512], BF16)
                        dma("pool", wbt[:], t_wb[g], wr=["wbt"])
                        sbt = SB(st, "sbt", [128, 2, 512], BF16)
                        for dl in range(2):
                            dma("pool", sbt[:, dl, :], t_sb[g, dl], wr=["sbt"])
                        cst = SB(st, "cst", [1, 512], BF16)
                        dma("pool", cst[:], t_cst[g], wr=["cst"])
                        addt = SB(st, "addt", [128, NTO, 64], F32)
                        dma("sp", addt[:], t_add, wr=["addt"])
                        cbt = [SB(st, "cbt%d" % i, [128, 2, 512], BF16) for i in range(2)]
                        Pc = [SB(st, "Pc%d" % i, [128, 2, 512], BF16) for i in range(2)]
                        Pb = [SB(st, "Pb%d" % i, [128, 512], BF16) for i in range(4)]
                        snT = [SB(st, "snT%d" % i, [65, 4, 128], BF16) for i in range(2)]
                        for i_ in range(2):
                            dma("pool", snT[i_][64:65, :, :].rearrange("p h t -> p (h t)"), t_cst[g], wr=["snT%d" % i_])
                        sc = [SB(st, "sc%d" % i, [128, 64], F32) for i in range(2)]
                        sc2 = [SB(st, "sc2%d" % i, [128, 64], F32) for i in range(2)]
                        m8 = [SB(st, "m8%d" % i, [128, 8], F32) for i in range(2)]
                        nm = [SB(st, "nm%d" % i, [128, 64], BF16) for i in range(2)]
                        rc = [SB(st, "rc%d" % i, [128, 3, 4], F32) for i in range(2)]
                        oacc = [SB(st, "oacc%d" % i, [128, 4, 64], F32) for i in range(2)]
                        yab = [SB(st, "yab%d" % i, [128, 256], BF16) for i in range(2)]
                        osb = [SB(st, "osb%d" % i, [128, 260], F32) for i in range(2)]
                        pS = [PS(st, "pS%d" % i, [128, 512], F32) for i in range(4)]
                        pCR = PS(st, "pCR", [128, 512], F32)
                        pOf = [None] + [PS(st, "pO%d" % i, [128, 512], F32) for i in (1, 2)]
                        pO = [pCR[:, 0:256].rearrange("p (h d) -> p h d", h=4)] + \
                             [t_[:, 0:260].rearrange("p (h d) -> p h d", h=4) for t_ in pOf[1:]]
                        pR = pCR[:, 256:512].rearrange("p (h d) -> p h d", h=4)
                        pM = PS(st, "pM", [128, 1024], BF16)
                        nS = [0]

                        def nextS():
                            i = nS[0] % 4
                            nS[0] += 1
                            return pS[i], "pS%d" % i, Pb[i], "Pb%d" % i

                        def attn_tile(i):
                            c = 16 + i
                            b2 = i % 2
                            qslice = qaT[:, :, i * 128:(i + 1) * 128]
                            r_ = rc[b2]
                            rk = "rc%d" % b2
                            sn = snT[b2]
                            snk = "snT%d" % b2
                            v3 = lambda p: p[:].rearrange("p (h t) -> p h t", h=4)
                            gv = gat[:, i, :].rearrange("p (h b) -> p b h", b=3)
                            oa = oacc[b2]
                            ok = "oacc%d" % b2

                            def cmp_S(it):
                                def f():
                                    p, pk, _, _ = nextS()
                                    mm(v3(p), kcT[:, it * 128:(it + 1) * 128], qslice, True, False, wr=[pk])
                                    mm(p[:], identb[:], cbt[b2][:, it, :], False, True, rd=["cbt%d" % b2], wr=[pk])
                                    act(Pc[b2][:, it, :], p[:], AF.Exp, rd=[pk], wr=["Pc%d_%d" % (b2, it)])
                                return f

                            def cmp_PV():
                                for h in range(4):
                                    for it in range(2):
                                        mm(pO[0][:, h, :], Pc[b2][:, it, h * 128:(h + 1) * 128], vca[:, it, 0:64], it == 0, it == 1,
                                           rd=["Pc%d_%d" % (b2, it)], wr=["pO0"])
                                for h in range(4):
                                    for it in range(2):
                                        mm(pR[:, h, :], Pc[b2][:, it, h * 128:(h + 1) * 128], ovl[:, it, :], it == 0, it == 1,
                                           rd=["Pc%d_%d" % (b2, it), "ovl"], wr=["pO0"])
                                S.op("dve", lambda e: e.tensor_reduce(out=r_[:, 0, :], in_=pR, axis=AX.X, op=ALU.add), ["pO0"], [rk])
                                ts("dve", r_[:, 0, :], r_[:, 0, :], 1e-30, None, ALU.max, rd=[rk], wr=[rk])
                                rcp(r_[:, 0, :], r_[:, 0, :], rd=[rk], wr=[rk])
                                s_ = sc[b2]
                                sk = "sc%d" % b2
                                cp("dve", s_[:], addt[:, i, :], rd=["addt"], wr=[sk])
                                for h in range(4):
                                    stt("dve", s_[:], pR[:, h, :], r_[:, 0, h:h + 1], s_[:], ALU.mult, ALU.add, rd=["pO0", rk, sk], wr=[sk])
                                s2_ = sc2[b2]
                                s2k = "sc2%d" % b2
                                m_ = m8[b2]
                                mk = "m8%d" % b2
                                S.op("dve", lambda e: e.max(out=m_[:], in_=s_[:]), [sk], [mk])
                                S.op("dve", lambda e: e.match_replace(out=s2_[:], in_to_replace=m_[:], in_values=s_[:], imm_value=-1e30),
                                     [sk, mk], [s2k])
                                S.op("dve", lambda e: e.max(out=m_[:], in_=s2_[:]), [s2k], [mk])
                                ts("dve", s2_[:], s_[:], m_[:, 7:8], None, ALU.is_ge, rd=[sk, mk, s2k], wr=[s2k])
                                ts("dve", s_[:], s_[:], -1e5, None, ALU.is_ge, rd=[sk, s2k], wr=[sk])
                                tt("dve", s2_[:], s2_[:], s_[:], ALU.mult, rd=[sk, s2k], wr=[s2k])
                                n_ = nm[b2]
                                nk = "nm%d" % b2
                                ts("dve", n_[:], s2_[:], -1.0, -NEG, ALU.add, ALU.mult, rd=[s2k], wr=[nk])
                                tr(pM[0:64, 0:128], n_[:], identb[:], rd=[nk], wr=["pM"])
                                cp("dve", sn[0:64, :, :], pM[0:64, 0:128].unsqueeze(1).to_broadcast([64, 4, 128]), rd=["pM"], wr=[snk])
                                tt("dve", r_[:, 0, :], r_[:, 0, :], gv[:, 0, :], ALU.mult, rd=[rk], wr=[rk])
                                for h in range(4):
                                    ts("dve", oa[:, h, :], pO[0][:, h, :], r_[:, 0, h:h + 1], None, ALU.mult, rd=["pO0", rk], wr=[ok])

                            cmp_stages = [(cmp_S(0), None), (cmp_S(1), cmp_PV)]
                            win_stages = []
                            sel_stages = []

                            def pair(kT_, j, bias_fn, V_, vkey, acc, acck, first, last, use_sel):
                                st_ = {}

                                def fS():
                                    p, pk, pb, pbk = nextS()
                                    st_["x"] = (pb, pbk)
                                    mm(v3(p), kT_[:, j * 128:(j + 1) * 128], qslice, True, False, wr=[pk])
                                    if use_sel == 2:
                                        mm(p[:], E[:, j, :], sn[:].rearrange("p h t -> p (h t)"), False, True, rd=["E", snk], wr=[pk])
                                    else:
                                        if use_sel == 1:
                                            mm(p[:], E[0:64, j, :], sn[0:64, :, :].rearrange("p h t -> p (h t)"), False, False,
                                               rd=["E", snk], wr=[pk])
                                        bias_fn(p, pk)
                                    act(pb[:], p[:], AF.Exp, rd=[pk], wr=[pbk])

                                def fPV():
                                    pb, pbk = st_["x"]
                                    for h in range(4):
                                        mm(acc[:, h, :], pb[:, h * 128:(h + 1) * 128], V_[:, j, :], (first and h == 0), last,
                                           rd=[pbk, vkey], wr=[acck], skip=True)
                                return fS, fPV

                            for dl in range(4, -1, -1):
                                j = c - dl
                                var = dl if j >= 16 else 5 + dl
                                bf = (lambda var: lambda p, pk: mm(p[:], identb[:], wbt[:, var, :], False, True, rd=["wbt"], wr=[pk]))(var)
                                win_stages.append(pair(kwT, j, bf, vwa, "vwa1", pO[2], "pO2", dl == 4, dl == 0, 0))
                            for j in range(c + 1):
                                dl = c - j
                                if dl <= 1:
                                    bf = (lambda dl: lambda p, pk: mm(p[:], identb[:], sbt[:, dl, :], False, True, rd=["sbt"], wr=[pk]))(dl)
                                else:
                                    bf = lambda p, pk: mm(p[:], ones_b[0:1, :], cst[:], False, True, rd=["cst"], wr=[pk])
                                sel_stages.append(pair(ksT, j, bf, vsa, "vsa1", pO[1], "pO1", j == 0, j == c, 1 if dl <= 1 else 2))
                            def merge():
                                for br in (1, 2):
                                    cp("dve", osb[br - 1][:], pOf[br][:, 0:260], rd=["pO%d" % br], wr=["osb%d" % (br - 1)])
                                for br in (1, 2):
                                    ov = osb[br - 1][:, 0:260].rearrange("p (h d) -> p h d", h=4)
                                    ts("dve", r_[:, br, :], ov[:, :, 64], 1e-30, None, ALU.max, rd=["osb%d" % (br - 1), rk], wr=[rk])
                                    rcp(r_[:, br, :], r_[:, br, :], rd=[rk], wr=[rk])
                                    tt("dve", r_[:, br, :], r_[:, br, :], gv[:, br, :], ALU.mult, rd=[rk], wr=[rk])
                                ov1 = osb[0][:, 0:260].rearrange("p (h d) -> p h d", h=4)
                                ov2 = osb[1][:, 0:260].rearrange("p (h d) -> p h d", h=4)
                                for h in range(4):
                                    stt("dve", oa[:, h, :], ov1[:, h, 0:64], r_[:, 1, h:h + 1], oa[:, h, :], ALU.mult, ALU.add,
                                        rd=["osb0", rk, ok], wr=[ok])
                                    stt("dve", yab[b2][:, h * 64:(h + 1) * 64], ov2[:, h, 0:64], r_[:, 2, h:h + 1], oa[:, h, :],
                                        ALU.mult, ALU.add, rd=["osb1", rk, ok], wr=["yab%d" % b2])

                            def mergeB():
                                for hp in range(2):
                                    tr(pM[:, 128 + hp * 128:256 + hp * 128], yab[b2][:, hp * 128:(hp + 1) * 128], identb[:],
                                       rd=["yab%d" % b2], wr=["pM"])
                                    cp("dve", yT[:, 4 + 2 * g + hp, i * 128:(i + 1) * 128], pM[:, 128 + hp * 128:256 + hp * 128],
                                       rd=["pM"], wr=[])

                            return cmp_stages, win_stages, sel_stages, merge, mergeB

                        tiles = []
                        allst = []
                        dma("pool", cbt[0][:], t_cb[g, 0], wr=["cbt0"])
                        tiles.append(attn_tile(0))
                        allst += tiles[0][0]
                        for i in range(NTO):
                            cs, ws, ss_, mg, mgB = tiles[i]
                            allst += ws
                            if i > 0:
                                allst.append((None, tiles[i - 1][4]))
                            if i + 1 < NTO:
                                allst.append(((lambda i=i: lambda: dma("pool", cbt[(i + 1) % 2][:], t_cb[g, i + 1],
                                                                       wr=["cbt%d" % ((i + 1) % 2)]))(), None))
                                tiles.append(attn_tile(i + 1))
                                allst += tiles[i + 1][0]
                            allst += ss_
                            allst.append((None, mg))
                        allst.append((None, tiles[NTO - 1][4]))
                        pend = []
                        for (fS, fPV) in allst:
                            if fS is not None:
                                fS()
                            if len(pend) >= 2:
                                f_ = pend.pop(0)
                                if f_ is not None:
                                    f_()
                            pend.append(fPV)
                        for f_ in pend:
                            if f_ is not None:
                                f_()
                        S.flush()
            if stage == 3:
                with ExitStack() as st:
                    tmp = SB(st, "dbgt", [128, 8, TO], F32)
                    for kc in range(8):
                        cp("dve", tmp[:, kc, :], yT[:, kc, :], wr=["t%d" % kc])
                        dma("sp", dbg[:, kc * TO:(kc + 1) * TO], tmp[:, kc, :], rd=["t%d" % kc])
                    S.flush()

            if stage >= 4:
              with ExitStack() as st:
                wo = SB(st, "wo", [128, 8, D], BF16)
                w_out_v = w_out.rearrange("(k p) f -> p k f", p=128)
                for kc in range(8):
                    dma("pool", wo[:, kc, :], w_out_v[:, kc, :], wr=["wo"])
                xo = [SB(st, "xo%d" % i, [128, D], F32) for i in range(2)]
                x1 = [SB(st, "x1%d" % i, [128, D], F32) for i in range(2)]
                ssb = [SB(st, "ssb%d" % i, [128, 4], F32) for i in range(2)]
                s5 = [SB(st, "s5%d" % i, [128, 4], F32) for i in range(2)]
                pT = [PS(st, "pT%d" % i, [128, D], BF16) for i in range(2)]
                pY = [PS(st, "pYe%d" % i, [128, D], F32) for i in range(2)]
                for i in range(NTO):
                    b2 = i % 2
                    dma("sp", xo[b2][:], xw_t[16 + i], wr=["xo%d" % b2])
                    py = pY[b2]
                    pyk = "pYe%d" % b2
                    for hb in range(2):
                        for kc in range(8):
                            mm(py[:, hb * 512:(hb + 1) * 512], yT[:, kc, i * 128:(i + 1) * 128], wo[:, kc, hb * 512:(hb + 1) * 512],
                               kc == 0, kc == 7, rd=["wo"], wr=[pyk])
                    s_ = s5[b2]
                    sk = "s5%d" % b2
                    S.op("act", (lambda py=py: lambda e: e.activation(out=junkf[:, 0:512], in_=py[:, 0:512], func=AF.Square))(), [pyk], ["junkf"])
                    S.op("act", (lambda py=py: lambda e: e.activation(out=junkf[:, 512:1024], in_=py[:, 512:1024], func=AF.Square))(), [pyk], ["junkf"])
                    S.op("dve", (lambda s_=s_: lambda e: e.tensor_reduce(out=s_[:, 2:3], in_=junkf[:], axis=AX.X, op=ALU.add))(), ["junkf", sk], [sk])
                    act(s_[:, 2:3], s_[:, 2:3], AF.Sqrt, rd=[sk], wr=[sk], scale=1.0 / D, bias=epsc[:, 0:1])
                    rcp(s_[:, 3:4], s_[:, 2:3], rd=[sk], wr=[sk])
                    xk = "x1%d" % b2
                    for hb in range(2):
                        stt("dve", x1[b2][:, hb * 512:(hb + 1) * 512], py[:, hb * 512:(hb + 1) * 512], s_[:, 3:4],
                            GT1[:, hb * 512:(hb + 1) * 512], ALU.mult, ALU.mult, rd=[pyk, sk], wr=[xk])
                    tt("pool", x1[b2][:], x1[b2][:], xo[b2][:], ALU.add, rd=[xk, "xo%d" % b2], wr=[xk])
                    import os
                    _de = int(os.environ.get("DBG_E", 0))
                    if _de != 1:
                        dma("sp", out_t[i], x1[b2][:], rd=[xk])
                    if _de != 2:
                        norm_transpose(x1[b2][:], xk, h2T, i * 128, A2, B2, xn[b2], pT[b2], ssb[b2], i, "e")
                S.flush()
        if stage == 4:
            with ExitStack() as st:
                tmp = SB(st, "dbgt", [128, 8, TO], F32)
                for kc in range(8):
                    cp("dve", tmp[:, kc, :], h2T[:, kc, 0:TO], wr=["t%d" % kc])
                    dma("sp", dbg[:, kc * TO:(kc + 1) * TO], tmp[:, kc, :], rd=["t%d" % kc])
                S.flush()

        if stage >= 5:
          with ExitStack() as st:
            yacc = SB(st, "yacc", [128, NTO, D], F32)
            gate = SB(st, "gate", [128, NTO, NE], F32)
            gateT = SB(st, "gateT", [NE, TO], BF16)
            bup = SB(st, "bup", [128, NE, 16], F32)
            dma("sp", bup[:], b_up_col, wr=["bup"])
            with ExitStack() as rs:
                wr_ = SB(rs, "wr_", [128, 8, NE], BF16)
                dma("pool", wr_[:], w_router.rearrange("(k p) e -> p k e", p=128), wr=["wr_"])
                brt = SB(rs, "brt", [128, NE], F32)
                dma("sp", brt[:], b_router.partition_broadcast(128), wr=["brt"])
                bdn = SB(rs, "bdn", [NE, D], BF16)
                dma("pool", bdn[:], b_down, wr=["bdn"])
                lg = [SB(rs, "lg%d" % i, [128, NE], F32) for i in range(2)]
                mk4 = [SB(rs, "mk4%d" % i, [128, NE], F32) for i in range(2)]
                m8 = [SB(rs, "m8r%d" % i, [128, 8], F32) for i in range(2)]
                s4 = [SB(rs, "s4r%d" % i, [128, 4], F32) for i in range(2)]
                pL = PS(rs, "pL", [128, NTO, NE], F32)
                pGf = [PS(rs, "pG%d" % i, [128, 512], F32) for i in range(2)]
                pG = [t_[0:NE, 0:128] for t_ in pGf]
                pB = [PS(rs, "pB%d" % i, [128, 512], F32) for i in range(2)]
                for i in range(NTO):
                    b2 = i % 2
                    for kc in range(8):
                        mm(pL[:, i, :], h2T[:, kc, i * 128:(i + 1) * 128], wr_[:, kc, :], kc == 0, kc == 7, rd=["wr_"], wr=["pL"])
                    l_ = lg[b2]
                    lk = "lg%d" % b2
                    tt("dve", l_[:], pL[:, i, :], brt[:], ALU.add, rd=["pL", "brt"], wr=[lk])
                    m_ = m8[b2]
                    mk = "m8r%d" % b2
                    S.op("dve", (lambda m_=m_, l_=l_: lambda e: e.max(out=m_[:], in_=l_[:]))(), [lk], [mk])
                    k4 = mk4[b2]
                    k4k = "mk4%d" % b2
                    ts("dve", k4[:], l_[:], m_[:, 3:4], None, ALU.is_ge, rd=[lk, mk], wr=[k4k])
                    s_ = s4[b2]
                    sk = "s4r%d" % b2
                    ts("dve", s_[:, 0:1], m_[:, 0:1], -1.0, None, ALU.mult, rd=[mk], wr=[sk])
                    act(l_[:], l_[:], AF.Exp, rd=[lk, sk, k4k], wr=[lk], bias=s_[:, 0:1])
                    tt("dve", l_[:], l_[:], k4[:], ALU.mult, rd=[lk, k4k], wr=[lk])
                    S.op("dve", (lambda s_=s_, l_=l_: lambda e: e.tensor_reduce(out=s_[:, 1:2], in_=l_[:], axis=AX.X, op=ALU.add))(),
                         [lk, sk], [sk])
                    rcp(s_[:, 2:3], s_[:, 1:2], rd=[sk], wr=[sk])
                    ts("dve", gate[:, i, :], l_[:], s_[:, 2:3], None, ALU.mult, rd=[lk, sk], wr=["gate%d" % i])
                    tr(pG[b2], gate[:, i, :], identf[:], rd=["gate%d" % i], wr=["pG%d" % b2])
                    cp("dve", gateT[:, i * 128:(i + 1) * 128], pG[b2], rd=["pG%d" % b2], wr=["gateT%d" % i])
                    for db in range(2):
                        mm(pB[db][:], gateT[:, i * 128:(i + 1) * 128], bdn[:, db * 512:(db + 1) * 512], True, True,
                           rd=["gateT%d" % i, "bdn"], wr=["pB%d" % db])
                        act(yacc[:, i, db * 512:(db + 1) * 512], pB[db][:], AF.Copy, rd=["pB%d" % db], wr=[])
                S.flush()

            with ExitStack() as es:
                wup = [hT[:, :, TO + i * 1024:TO + (i + 1) * 1024] for i in range(2)]
                wdn = [SB(es, "wdn%d" % i, [128, 4, D], BF16) for i in range(2)]
                gg = [SB(es, "gg%d" % i, [128, 512], F32) for i in range(2)]
                sg = [SB(es, "sg%d" % i, [128, 512], F32) for i in range(2)]
                ll = [SB(es, "ll%d" % i, [128, 512], F32) for i in range(2)]
                actT = [SB(es, "actT%d" % i, [128, 4, 512], BF16) for i in range(2)]
                pU = [PS(es, "pU%d" % i, [128, 512], F32) for i in range(4)]
                pD = [PS(es, "pD%d" % i, [128, 512], F32) for i in range(4)]
                nU = 0
                nD = 0
                nA = 0
                w_up_v = w_up.rearrange("e (k p) f -> e p k f", p=128)
                w_dn_v = w_down.rearrange("e (c p) d -> e p c d", p=128)
                units = [(e_, u) for e_ in range(NE) for u in range(2)]

                def load_unit(n):
                    e_, u = units[n]
                    wb = n % 2
                    for kc in range(8):
                        dma("pool", wup[wb][:, kc, 0:512], w_up_v[e_, :, kc, 512 * u:512 * u + 512], wr=["wup%d" % wb])
                        dma("pool", wup[wb][:, kc, 512:1024], w_up_v[e_, :, kc, 1024 + 512 * u:1024 + 512 * u + 512], wr=["wup%d" % wb])
                    for fc in range(4):
                        dma("pool", wdn[wb][:, fc, :], w_dn_v[e_, :, 4 * u + fc, :], wr=["wdn%d" % wb])

                load_unit(0)
                for n, (e_, u) in enumerate(units):
                    if n + 1 < len(units):
                        load_unit(n + 1)
                    wb = n % 2
                    wuk = "wup%d" % wb
                    wdk = "wdn%d" % wb
                    for tb in range(4):
                        ab = nA % 2
                        nA += 1
                        ak = "actT%d" % ab
                        for fc in range(4):
                            pg = pU[nU % 4]
                            pgk = "pU%d" % (nU % 4)
                            nU += 1
                            pl = pU[nU % 4]
                            plk = "pU%d" % (nU % 4)
                            nU += 1
                            for kc in range(8):
                                mm(pg[:], wup[wb][:, kc, fc * 128:(fc + 1) * 128], h2T[:, kc, tb * 512:(tb + 1) * 512],
                                   kc == 0, kc == 7, rd=[wuk], wr=[pgk])
                            for kc in range(8):
                                mm(pl[:], wup[wb][:, kc, 512 + fc * 128:512 + (fc + 1) * 128], h2T[:, kc, tb * 512:(tb + 1) * 512],
                                   kc == 0, kc == 7, rd=[wuk], wr=[plk])
                            b2 = fc % 2
                            jg = 4 * u + fc
                            ts("dve", gg[b2][:], pg[:], bup[:, e_, jg:jg + 1], 7.0, ALU.add, ALU.min, rd=[pgk], wr=["gg%d" % b2])
                            act(sg[b2][:], gg[b2][:], AF.Sigmoid, rd=["gg%d" % b2], wr=["sg%d" % b2], scale=1.702)
                            ts("dve", ll[b2][:], pl[:], bup[:, e_, 8 + jg:8 + jg + 1], 7.0, ALU.add, ALU.min, rd=[plk], wr=["ll%d" % b2])
                            ts("dve", ll[b2][:], ll[b2][:], -7.0, 1.0, ALU.max, ALU.add, rd=["ll%d" % b2], wr=["ll%d" % b2])
                            tt("dve", gg[b2][:], gg[b2][:], sg[b2][:], ALU.mult, rd=["gg%d" % b2, "sg%d" % b2], wr=["gg%d" % b2])
                            tt("dve", actT[ab][:, fc, :], gg[b2][:], ll[b2][:], ALU.mult, rd=["gg%d" % b2, "ll%d" % b2],
                               wr=[ak + "_%d" % fc])
                        for tt_ in range(4):
                            ti = tb * 4 + tt_
                            for db in range(2):
                                pd = pD[nD % 4]
                                pdk = "pD%d" % (nD % 4)
                                nD += 1
                                for fc in range(4):
                                    mm(pd[:], actT[ab][:, fc, tt_ * 128:(tt_ + 1) * 128], wdn[wb][:, fc, db * 512:(db + 1) * 512],
                                       fc == 0, fc == 3, rd=[ak + "_%d" % fc, wdk], wr=[pdk])
                                ya = yacc[:, ti, db * 512:(db + 1) * 512]
                                stt("dve", ya, pd[:], gate[:, ti, e_:e_ + 1], ya, ALU.mult, ALU.add, rd=[pdk, "ya%d_%d" % (ti, db)],
                                    wr=["ya%d_%d" % (ti, db)])
                S.flush()

            with ExitStack() as fs:
                xr = [SB(fs, "xr%d" % i, [128, D], F32) for i in range(2)]
                ob = [SB(fs, "ob%d" % i, [128, D], F32) for i in range(2)]
                s5 = [SB(fs, "s6%d" % i, [128, 4], F32) for i in range(2)]
                for i in range(NTO):
                    b2 = i % 2
                    dma("sp", xr[b2][:], out_t[i], wr=["xr%d" % b2])
                    s_ = s5[b2]
                    sk = "s6%d" % b2
                    sumsq(yacc[:, i, :], [], s_[:, 0:1], sk)
                    act(s_[:, 1:2], s_[:, 0:1], AF.Sqrt, rd=[sk], wr=[sk], scale=1.0 / D, bias=epsc[:, 0:1])
                    rcp(s_[:, 2:3], s_[:, 1:2], rd=[sk], wr=[sk])
                    stt("dve", ob[b2][:], yacc[:, i, :], s_[:, 2:3], GT2[:], ALU.mult, ALU.mult, rd=[sk], wr=["ob%d" % b2])
                    tt("pool", ob[b2][:], ob[b2][:], xr[b2][:], ALU.add, rd=["ob%d" % b2, "xr%d" % b2], wr=["ob%d" % b2])
                    dma("sp", out_t[i], ob[b2][:], rd=["ob%d" % b2])
                S.flush()
    return nc


_TABLE_CACHE = {}


def _tables(rel_bias, s):
    rb = np.asarray(rel_bias, np.float32)
    lo = 0 if s == 1 else 2048
    jl = np.arange(128)[:, None]
    tl = np.arange(128)[None, :]
    t_sb = np.empty((2, 2, 128, 4, 128), np.float32)
    t_wb = np.empty((2, 128, 10, 4, 128), np.float32)
    t_cst = np.empty((2, 1, 4, 128), np.float32)
    for g in range(2):
        for h in range(4):
            col = rb[:, 4 * g + h]
            t_cst[g, 0, h, :] = col[31]
            for dl in range(5):
                dist = dl * 128 + tl - jl
                bias = col[_t5_bucket(dist)]
                if dl < 2:
                    t_sb[g, dl, :, h, :] = np.where(dist >= 0, bias, np.float32(NEG))
                wv = np.where((dist >= 0) & (dist < 512), bias, np.float32(NEG))
                t_wb[g, :, dl, h, :] = wv
                t_wb[g, :, 5 + dl, h, :] = wv if s == 1 else np.float32(NEG)
    t_cb = np.empty((2, 16, 128, 2, 4, 128), np.float32)
    m = (np.arange(2)[None, :] * 128 + np.arange(128)[:, None])
    end = 16 * m + 31
    for i in range(16):
        tw = 2048 + 128 * i + np.arange(128)
        dist = tw[None, None, :] - end[:, :, None]
        valid = (16 * m[:, :, None] >= lo) & (dist >= 0) & (m[:, :, None] <= 254)
        bk = _t5_bucket(dist)
        for g in range(2):
            for h in range(4):
                t_cb[g, i, :, :, h, :] = np.where(valid, rb[:, 4 * g + h][bk], np.float32(NEG))
    t_add = np.empty((128, 16, 64), np.float32)
    blk = np.arange(64)[None, :]
    for i in range(16):
        tw = (2048 + 128 * i + np.arange(128))[:, None]
        cur = tw // 64
        first = lo // 64
        valid = (64 * blk >= lo) & (64 * blk <= tw)
        forced = (blk == first) | (blk == cur) | (blk == cur - 1)
        t_add[:, i, :] = np.where(valid, np.where(forced, np.float32(1000.0), np.float32(0.0)), np.float32(-1e6))
    return dict(t_sb=t_sb.reshape(2, 2, 128, 512), t_cst=t_cst.reshape(2, 1, 512), t_wb=t_wb.reshape(2, 128, 10, 512),
                t_cb=t_cb.reshape(2, 16, 128, 2, 512), t_add=t_add)


def _consts():
    E = np.zeros((65, 32, 128), np.float32)
    for j in range(32):
        for k in range(128):
            E[2 * j + k // 64, j, k] = 1.0
    E[64] = 1.0
    ovl = np.zeros((256, 64), np.float32)
    for m in range(255):
        for p in range(16 * m, 16 * m + 32):
            ovl[m, p // 64] += 1.0 / 32
    ovl = ovl.reshape(2, 128, 64).transpose(1, 0, 2).copy()
    tri = (np.arange(128)[:, None] <= np.arange(128)[None, :]).astype(np.float32)
    return dict(t_E=E, t_ovl=ovl, t_tri=tri)


_NC_CACHE = {}


def make_in_maps(inputs):
    f = lambda a: np.ascontiguousarray(np.asarray(a, np.float32))
    x = f(inputs["x"])
    c = f(inputs["c"])
    col = lambda v, n: f(np.asarray(v, np.float32).reshape(n, 128).T)
    perm = _perm()
    shared = dict(
        w_ada=f(inputs["w_ada"][0]),
        b_ada_col=col(inputs["b_ada"][0], 48),
        b_ada_row=f(inputs["b_ada"][0][None, :]),
        gpm_col=col(inputs["g_pre_mix"][0], 8),
        gpf_col=col(inputs["g_pre_ffn"][0], 8),
        gpostm=f(inputs["g_post_mix"][0][None, :]),
        gpostf=f(inputs["g_post_ffn"][0][None, :]),
        w_in=f(np.asarray(inputs["w_in"][0])[:, perm]),
        b_gates=f(inputs["b_gates"][0][None, :]),
        conv_col=f(np.asarray(inputs["conv_qk"][0]).reshape(4, 8, 128).transpose(2, 1, 0)),
        wck=f(np.asarray(inputs["w_cmp_k"][0]).transpose(1, 0, 2)),
        wcv=f(np.asarray(inputs["w_cmp_v"][0]).transpose(1, 0, 2)),
        posk=f(np.asarray(inputs["pos_cmp_k"][0]).T),
        posv=f(np.asarray(inputs["pos_cmp_v"][0]).T),
        w_out=f(inputs["w_out"][0]),
        w_router=f(inputs["w_router"][0]),
        b_router=f(inputs["b_router"][0][None, :]),
        w_up=f(inputs["w_up"][0]),
        b_up_col=f(np.asarray(inputs["b_up"][0]).reshape(NE, 16, 128).transpose(2, 0, 1)),
        w_down=f(inputs["w_down"][0]),
        b_down=f(inputs["b_down"][0]),
    )
    shared.update(_consts())
    tabs = [_tables(inputs["rel_bias"], s) for s in range(2)]
    in_maps = []
    for core in range(8):
        b, s = core // 2, core % 2
        m = dict(shared)
        m.update(tabs[s])
        if s == 1:
            m["xw"] = x[b]
        else:
            m["xw"] = np.ascontiguousarray(np.concatenate([x[b, TO:], x[b, :TO]], axis=0))
        m["c_col"] = col(c[b], 8)
        m["flag"] = np.full((128, 1), float(s), np.float32)
        in_maps.append(m)
    return in_maps


def kernel(**inputs):
    if "nc" not in _NC_CACHE:
        _NC_CACHE["nc"] = build_nc()
    nc = _NC_CACHE["nc"]
    in_maps = make_in_maps(inputs)
    res = run_bass_kernel_spmd(nc, in_maps, core_ids=list(range(8)))
    out = np.empty((4, 4096, D), np.float32)
    for core in range(8):
        b, s = core // 2, core % 2
        out[b, s * TO:(s + 1) * TO] = res.results[core]["out"]
    return out
```

```python
import numpy as np
from contextlib import ExitStack
import concourse.bass as bass
import concourse.mybir as mybir
from concourse.bass_utils import run_bass_kernel_spmd

F32 = mybir.dt.float32
BF16 = mybir.dt.bfloat16
AF = mybir.ActivationFunctionType
ALU = mybir.AluOpType
AX = mybir.AxisListType

COMPUTE = ("pe", "act", "dve", "pool")
NDMASEM = 24
NEG = -30000.0


class _Op:
    __slots__ = ("id", "eng", "fn", "deps", "dma", "needs_inc", "inc_idx", "dsem", "dval", "dprev")


class Sched:
    def __init__(self, nc, stack, same_engine_sync=True):
        self.nc = nc
        self.same = same_engine_sync
        self.sem = {e: stack.enter_context(nc.semaphore("sem_" + e)) for e in COMPUTE}
        self.dsem = [stack.enter_context(nc.semaphore("dsem%d" % i)) for i in range(NDMASEM)]
        self.bar = stack.enter_context(nc.semaphore("sem_bar"))
        self.cnt = {e: 0 for e in COMPUTE}
        self.ndma = 0
        self.dfinal = {}
        self.phase = 0
        self.scr = stack.enter_context(nc.sbuf_tensor("sched_scr", [128, 8], F32))
        self._reset()

    def _reset(self):
        self.ops = []
        self.lastw = {}
        self.readers = {}
        self.dma_hist = {}

    def _add(self, eng, fn, reads, writes, dma):
        deps = {}

        def add_dep(p):
            o = self.ops[p]
            if o.dma:
                deps[("d", p)] = p
            else:
                k = o.eng
                if k not in deps or deps[k] < p:
                    deps[k] = p

        for k in reads:
            for p in self.lastw.get(k, ()):
                add_dep(p)
        for k in writes:
            lw = self.lastw.get(k, ())
            if not (dma and lw and all(self.ops[p].dma for p in lw) and not self.readers.get(k)):
                for p in lw:
                    add_dep(p)
            for r in self.readers.get(k, ()):
                add_dep(r)
        op = _Op()
        op.id = len(self.ops)
        op.eng = eng
        op.fn = fn
        op.deps = sorted(set(deps.values()))
        op.dma = dma
        op.needs_inc = False
        op.inc_idx = 0
        op.dsem = None
        op.dval = 0
        op.dprev = None
        if dma:
            op.dsem = self.ndma % NDMASEM
            op.dval = 16 * (self.ndma // NDMASEM + 1)
            op.dprev = self.dma_hist.get(op.dsem)
            self.dma_hist[op.dsem] = op.id
            self.dfinal[op.dsem] = op.dval
            self.ndma += 1
        self.ops.append(op)
        for k in writes:
            lw = self.lastw.get(k, ())
            if dma and lw and all(self.ops[p].dma for p in lw) and not self.readers.get(k):
                self.lastw[k] = list(lw) + [op.id]
            else:
                self.lastw[k] = [op.id]
            self.readers[k] = []
        for k in reads:
            self.readers.setdefault(k, []).append(op.id)
        return op.id

    def op(self, eng, fn, reads=(), writes=()):
        return self._add(eng, fn, tuple(reads), tuple(writes), False)

    def dma(self, eng, fn, reads=(), writes=()):
        return self._add(eng, fn, tuple(reads), tuple(writes), True)

    def flush(self):
        nc = self.nc
        ops = self.ops
        for o in ops:
            for p in o.deps:
                po = ops[p]
                if po.dma:
                    continue
                if po.eng == o.eng and (po.eng == "pe" or not self.same):
                    continue
                po.needs_inc = True
        for o in ops:
            if not o.dma and o.needs_inc:
                self.cnt[o.eng] += 1
                o.inc_idx = self.cnt[o.eng]
        per = {e: [] for e in ("pe", "act", "dve", "pool", "sp")}
        for o in ops:
            per[o.eng].append(o)
        self.phase += 1
        phase = self.phase
        dfinal = dict(self.dfinal)
        sem, dsem, bar, scr = self.sem, self.dsem, self.bar, self.scr
        same = self.same

        def run(ename, e):
            waited_e = {f: 0 for f in COMPUTE}
            waited_d = {}
            for o in per[ename]:
                for p in o.deps:
                    po = ops[p]
                    if po.dma:
                        if waited_d.get(po.dsem, 0) < po.dval:
                            e.wait_ge(dsem[po.dsem], po.dval)
                            waited_d[po.dsem] = po.dval
                    else:
                        if not po.needs_inc:
                            continue
                        if po.eng == ename and (ename == "pe" or not same):
                            continue
                        if waited_e[po.eng] < po.inc_idx:
                            e.wait_ge(sem[po.eng], po.inc_idx)
                            waited_e[po.eng] = po.inc_idx
                if o.dma and o.dprev is not None:
                    po = ops[o.dprev]
                    if waited_d.get(po.dsem, 0) < po.dval:
                        e.wait_ge(dsem[po.dsem], po.dval)
                        waited_d[po.dsem] = po.dval
                inst = o.fn(e)
                if o.dma:
                    inst.then_inc(dsem[o.dsem], 16)
                elif o.needs_inc:
                    inst.then_inc(sem[o.eng], 1)
            if ename == "act":
                e.memzero(scr[0:1, 0:1]).then_inc(bar, 1)
            elif ename == "dve":
                e.memset(scr[0:1, 1:2], 0.0).then_inc(bar, 1)
            elif ename == "pool":
                e.memset(scr[0:1, 2:3], 0.0).then_inc(bar, 1)
            e.wait_ge(bar, 3 * phase)
            for s, v in dfinal.items():
                e.wait_ge(dsem[s], v)

        with nc.Block() as block:
            @block.tensor
            def _(e):
                run("pe", e)

            @block.scalar
            def _(e):
                run("act", e)

            @block.vector
            def _(e):
                run("dve", e)

            @block.gpsimd
            def _(e):
                run("pool", e)

            @block.sync
            def _(e):
                run("sp", e)
        self._reset()


D = 1024
T = 4096
TO = 2048
NT = 32
NTO = 16
D_IN = 3360
NE = 32
def KM(h): return 512 + 128 * h
def QM(h): return 128 * h
def VM(h): return 1024 + 128 * h
def OM(h): return 1536 + 128 * h
GIF = 2048
def QA(h8): return 2056 + 64 * h8
def KC(g): return 2568 + 64 * g
def VC(g): return 2696 + 64 * g
def KS(g): return 2824 + 64 * g
def KW(g): return 2952 + 64 * g
def VSW(g): return 3080 + 128 * g
GA = 3336


def _perm():
    p = list(range(D_IN))
    new = list(range(2568, 2952))
    new += list(range(3080, 3208))
    for g in range(2):
        new += list(range(2952 + 64 * g, 2952 + 64 * g + 64))
        new += list(range(3208 + 64 * g, 3208 + 64 * g + 64))
    p[2568:3336] = new
    return np.array(p)


def _t5_bucket(dist):
    n = np.maximum(dist, 0)
    nf = np.maximum(n, 1).astype(np.float32)
    large = 16 + (np.log(nf / np.float32(16)) / np.float32(np.log(128 / 16)) * np.float32(16)).astype(np.int32)
    large = np.minimum(large, 31)
    return np.where(n < 16, n, large)


def build_nc(stage=99, same=True):
    nc = bass.Bass("TRN2", target_bir_lowering=False)
    din = lambda name, shape: nc.dram_tensor(name, list(shape), F32, kind="ExternalInput").ap()
    xw = din("xw", [T, D])
    c_col = din("c_col", [128, 8])
    w_ada = din("w_ada", [D, 6 * D])
    b_ada_col = din("b_ada_col", [128, 48])
    b_ada_row = din("b_ada_row", [1, 6 * D])
    gpm_col = din("gpm_col", [128, 8])
    gpf_col = din("gpf_col", [128, 8])
    gpostm = din("gpostm", [1, D])
    gpostf = din("gpostf", [1, D])
    w_in = din("w_in", [D, D_IN])
    b_gates = din("b_gates", [1, 8])
    conv_col = din("conv_col", [128, 8, 4])
    wck = din("wck", [64, 32, 64])
    wcv = din("wcv", [64, 32, 64])
    posk = din("posk", [64, 32])
    posv = din("posv", [64, 32])
    w_out = din("w_out", [D, D])
    w_router = din("w_router", [D, NE])
    b_router = din("b_router", [1, NE])
    w_up = din("w_up", [NE, D, 2 * D])
    b_up_col = din("b_up_col", [128, NE, 16])
    w_down = din("w_down", [NE, D, D])
    b_down = din("b_down", [NE, D])
    flag = din("flag", [128, 1])
    t_sb = din("t_sb", [2, 2, 128, 512])
    t_cst = din("t_cst", [2, 1, 512])
    t_wb = din("t_wb", [2, 128, 10, 512])
    t_cb = din("t_cb", [2, 16, 128, 2, 512])
    t_add = din("t_add", [128, 16, 64])
    t_E = din("t_E", [65, 32, 128])
    t_ovl = din("t_ovl", [128, 2, 64])
    t_tri = din("t_tri", [128, 128])
    out = nc.dram_tensor("out", [TO, D], F32, kind="ExternalOutput").ap()
    dbg = None
    if stage < 99:
        dbg = nc.dram_tensor("dbg", [128, 8 * TO], F32, kind="ExternalOutput").ap()

    xw_t = xw.rearrange("(n p) d -> n p d", p=128)
    out_t = out.rearrange("(n p) d -> n p d", p=128)

    with ExitStack() as gs:
        S = Sched(nc, gs, same_engine_sync=same)
        _uid = [0]

        def _nm(name):
            _uid[0] += 1
            return "%s_u%d" % (name, _uid[0])

        SB = lambda st, name, shape, dt: st.enter_context(nc.sbuf_tensor(_nm(name), list(shape), dt))
        PS = lambda st, name, shape, dt: st.enter_context(nc.psum_tensor(_nm(name), list(shape), dt))

        def mm(o, l, r, start, stop, rd=(), wr=(), skip=False):
            if skip:
                S.op("pe", lambda e: e.matmul(o, lhsT=l, rhs=r, start=start, stop=stop, skip_group_check=True), rd, wr)
            else:
                S.op("pe", lambda e: e.matmul(o, lhsT=l, rhs=r, start=start, stop=stop), rd, wr)

        def tr(o, i, idn, rd=(), wr=()):
            S.op("pe", lambda e: e.transpose(o, i, idn), rd, wr)

        def act(o, i, func, rd=(), wr=(), **kw):
            S.op("act", lambda e: e.activation(out=o, in_=i, func=func, **kw), rd, wr)

        def ts(eng, o, i, s1, s2, op0, op1=None, rd=(), wr=()):
            if op1 is None:
                S.op(eng, lambda e: e.tensor_scalar(out=o, in0=i, scalar1=s1, scalar2=None, op0=op0), rd, wr)
            else:
                S.op(eng, lambda e: e.tensor_scalar(out=o, in0=i, scalar1=s1, scalar2=s2, op0=op0, op1=op1), rd, wr)

        def tt(eng, o, a, b, op, rd=(), wr=()):
            S.op(eng, lambda e: e.tensor_tensor(out=o, in0=a, in1=b, op=op), rd, wr)

        def stt(eng, o, a, sc, b, op0, op1, rd=(), wr=()):
            S.op(eng, lambda e: e.scalar_tensor_tensor(out=o, in0=a, scalar=sc, in1=b, op0=op0, op1=op1), rd, wr)

        def cp(eng, o, i, rd=(), wr=()):
            S.op(eng, lambda e: e.tensor_copy(out=o, in_=i), rd, wr)

        def ms(eng, o, v, rd=(), wr=()):
            S.op(eng, lambda e: e.memset(o, v), rd, wr)

        def dma(q, o, i, rd=(), wr=()):
            S.dma(q, lambda e: e.dma_start(out=o, in_=i), rd, wr)

        def rcp(o, i, rd=(), wr=()):
            S.op("dve", lambda e: e.reciprocal(out=o, in_=i), rd, wr)

        identb = SB(gs, "identb", [128, 128], BF16)
        identf = SB(gs, "identf", [128, 128], F32)
        tri_f = SB(gs, "tri_f", [128, 128], F32)
        tri_b = SB(gs, "tri_b", [128, 128], BF16)
        ones_f = SB(gs, "ones_f", [128, 128], F32)
        ones_b = SB(gs, "ones_b", [128, 128], BF16)
        A1 = SB(gs, "A1", [128, 8], F32)
        B1 = SB(gs, "B1", [128, 8], F32)
        A2 = SB(gs, "A2", [128, 8], F32)
        B2 = SB(gs, "B2", [128, 8], F32)
        GT1 = SB(gs, "GT1", [128, D], F32)
        GT2 = SB(gs, "GT2", [128, D], F32)
        flg = SB(gs, "flg", [128, 1], F32)
        junk = SB(gs, "junk", [128, D], BF16)
        epsc = SB(gs, "epsc", [128, 1], F32)
        xn = [SB(gs, "xng%d" % i, [128, D], BF16) for i in range(2)]
        junkf = SB(gs, "junkf", [128, D], F32)

        def sumsq(src, srckeys, dst, dkey):
            S.op("act", lambda e: e.activation(out=junkf[:], in_=src, func=AF.Square), list(srckeys), ["junkf"])
            S.op("dve", lambda e: e.tensor_reduce(out=dst, in_=junkf[:], axis=AX.X, op=ALU.add), ["junkf", dkey], [dkey])
        hT = SB(gs, "hT", [128, 8, T], BF16)
        h2T = hT

        with ExitStack() as st:
            wada = [SB(st, "wada%d" % i, [128, 8, 1536], BF16) for i in range(2)]
            ccol = SB(st, "ccol", [128, 8], F32)
            scb = SB(st, "scb", [128, 8], BF16)
            scB = SB(st, "scB", [128, 8, 128], BF16)
            bcol = SB(st, "bcol", [128, 48], F32)
            modc = SB(st, "modc", [128, 48], F32)
            gpm = SB(st, "gpm", [128, 8], F32)
            gpf = SB(st, "gpf", [128, 8], F32)
            brow = SB(st, "brow", [128, 2, D], F32)
            grow = SB(st, "grow", [128, 2, D], F32)
            psmod = PS(st, "psmod", [128, 48], F32)
            psg = [PS(st, "psg%d" % i, [128, 512], F32) for i in range(4)]

            ms("pool", identf[:], 1.0, rd=[], wr=["tri_tmp"])
            S.op("pool", lambda e: e.affine_select(out=identf[:], in_=identf[:], pattern=[[-1, 128]],
                                                   compare_op=ALU.is_equal, fill=0.0, base=0, channel_multiplier=1),
                 ["tri_tmp"], ["tri_tmp"])
            cp("dve", identb[:], identf[:], rd=["tri_tmp"], wr=["identb"])
            dma("sp", tri_f[:], t_tri, wr=["tri_f"])
            cp("dve", tri_b[:], tri_f[:], rd=["tri_f"], wr=["tri_b"])
            ms("pool", epsc[:], 1e-6, wr=["epsc"])
            ms("pool", ones_f[:], 1.0, wr=["ones_f"])
            ms("pool", ones_b[:], 1.0, wr=["ones_b"])
            dma("sp", flg[:], flag, wr=["flg"])
            dma("sp", ccol[:], c_col, wr=["ccol"])
            dma("sp", bcol[:], b_ada_col, wr=["bcol"])
            dma("sp", gpm[:], gpm_col, wr=["gpm"])
            dma("sp", gpf[:], gpf_col, wr=["gpf"])
            dma("sp", brow[:, 0, :], b_ada_row[:, 2048:3072].partition_broadcast(128), wr=["brow0"])
            dma("sp", brow[:, 1, :], b_ada_row[:, 5120:6144].partition_broadcast(128), wr=["brow1"])
            dma("sp", grow[:, 0, :], gpostm.partition_broadcast(128), wr=["grow0"])
            dma("sp", grow[:, 1, :], gpostf.partition_broadcast(128), wr=["grow1"])
            act(scb[:], ccol[:], AF.Silu, rd=["ccol"], wr=["scb"])
            for kc in range(8):
                cp("dve", scB[:, kc, :], scb[:, kc:kc + 1].to_broadcast([128, 128]), rd=["scb"], wr=["scB"])
            wada_v = w_ada.rearrange("(k p) f -> p k f", p=128)
            for pc in range(4):
                wb = wada[pc % 2]
                key = "wada%d" % (pc % 2)
                for kc in range(8):
                    dma("pool", wb[:, kc, :], wada_v[:, kc, pc * 1536:(pc + 1) * 1536], wr=[key])
                for fl in range(12):
                    fc = pc * 12 + fl
                    for kc in range(8):
                        mm(psmod[:, fc:fc + 1], wb[:, kc, fl * 128:(fl + 1) * 128], scb[:, kc:kc + 1],
                           kc == 0, kc == 7, rd=[key, "scb"], wr=["psmod"])
                if pc in (1, 3):
                    j = 0 if pc == 1 else 1
                    for hb in range(2):
                        pt = psg[j * 2 + hb]
                        for kc in range(8):
                            mm(pt[:], scB[:, kc, :], wb[:, kc, 512 + hb * 512:512 + (hb + 1) * 512],
                               kc == 0, kc == 7, rd=[key, "scB"], wr=["psg%d" % (j * 2 + hb)])
                        dst = (GT1 if j == 0 else GT2)
                        tt("dve", dst[:, hb * 512:(hb + 1) * 512], pt[:], brow[:, j, hb * 512:(hb + 1) * 512], ALU.add,
                           rd=["psg%d" % (j * 2 + hb), "brow%d" % j], wr=["GT%d%d" % (j, hb)])
                        tt("dve", dst[:, hb * 512:(hb + 1) * 512], dst[:, hb * 512:(hb + 1) * 512],
                           grow[:, j, hb * 512:(hb + 1) * 512], ALU.mult,
                           rd=["GT%d%d" % (j, hb), "grow%d" % j], wr=["GT%d%d" % (j, hb)])
            tt("dve", modc[:], psmod[:], bcol[:], ALU.add, rd=["psmod", "bcol"], wr=["modc"])
            stt("dve", A1[:], modc[:, 8:16], 1.0, gpm[:], ALU.add, ALU.mult, rd=["modc", "gpm"], wr=["A1"])
            cp("dve", B1[:], modc[:, 0:8], rd=["modc"], wr=["B1"])
            stt("dve", A2[:], modc[:, 32:40], 1.0, gpf[:], ALU.add, ALU.mult, rd=["modc", "gpf"], wr=["A2"])
            cp("dve", B2[:], modc[:, 24:32], rd=["modc"], wr=["B2"])
            S.flush()

        def norm_transpose(src_f32, srckey, dstT, col0, Acol, Bcol, xn, pT, ss, idx, tag):
            import os
            STEP = int(os.environ.get("DBG_STEP", 99))
            if os.environ.get("DBG_XNJ"):
                xn = junk
            k = "%s%d" % (tag, idx % 2)
            if STEP < 2:
                return
            sumsq(src_f32, [srckey], ss[:, 0:1], "ss" + k)
            if STEP < 3:
                return
            act(ss[:, 1:2], ss[:, 0:1], AF.Sqrt, rd=["ss" + k], wr=["ss" + k], scale=1.0 / D, bias=epsc[:, 0:1])
            rcp(ss[:, 2:3], ss[:, 1:2], rd=["ss" + k], wr=["ss" + k])
            if STEP < 4:
                return
            act(xn[:], src_f32, AF.Identity, rd=[srckey, "ss" + k], wr=["xn" + k], scale=ss[:, 2:3])
            if STEP < 5:
                return
            for kc in range(8):
                tr(pT[:, kc * 128:(kc + 1) * 128], xn[:, kc * 128:(kc + 1) * 128], identb[:], rd=["xn" + k], wr=["pT" + k])
            if STEP < 6:
                return
            for kc in range(8):
                o = dstT[:, kc, col0:col0 + 128]
                i = pT[:, kc * 128:(kc + 1) * 128]
                if idx % 2 == 0:
                    act(o, i, AF.Identity, rd=["pT" + k], wr=[], scale=Acol[:, kc:kc + 1], bias=Bcol[:, kc:kc + 1])
                else:
                    ts("dve", o, i, Acol[:, kc:kc + 1], Bcol[:, kc:kc + 1], ALU.mult, ALU.add, rd=["pT" + k], wr=[])

        if stage == 0:
            dma("sp", dbg[:, 0:1024], GT1[:])
            dma("sp", dbg[:, 1024:2048], GT2[:])
            dma("sp", dbg[:, 2048:2056], A1[:])
            dma("sp", dbg[:, 2056:2064], B1[:])
            dma("sp", dbg[:, 2064:2072], A2[:])
            dma("sp", dbg[:, 2072:2080], B2[:])
            S.flush()
            return nc
        with ExitStack() as mix:
            yT = SB(mix, "yT", [128, 8, TO], BF16)
            with ExitStack() as st:
                xt = [SB(st, "xt%d" % i, [128, D], F32) for i in range(3)]
                ssb = [SB(st, "ssb%d" % i, [128, 4], F32) for i in range(2)]
                pT = [PS(st, "pT%d" % i, [128, D], BF16) for i in range(2)]
                import os
                for wt in range(int(os.environ.get("DBG_NT", NT))):
                    dma("sp", xt[wt % 3][:], xw_t[wt], wr=["xt%d" % (wt % 3)])
                    norm_transpose(xt[wt % 3][:], "xt%d" % (wt % 3), hT, wt * 128, A1, B1, xn[wt % 2], pT[wt % 2],
                                   ssb[wt % 2], wt, "b")
                S.flush()
            if stage == 1:
                with ExitStack() as st:
                    tmp = SB(st, "dbgt", [128, 8, TO], F32)
                    for kc in range(8):
                        cp("dve", tmp[:, kc, :], hT[:, kc, TO:T], wr=["t%d" % kc])
                        dma("sp", dbg[:, kc * TO:(kc + 1) * TO], tmp[:, kc, :], rd=["t%d" % kc])
                    S.flush()

            if stage >= 2:
              with ExitStack() as st:
                wing = SB(st, "win_g", [128, 8, 8], BF16)
                w_in_v = w_in.rearrange("(k p) f -> p k f", p=128)
                dma("pool", wing[:], w_in_v[:, :, GIF:GIF + 8], wr=["win"])
                winh = [SB(st, "win_h%d" % h_, [128, 8, 512], BF16) for h_ in range(2)]

                def load_winh(h_):
                    for jj, c0 in enumerate((QM(h_), KM(h_), VM(h_), OM(h_))):
                        dma("pool", winh[h_ % 2][:, :, jj * 128:(jj + 1) * 128], w_in_v[:, :, c0:c0 + 128], wr=["winh%d" % (h_ % 2)])

                load_winh(0)
                bg = SB(st, "bg", [128, 8], F32)
                dma("sp", bg[:], b_gates.partition_broadcast(128), wr=["bg"])
                convw = SB(st, "convw", [128, 8, 4], F32)
                dma("sp", convw[:], conv_col, wr=["convw"])
                gpre = SB(st, "gpre", [128, NT, 8], F32)
                lf = SB(st, "lf", [128, NT, 4], F32)
                gcs = SB(st, "gcs", [128, NT, 4], F32)
                ea = SB(st, "ea", [128, NT, 4], F32)
                qs = SB(st, "qs", [128, NT, 4], F32)
                eG = SB(st, "eG", [128, NT, 4], F32)
                psgt = PS(st, "psgt", [128, NT, 8], F32)
                for wt in range(NT):
                    for kc in range(8):
                        mm(psgt[:, wt, :], hT[:, kc, wt * 128:(wt + 1) * 128], wing[:, kc, :], kc == 0, kc == 7,
                           rd=["win"], wr=["psgt"])
                tt("dve", gpre[:], psgt[:], bg[:].unsqueeze(1).to_broadcast([128, NT, 8]), ALU.add, rd=["psgt", "bg"], wr=["gpre"])
                act(lf[:], gpre[:, :, 4:8], AF.Exp, rd=["gpre"], wr=["lf"], scale=-1.0)
                act(lf[:], lf[:], AF.Ln, rd=["lf"], wr=["lf"], bias=1.0)
                ts("dve", lf[:], lf[:], -1.0, None, ALU.mult, rd=["lf"], wr=["lf"])
                lf2 = lf[:].rearrange("p n h -> p (n h)")
                mm(psgt[:].rearrange("p n h -> p (n h)")[:, 0:128], tri_f[:], lf2, True, True, rd=["lf", "tri_f", "gpre"], wr=["psgt"])
                mm(psgt[:].rearrange("p n h -> p (n h)")[:, 128:256], ones_f[:], lf2, True, True, rd=["lf", "ones_f"], wr=["psgt"])
                psv = psgt[:].rearrange("p n h -> p (n h)")
                act(gcs[:].rearrange("p n h -> p (n h)"), psv[:, 0:128], AF.Copy, rd=["psgt"], wr=["gcs"])
                act(eG[:].rearrange("p n h -> p (n h)"), psv[:, 128:256], AF.Exp, rd=["psgt"], wr=["eG"])
                tt("dve", ea[:], gpre[:, :, 0:4], gcs[:], ALU.subtract, rd=["gpre", "gcs"], wr=["ea"])
                act(ea[:], ea[:], AF.Exp, rd=["ea"], wr=["ea"])
                act(qs[:], gcs[:], AF.Exp, rd=["gcs"], wr=["qs"])
                ts("dve", qs[:], qs[:], float(128 ** -0.5), None, ALU.mult, rd=["qs"], wr=["qs"])
                ts("dve", eG[:, 15, :], eG[:, 15, :], flg[:, 0:1], None, ALU.mult, rd=["eG", "flg"], wr=["eG"])
                S.flush()

                for hd in range(4):
                    with ExitStack() as hs:
                        win = winh[hd % 2]
                        if hd + 1 < 4:
                            load_winh(hd + 1)
                        kraw = SB(hs, "kraw", [128, T + 3], BF16)
                        qraw = SB(hs, "qraw", [128, TO + 3], BF16)
                        ctmp = [SB(hs, "ctmp%d" % i, [128, 1024], F32) for i in range(2)]
                        kT = SB(hs, "kT", [128, T], BF16)
                        qT = SB(hs, "qT", [128, TO], BF16)
                        vt = SB(hs, "vt", [128, NT, 129], BF16)
                        ktm = SB(hs, "ktm", [128, NT, 128], BF16)
                        og = SB(hs, "og", [128, NTO, 128], BF16)
                        Cst = SB(hs, "Cst", [128, 129], F32)
                        Ct = SB(hs, "Ct", [128, 129], F32)
                        Cbf = [SB(hs, "Cbf%d" % i, [128, 129], BF16) for i in range(2)]
                        Sm = [SB(hs, "Sm%d" % i, [128, 128], BF16) for i in range(2)]
                        sm4 = [SB(hs, "sm4%d" % i, [128, 4], F32) for i in range(2)]
                        ytm = [SB(hs, "ytm%d" % i, [128, 128], BF16) for i in range(2)]
                        pp = [PS(hs, "pp%d" % i, [128, 512], F32) for i in range(3)]
                        pCf = [PS(hs, "pCf%d" % i, [128, 512], F32) for i in range(2)]
                        pC = [t_[:, 0:129] for t_ in pCf]
                        pYf = [PS(hs, "pYf%d" % i, [128, 1024], BF16) for i in range(2)]
                        pY = [t_[:, 0:128] for t_ in pYf]
                        npp = [0]

                        def nextpp():
                            i = npp[0] % 3
                            npp[0] += 1
                            return pp[i], "pp%d" % i

                        ms("dve", kraw[:, 0:3], 0.0, wr=["kraw_h"])
                        for blk in range(8):
                            p, pk = nextpp()
                            for kc in range(8):
                                mm(p[:], win[:, kc, 128:256], hT[:, kc, blk * 512:(blk + 1) * 512], kc == 0, kc == 7, rd=["win"], wr=[pk])
                            act(kraw[:, 3 + blk * 512:3 + (blk + 1) * 512], p[:], AF.Copy, rd=[pk], wr=["kraw%d" % blk])
                        ts("dve", kraw[:, 3 + 2045:3 + 2048], kraw[:, 3 + 2045:3 + 2048], flg[:, 0:1], None, ALU.mult,
                           rd=["kraw3"], wr=["kraw3"])
                        p, pk = nextpp()
                        for kc in range(8):
                            mm(p[:, 0:128], win[:, kc, 0:128], hT[:, kc, 1920:2048], kc == 0, kc == 7, rd=["win"], wr=[pk])
                        ts("dve", qraw[:, 0:3], p[:, 125:128], flg[:, 0:1], None, ALU.mult, rd=[pk], wr=["qraw_h"])
                        for blk in range(4):
                            p, pk = nextpp()
                            for kc in range(8):
                                mm(p[:], win[:, kc, 0:128], hT[:, kc, TO + blk * 512:TO + (blk + 1) * 512],
                                   kc == 0, kc == 7, rd=["win"], wr=[pk])
                            act(qraw[:, 3 + blk * 512:3 + (blk + 1) * 512], p[:], AF.Copy, rd=[pk], wr=["qraw%d" % blk])
                        nct = [0]

                        def conv(raw, rawkeys, dstT, nblk, wch, eng):
                            for b in range(nblk):
                                ci = nct[0] % 2
                                nct[0] += 1
                                ck = "ctmp%d" % ci
                                c_ = ctmp[ci]
                                ts(eng, c_[:], raw[:, b * 1024:b * 1024 + 1024], convw[:, wch, 0:1], None, ALU.mult,
                                   rd=rawkeys, wr=[ck])
                                for j in range(1, 4):
                                    stt(eng, c_[:], raw[:, b * 1024 + j:b * 1024 + j + 1024], convw[:, wch, j:j + 1], c_[:],
                                        ALU.mult, ALU.add, rd=rawkeys + [ck], wr=[ck])
                                act(dstT[:, b * 1024:(b + 1) * 1024], c_[:], AF.Silu, rd=[ck], wr=["cv%d_%d" % (wch, b)])

                        conv(kraw, ["kraw%d" % b for b in range(8)] + ["kraw_h"], kT, 4, 4 + hd, "dve")
                        conv(qraw, ["qraw%d" % b for b in range(4)] + ["qraw_h"], qT, 2, hd, "dve")
                        kTkeys = ["cv%d_%d" % (4 + hd, b) for b in range(4)]
                        qTkeys = ["cv%d_%d" % (hd, b) for b in range(2)]
                        for wt in range(NT):
                            if wt % 4 == 0:
                                p, pk = nextpp()
                            o = p[:, (wt % 4) * 128:(wt % 4 + 1) * 128]
                            for kc in range(8):
                                mm(o, hT[:, kc, wt * 128:(wt + 1) * 128], win[:, kc, 256:384], kc == 0, kc == 7, rd=["win"], wr=[pk])
                            ts("dve", vt[:, wt, 0:128], o, ea[:, wt, hd:hd + 1], None, ALU.mult, rd=[pk], wr=["vt%d" % wt])
                            cp("pool", vt[:, wt, 128:129], ea[:, wt, hd:hd + 1], wr=["vt1_%d" % wt])
                        for i in range(NTO):
                            if i % 4 == 0:
                                p, pk = nextpp()
                            o = p[:, (i % 4) * 128:(i % 4 + 1) * 128]
                            for kc in range(8):
                                mm(o, hT[:, kc, TO + i * 128:TO + (i + 1) * 128], win[:, kc, 384:512], kc == 0, kc == 7, rd=["win"], wr=[pk])
                            act(og[:, i, :], o, AF.Sigmoid, rd=[pk], wr=["og%d" % i])
                        for wt in range(NT - 1):
                            py = pY[wt % 2]
                            tr(py, kT[:, wt * 128:(wt + 1) * 128], identb[:], rd=[kTkeys[wt // 8]], wr=["pY%d" % (wt % 2)])
                            act(ktm[:, wt, :], py, AF.Copy, rd=["pY%d" % (wt % 2)], wr=["ktm%d" % wt])
                        ms("dve", Cst[:], 0.0, wr=["Cst"])
                        ms("dve", Cbf[0][:], 0.0, wr=["Cbf0"])
                        for c in range(NT):
                            cb = Cbf[c % 2]
                            cbk = "Cbf%d" % (c % 2)
                            if c >= 16:
                                i = c - 16
                                p, pk = nextpp()
                                mm(p[:, 0:128], kT[:, c * 128:(c + 1) * 128], qT[:, i * 128:(i + 1) * 128], True, True,
                                   rd=[kTkeys[c // 8], qTkeys[i // 8]], wr=[pk])
                                sm = Sm[i % 2]
                                smk = "Sm%d" % (i % 2)
                                tt("dve", sm[:], p[:, 0:128], tri_f[:], ALU.mult, rd=[pk], wr=[smk])
                                pn = p[:, 256:385]
                                mm(pn, sm[:], vt[:, c, :], True, False, rd=[smk, "vt%d" % c, "vt1_%d" % c], wr=[pk])
                                mm(pn, qT[:, i * 128:(i + 1) * 128], cb[:], False, True, rd=[cbk, qTkeys[i // 8]], wr=[pk])
                                s4 = sm4[i % 2]
                                s4k = "sm4%d" % (i % 2)
                                ts("dve", s4[:, 0:1], p[:, 384:385], qs[:, c, hd:hd + 1], None, ALU.mult, rd=[pk], wr=[s4k])
                                ts("dve", s4[:, 3:4], s4[:, 0:1], -1.0, None, ALU.mult, rd=[s4k], wr=[s4k])
                                tt("dve", s4[:, 0:1], s4[:, 0:1], s4[:, 3:4], ALU.max, rd=[s4k], wr=[s4k])
                                ts("dve", s4[:, 0:1], s4[:, 0:1], 1.0, None, ALU.max, rd=[s4k], wr=[s4k])
                                rcp(s4[:, 1:2], s4[:, 0:1], rd=[s4k], wr=[s4k])
                                tt("dve", s4[:, 2:3], s4[:, 1:2], qs[:, c, hd:hd + 1], ALU.mult, rd=[s4k], wr=[s4k])
                                yt_ = ytm[i % 2]
                                ytk = "ytm%d" % (i % 2)
                                stt("dve", yt_[:], p[:, 256:384], s4[:, 2:3], og[:, i, :], ALU.mult, ALU.mult,
                                    rd=[pk, s4k, "og%d" % i], wr=[ytk])
                                py = pY[i % 2]
                                tr(py, yt_[:], identb[:], rd=[ytk], wr=["pY%d" % (i % 2)])
                                act(yT[:, hd, i * 128:(i + 1) * 128], py, AF.Copy, rd=["pY%d" % (i % 2)], wr=[])
                            if c < NT - 1:
                                pc_ = pC[c % 2]
                                pck = "pC%d" % (c % 2)
                                mm(pc_, ktm[:, c, :], vt[:, c, :], True, True, rd=["ktm%d" % c, "vt%d" % c, "vt1_%d" % c], wr=[pck])
                                tt("dve", Ct[:], pc_, Cst[:], ALU.add, rd=[pck, "Cst"], wr=["Ct"])
                                ts("dve", Cst[:], Ct[:], eG[:, c, hd:hd + 1], None, ALU.mult, rd=["Ct"], wr=["Cst"])
                                nb = Cbf[(c + 1) % 2]
                                act(nb[:], Ct[:], AF.Identity, rd=["Ct"], wr=["Cbf%d" % ((c + 1) % 2)], scale=eG[:, c, hd:hd + 1])
                        S.flush()
            if stage == 2:
                with ExitStack() as st:
                    tmp = SB(st, "dbgt", [128, 8, TO], F32)
                    for kc in range(8):
                        if kc < 4:
                            cp("dve", tmp[:, kc, :], yT[:, kc, :], wr=["t%d" % kc])
                        else:
                            ms("dve", tmp[:, kc, :], 0.0, wr=["t%d" % kc])
                        dma("sp", dbg[:, kc * TO:(kc + 1) * TO], tmp[:, kc, :], rd=["t%d" % kc])
                    S.flush()

            if stage >= 3:
              w_in_v = w_in.rearrange("(k p) f -> p k f", p=128)
              for g in range(2):
                with ExitStack() as gsx:
                    qaT = SB(gsx, "qaT", [64, 4, TO], BF16)
                    ksT = SB(gsx, "ksT", [64, T], BF16)
                    kwT = SB(gsx, "kwT", [64, T], BF16)
                    vsa = SB(gsx, "vsa", [128, NT, 65], BF16)
                    vwa = SB(gsx, "vwa", [128, NT, 65], BF16)
                    kcT = SB(gsx, "kcT", [64, 256], BF16)
                    vca = SB(gsx, "vca", [128, 2, 65], BF16)
                    gat = SB(gsx, "gat", [128, NTO, 12], F32)
                    with ExitStack() as st:
                        win = SB(st, "win_a", [128, 8, 652], BF16)
                        dma("pool", win[:, :, 0:256], w_in_v[:, :, QA(4 * g):QA(4 * g) + 256], wr=["win"])
                        dma("pool", win[:, :, 256:320], w_in_v[:, :, KC(g):KC(g) + 64], wr=["win"])
                        dma("pool", win[:, :, 320:384], w_in_v[:, :, VC(g):VC(g) + 64], wr=["win"])
                        dma("pool", win[:, :, 384:448], w_in_v[:, :, KS(g):KS(g) + 64], wr=["win"])
                        dma("pool", win[:, :, 448:512], w_in_v[:, :, KW(g):KW(g) + 64], wr=["win"])
                        dma("pool", win[:, :, 512:640], w_in_v[:, :, VSW(g):VSW(g) + 128], wr=["win"])
                        dma("pool", win[:, :, 640:652], w_in_v[:, :, GA + 12 * g:GA + 12 * g + 12], wr=["win"])
                        kcr = SB(st, "kcr", [64, T + 32], BF16)
                        vcr = SB(st, "vcr", [64, T + 32], BF16)
                        wk = SB(st, "wk", [64, 32, 64], BF16)
                        wv = SB(st, "wv", [64, 32, 64], BF16)
                        pk_ = SB(st, "pk_", [64, 32], BF16)
                        pv_ = SB(st, "pv_", [64, 32], BF16)
                        kcb = SB(st, "kcb", [64, 1], F32)
                        cvr = SB(st, "cvr", [1, 64], BF16)
                        dma("pool", wk[:], wck, wr=["wk"])
                        dma("pool", wv[:], wcv, wr=["wv"])
                        dma("pool", pk_[:], posk, wr=["pk_"])
                        dma("pool", pv_[:], posv, wr=["pv_"])
                        ms("dve", kcr[:, T:T + 32], 0.0, wr=["kcr_t"])
                        ms("dve", vcr[:, T:T + 32], 0.0, wr=["vcr_t"])
                        ms("pool", vsa[:, :, 64:65], 1.0, wr=["vsa1"])
                        ms("pool", vwa[:, :, 64:65], 1.0, wr=["vwa1"])
                        ms("pool", vca[:, :, 64:65], 1.0, wr=["vca1"])
                        ms("pool", kcT[:, 255:256], 0.0, wr=["kcT1"])
                        pp = [PS(st, "pp%d" % i, [128, 512], F32) for i in range(6)]
                        npp = [0]

                        def nextpp():
                            i = npp[0] % 6
                            npp[0] += 1
                            return pp[i], "pp%d" % i

                        ne = [0]

                        def evac(o, i, rd, wr, scale=None):
                            if int(rd[0][2:]) % 2 == 0:
                                if scale is None:
                                    act(o, i, AF.Copy, rd=rd, wr=wr)
                                else:
                                    act(o, i, AF.Identity, rd=rd, wr=wr, scale=scale)
                            else:
                                if scale is None:
                                    cp("dve", o, i, rd=rd, wr=wr)
                                else:
                                    ts("dve", o, i, scale, None, ALU.mult, rd=rd, wr=wr)

                        for h in range(4):
                            for blk in range(4):
                                p, pk = nextpp()
                                for kc in range(8):
                                    mm(p[0:64, :], win[:, kc, h * 64:(h + 1) * 64], hT[:, kc, TO + blk * 512:TO + (blk + 1) * 512],
                                       kc == 0, kc == 7, rd=["win"], wr=[pk])
                                evac(qaT[:, h, blk * 512:(blk + 1) * 512], p[0:64, :], [pk], [], scale=0.125)
                        for (dst, c0, nm) in ((kcr, 256, "kcr"), (vcr, 320, "vcr"), (ksT, 384, "ksT"), (kwT, 448, "kwT")):
                            for blk in range(8):
                                p, pk = nextpp()
                                for kc in range(8):
                                    mm(p[0:64, :], win[:, kc, c0:c0 + 64], hT[:, kc, blk * 512:(blk + 1) * 512], kc == 0, kc == 7,
                                       rd=["win"], wr=[pk])
                                evac(dst[:, blk * 512:(blk + 1) * 512], p[0:64, :], [pk], [nm])
                        for wt in range(NT):
                            if wt % 4 == 0:
                                p, pk = nextpp()
                            o = p[:, (wt % 4) * 128:(wt % 4 + 1) * 128]
                            for kc in range(8):
                                mm(o, hT[:, kc, wt * 128:(wt + 1) * 128], win[:, kc, 512:640], kc == 0, kc == 7, rd=["win"], wr=[pk])
                            evac(vsa[:, wt, 0:64], o[:, 0:64], [pk], [])
                            evac(vwa[:, wt, 0:64], o[:, 64:128], [pk], [])
                        for i in range(NTO):
                            if i % 4 == 0:
                                p, pk = nextpp()
                            o = p[:, (i % 4) * 16:(i % 4) * 16 + 12]
                            for kc in range(8):
                                mm(o, hT[:, kc, TO + i * 128:TO + (i + 1) * 128], win[:, kc, 640:652], kc == 0, kc == 7, rd=["win"], wr=[pk])
                            act(gat[:, i, :], o, AF.Sigmoid, rd=[pk], wr=[])
                        p, pk = nextpp()
                        for l in range(32):
                            mm(p[0:64, 0:255], wk[:, l, :], kcr[:, l:l + 4065:16], l == 0, l == 31,
                               rd=["wk", "kcr", "kcr_t"], wr=[pk])
                        p2, pk2 = nextpp()
                        for l in range(32):
                            mm(p2[0:64, 0:1], wk[:, l, :], pk_[:, l:l + 1], l == 0, l == 31, rd=["wk", "pk_"], wr=[pk2])
                        cp("dve", kcb[:], p2[0:64, 0:1], rd=[pk2], wr=["kcb"])
                        ts("dve", kcT[:, 0:255], p[0:64, 0:255], kcb[:, 0:1], None, ALU.add, rd=[pk, "kcb"], wr=[])
                        p3, pk3 = nextpp()
                        for l in range(32):
                            mm(p3[0:1, 0:64], pv_[:, l:l + 1], wv[:, l, :], l == 0, l == 31, rd=["wv", "pv_"], wr=[pk3])
                        cp("dve", cvr[:], p3[0:1, 0:64], rd=[pk3], wr=["cvr"])
                        for it in range(2):
                            p, pk = nextpp()
                            for l in range(32):
                                mm(p[:, 0:64], vcr[:, it * 2048 + l:it * 2048 + l + 2033:16], wv[:, l, :], l == 0, False,
                                   rd=["wv", "vcr", "vcr_t"], wr=[pk])
                            mm(p[:, 0:64], ones_b[0:1, :], cvr[:], False, True, rd=["cvr"], wr=[pk])
                            cp("dve", vca[:, it, 0:64], p[:, 0:64], rd=[pk], wr=[])
                        S.flush()

                    with ExitStack() as st:
                        E = SB(st, "E", [65, NT, 128], BF16)
                        dma("pool", E[:], t_E, wr=["E"])
                        ovl = SB(st, "ovl", [128, 2, 64], BF16)
                        dma("pool", ovl[:], t_ovl, wr=["ovl"])
                        wbt = SB(st, "wbt", [128, 10, 512], BF16)
                        dma("pool", wbt[:], t_wb[g], wr=["wbt"])
                        sbt = SB(st, "sbt", [128, 2, 512], BF16)
                        for dl in range(2):
                            dma("pool", sbt[:, dl, :], t_sb[g, dl], wr=["sbt"])
                        cst = SB(st, "cst", [1, 512], BF16)
                        dma("pool", cst[:], t_cst[g], wr=["cst"])
                        addt = SB(st, "addt", [128, NTO, 64], F32)
                        dma("sp", addt[:], t_add, wr=["addt"])
                        cbt = [SB(st, "cbt%d" % i, [128, 2, 512], BF16) for i in range(2)]
                        Pc = [SB(st, "Pc%d" % i, [128, 2, 512], BF16) for i in range(2)]
                        Pb = [SB(st, "Pb%d" % i, [128, 512], BF16) for i in range(4)]
                        snT = [SB(st, "snT%d" % i, [65, 4, 128], BF16) for i in range(2)]
                        for i_ in range(2):
                            dma("pool", snT[i_][64:65, :, :].rearrange("p h t -> p (h t)"), t_cst[g], wr=["snT%d" % i_])
                        sc = [SB(st, "sc%d" % i, [128, 64], F32) for i in range(2)]
                        sc2 = [SB(st, "sc2%d" % i, [128, 64], F32) for i in range(2)]
                        m8 = [SB(st, "m8%d" % i, [128, 8], F32) for i in range(2)]
                        nm = [SB(st, "nm%d" % i, [128, 64], BF16) for i in range(2)]
                        rc = [SB(st, "rc%d" % i, [128, 3, 4], F32) for i in range(2)]
                        oacc = [SB(st, "oacc%d" % i, [128, 4, 64], F32) for i in range(2)]
                        yab = [SB(st, "yab%d" % i, [128, 256], BF16) for i in range(2)]
                        osb = [SB(st, "osb%d" % i, [128, 260], F32) for i in range(2)]
                        pS = [PS(st, "pS%d" % i, [128, 512], F32) for i in range(4)]
                        pCR = PS(st, "pCR", [128, 512], F32)
                        pOf = [None] + [PS(st, "pO%d" % i, [128, 512], F32) for i in (1, 2)]
                        pO = [pCR[:, 0:256].rearrange("p (h d) -> p h d", h=4)] + \
                             [t_[:, 0:260].rearrange("p (h d) -> p h d", h=4) for t_ in pOf[1:]]
                        pR = pCR[:, 256:512].rearrange("p (h d) -> p h d", h=4)
                        pM = PS(st, "pM", [128, 1024], BF16)
                        nS = [0]

                        def nextS():
                            i = nS[0] % 4
                            nS[0] += 1
                            return pS[i], "pS%d" % i, Pb[i], "Pb%d" % i

                        def attn_tile(i):
                            c = 16 + i
                            b2 = i % 2
                            qslice = qaT[:, :, i * 128:(i + 1) * 128]
                            r_ = rc[b2]
                            rk = "rc%d" % b2
                            sn = snT[b2]
                            snk = "snT%d" % b2
                            v3 = lambda p: p[:].rearrange("p (h t) -> p h t", h=4)
                            gv = gat[:, i, :].rearrange("p (h b) -> p b h", b=3)
                            oa = oacc[b2]
                            ok = "oacc%d" % b2

                            def cmp_S(it):
                                def f():
                                    p, pk, _, _ = nextS()
                                    mm(v3(p), kcT[:, it * 128:(it + 1) * 128], qslice, True, False, wr=[pk])
                                    mm(p[:], identb[:], cbt[b2][:, it, :], False, True, rd=["cbt%d" % b2], wr=[pk])
                                    act(Pc[b2][:, it, :], p[:], AF.Exp, rd=[pk], wr=["Pc%d_%d" % (b2, it)])
                                return f

                            def cmp_PV():
                                for h in range(4):
                                    for it in range(2):
                                        mm(pO[0][:, h, :], Pc[b2][:, it, h * 128:(h + 1) * 128], vca[:, it, 0:64], it == 0, it == 1,
                                           rd=["Pc%d_%d" % (b2, it)], wr=["pO0"])
                                for h in range(4):
                                    for it in range(2):
                                        mm(pR[:, h, :], Pc[b2][:, it, h * 128:(h + 1) * 128], ovl[:, it, :], it == 0, it == 1,
                                           rd=["Pc%d_%d" % (b2, it), "ovl"], wr=["pO0"])
                                S.op("dve", lambda e: e.tensor_reduce(out=r_[:, 0, :], in_=pR, axis=AX.X, op=ALU.add), ["pO0"], [rk])
                                ts("dve", r_[:, 0, :], r_[:, 0, :], 1e-30, None, ALU.max, rd=[rk], wr=[rk])
                                rcp(r_[:, 0, :], r_[:, 0, :], rd=[rk], wr=[rk])
                                s_ = sc[b2]
                                sk = "sc%d" % b2
                                cp("dve", s_[:], addt[:, i, :], rd=["addt"], wr=[sk])
                                for h in range(4):
                                    stt("dve", s_[:], pR[:, h, :], r_[:, 0, h:h + 1], s_[:], ALU.mult, ALU.add, rd=["pO0", rk, sk], wr=[sk])
                                s2_ = sc2[b2]
                                s2k = "sc2%d" % b2
                                m_ = m8[b2]
                                mk = "m8%d" % b2
                                S.op("dve", lambda e: e.max(out=m_[:], in_=s_[:]), [sk], [mk])
                                S.op("dve", lambda e: e.match_replace(out=s2_[:], in_to_replace=m_[:], in_values=s_[:], imm_value=-1e30),
                                     [sk, mk], [s2k])
                                S.op("dve", lambda e: e.max(out=m_[:], in_=s2_[:]), [s2k], [mk])
                                ts("dve", s2_[:], s_[:], m_[:, 7:8], None, ALU.is_ge, rd=[sk, mk, s2k], wr=[s2k])
                                ts("dve", s_[:], s_[:], -1e5, None, ALU.is_ge, rd=[sk, s2k], wr=[sk])
                                tt("dve", s2_[:], s2_[:], s_[:], ALU.mult, rd=[sk, s2k], wr=[s2k])
                                n_ = nm[b2]
                                nk = "nm%d" % b2
                                ts("dve", n_[:], s2_[:], -1.0, -NEG, ALU.add, ALU.mult, rd=[s2k], wr=[nk])
                                tt("dve", r_[:, 0, :], r_[:, 0, :], gv[:, 0, :], ALU.mult, rd=[rk], wr=[rk])
                                for h in range(4):
                                    ts("dve", oa[:, h, :], pO[0][:, h, :], r_[:, 0, h:h + 1], None, ALU.mult, rd=["pO0", rk], wr=[ok])

                            def cmpB():
                                n_ = nm[b2]
                                tr(pM[0:64, 0:128], n_[:], identb[:], rd=["nm%d" % b2], wr=["pM"])
                                cp("dve", sn[0:64, :, :], pM[0:64, 0:128].unsqueeze(1).to_broadcast([64, 4, 128]), rd=["pM"], wr=[snk])

                            cmp_stages = [(cmp_S(0), None), (cmp_S(1), cmp_PV)]
                            win_stages = []
                            sel_stages = []

                            def pair(kT_, j, bias_fn, V_, vkey, acc, acck, first, last, use_sel):
                                st_ = {}

                                def fS():
                                    p, pk, pb, pbk = nextS()
                                    st_["x"] = (pb, pbk)
                                    mm(v3(p), kT_[:, j * 128:(j + 1) * 128], qslice, True, False, wr=[pk])
                                    if use_sel == 2:
                                        mm(p[:], E[:, j, :], sn[:].rearrange("p h t -> p (h t)"), False, True, rd=["E", snk], wr=[pk])
                                    else:
                                        if use_sel == 1:
                                            mm(p[:], E[0:64, j, :], sn[0:64, :, :].rearrange("p h t -> p (h t)"), False, False,
                                               rd=["E", snk], wr=[pk])
                                        bias_fn(p, pk)
                                    act(pb[:], p[:], AF.Exp, rd=[pk], wr=[pbk])

                                def fPV():
                                    pb, pbk = st_["x"]
                                    for h in range(4):
                                        mm(acc[:, h, :], pb[:, h * 128:(h + 1) * 128], V_[:, j, :], (first and h == 0), last,
                                           rd=[pbk, vkey], wr=[acck], skip=True)
                                return fS, fPV

                            for dl in range(4, -1, -1):
                                j = c - dl
                                var = dl if j >= 16 else 5 + dl
                                bf = (lambda var: lambda p, pk: mm(p[:], identb[:], wbt[:, var, :], False, True, rd=["wbt"], wr=[pk]))(var)
                                win_stages.append(pair(kwT, j, bf, vwa, "vwa1", pO[2], "pO2", dl == 4, dl == 0, 0))
                            for j in range(c + 1):
                                dl = c - j
                                if dl <= 1:
                                    bf = (lambda dl: lambda p, pk: mm(p[:], identb[:], sbt[:, dl, :], False, True, rd=["sbt"], wr=[pk]))(dl)
                                else:
                                    bf = lambda p, pk: mm(p[:], ones_b[0:1, :], cst[:], False, True, rd=["cst"], wr=[pk])
                                sel_stages.append(pair(ksT, j, bf, vsa, "vsa1", pO[1], "pO1", j == 0, j == c, 1 if dl <= 1 else 2))
                            def merge():
                                for br in (1, 2):
                                    cp("dve", osb[br - 1][:], pOf[br][:, 0:260], rd=["pO%d" % br], wr=["osb%d" % (br - 1)])
                                for br in (1, 2):
                                    ov = osb[br - 1][:, 0:260].rearrange("p (h d) -> p h d", h=4)
                                    ts("dve", r_[:, br, :], ov[:, :, 64], 1e-30, None, ALU.max, rd=["osb%d" % (br - 1), rk], wr=[rk])
                                    rcp(r_[:, br, :], r_[:, br, :], rd=[rk], wr=[rk])
                                    tt("dve", r_[:, br, :], r_[:, br, :], gv[:, br, :], ALU.mult, rd=[rk], wr=[rk])
                                ov1 = osb[0][:, 0:260].rearrange("p (h d) -> p h d", h=4)
                                ov2 = osb[1][:, 0:260].rearrange("p (h d) -> p h d", h=4)
                                for h in range(4):
                                    stt("dve", oa[:, h, :], ov1[:, h, 0:64], r_[:, 1, h:h + 1], oa[:, h, :], ALU.mult, ALU.add,
                                        rd=["osb0", rk, ok], wr=[ok])
                                    stt("dve", yab[b2][:, h * 64:(h + 1) * 64], ov2[:, h, 0:64], r_[:, 2, h:h + 1], oa[:, h, :],
                                        ALU.mult, ALU.add, rd=["osb1", rk, ok], wr=["yab%d" % b2])

                            def mergeB():
                                for hp in range(2):
                                    tr(pM[:, 128 + hp * 128:256 + hp * 128], yab[b2][:, hp * 128:(hp + 1) * 128], identb[:],
                                       rd=["yab%d" % b2], wr=["pM"])
                                    cp("dve", yT[:, 4 + 2 * g + hp, i * 128:(i + 1) * 128], pM[:, 128 + hp * 128:256 + hp * 128],
                                       rd=["pM"], wr=[])

                            return cmp_stages, win_stages, sel_stages, merge, mergeB, cmpB

                        tiles = []
                        allst = []
                        dma("pool", cbt[0][:], t_cb[g, 0], wr=["cbt0"])
                        tiles.append(attn_tile(0))
                        allst += tiles[0][0]
                        allst.append((None, tiles[0][5]))
                        for i in range(NTO):
                            cs, ws, ss_, mg, mgB, cB = tiles[i]
                            allst += ws
                            if i > 0:
                                allst.append((None, tiles[i - 1][4]))
                            if i + 1 < NTO:
                                allst.append(((lambda i=i: lambda: dma("pool", cbt[(i + 1) % 2][:], t_cb[g, i + 1],
                                                                       wr=["cbt%d" % ((i + 1) % 2)]))(), None))
                                tiles.append(attn_tile(i + 1))
                                allst += tiles[i + 1][0]
                            allst += ss_
                            allst.append((None, mg))
                            if i + 1 < NTO:
                                allst.append((None, tiles[i + 1][5]))
                        allst.append((None, tiles[NTO - 1][4]))
                        pend = []
                        for (fS, fPV) in allst:
                            if fS is not None:
                                fS()
                            if len(pend) >= 2:
                                f_ = pend.pop(0)
                                if f_ is not None:
                                    f_()
                            pend.append(fPV)
                        for f_ in pend:
                            if f_ is not None:
                                f_()
                        S.flush()
            if stage == 3:
                with ExitStack() as st:
                    tmp = SB(st, "dbgt", [128, 8, TO], F32)
                    for kc in range(8):
                        cp("dve", tmp[:, kc, :], yT[:, kc, :], wr=["t%d" % kc])
                        dma("sp", dbg[:, kc * TO:(kc + 1) * TO], tmp[:, kc, :], rd=["t%d" % kc])
                    S.flush()

            if stage >= 4:
              with ExitStack() as st:
                wo = SB(st, "wo", [128, 8, D], BF16)
                w_out_v = w_out.rearrange("(k p) f -> p k f", p=128)
                for kc in range(8):
                    dma("pool", wo[:, kc, :], w_out_v[:, kc, :], wr=["wo"])
                xo = [SB(st, "xo%d" % i, [128, D], F32) for i in range(2)]
                x1 = [SB(st, "x1%d" % i, [128, D], F32) for i in range(2)]
                ssb = [SB(st, "ssb%d" % i, [128, 4], F32) for i in range(2)]
                s5 = [SB(st, "s5%d" % i, [128, 4], F32) for i in range(2)]
                pT = [PS(st, "pT%d" % i, [128, D], BF16) for i in range(2)]
                pY = [PS(st, "pYe%d" % i, [128, D], F32) for i in range(2)]
                for i in range(NTO):
                    b2 = i % 2
                    dma("sp", xo[b2][:], xw_t[16 + i], wr=["xo%d" % b2])
                    py = pY[b2]
                    pyk = "pYe%d" % b2
                    for hb in range(2):
                        for kc in range(8):
                            mm(py[:, hb * 512:(hb + 1) * 512], yT[:, kc, i * 128:(i + 1) * 128], wo[:, kc, hb * 512:(hb + 1) * 512],
                               kc == 0, kc == 7, rd=["wo"], wr=[pyk])
                    s_ = s5[b2]
                    sk = "s5%d" % b2
                    S.op("act", (lambda py=py: lambda e: e.activation(out=junkf[:, 0:512], in_=py[:, 0:512], func=AF.Square))(), [pyk], ["junkf"])
                    S.op("act", (lambda py=py: lambda e: e.activation(out=junkf[:, 512:1024], in_=py[:, 512:1024], func=AF.Square))(), [pyk], ["junkf"])
                    S.op("dve", (lambda s_=s_: lambda e: e.tensor_reduce(out=s_[:, 2:3], in_=junkf[:], axis=AX.X, op=ALU.add))(), ["junkf", sk], [sk])
                    act(s_[:, 2:3], s_[:, 2:3], AF.Sqrt, rd=[sk], wr=[sk], scale=1.0 / D, bias=epsc[:, 0:1])
                    rcp(s_[:, 3:4], s_[:, 2:3], rd=[sk], wr=[sk])
                    xk = "x1%d" % b2
                    for hb in range(2):
                        stt("dve", x1[b2][:, hb * 512:(hb + 1) * 512], py[:, hb * 512:(hb + 1) * 512], s_[:, 3:4],
                            GT1[:, hb * 512:(hb + 1) * 512], ALU.mult, ALU.mult, rd=[pyk, sk], wr=[xk])
                    tt("pool", x1[b2][:], x1[b2][:], xo[b2][:], ALU.add, rd=[xk, "xo%d" % b2], wr=[xk])
                    import os
                    _de = int(os.environ.get("DBG_E", 0))
                    if _de != 1:
                        dma("sp", out_t[i], x1[b2][:], rd=[xk])
                    if _de != 2:
                        norm_transpose(x1[b2][:], xk, h2T, i * 128, A2, B2, xn[b2], pT[b2], ssb[b2], i, "e")
                S.flush()
        if stage == 4:
            with ExitStack() as st:
                tmp = SB(st, "dbgt", [128, 8, TO], F32)
                for kc in range(8):
                    cp("dve", tmp[:, kc, :], h2T[:, kc, 0:TO], wr=["t%d" % kc])
                    dma("sp", dbg[:, kc * TO:(kc + 1) * TO], tmp[:, kc, :], rd=["t%d" % kc])
                S.flush()

        if stage >= 5:
          with ExitStack() as st:
            yacc = SB(st, "yacc", [128, NTO, D], F32)
            gate = SB(st, "gate", [128, NTO, NE], F32)
            gateT = SB(st, "gateT", [NE, TO], BF16)
            bup = SB(st, "bup", [128, NE, 16], F32)
            dma("sp", bup[:], b_up_col, wr=["bup"])
            with ExitStack() as rs:
                wr_ = SB(rs, "wr_", [128, 8, NE], BF16)
                dma("pool", wr_[:], w_router.rearrange("(k p) e -> p k e", p=128), wr=["wr_"])
                brt = SB(rs, "brt", [128, NE], F32)
                dma("sp", brt[:], b_router.partition_broadcast(128), wr=["brt"])
                bdn = SB(rs, "bdn", [NE, D], BF16)
                dma("pool", bdn[:], b_down, wr=["bdn"])
                lg = [SB(rs, "lg%d" % i, [128, NE], F32) for i in range(2)]
                mk4 = [SB(rs, "mk4%d" % i, [128, NE], F32) for i in range(2)]
                m8 = [SB(rs, "m8r%d" % i, [128, 8], F32) for i in range(2)]
                s4 = [SB(rs, "s4r%d" % i, [128, 4], F32) for i in range(2)]
                pL = PS(rs, "pL", [128, NTO, NE], F32)
                pGf = [PS(rs, "pG%d" % i, [128, 512], F32) for i in range(2)]
                pG = [t_[0:NE, 0:128] for t_ in pGf]
                pB = [PS(rs, "pB%d" % i, [128, 512], F32) for i in range(2)]
                for i in range(NTO):
                    b2 = i % 2
                    for kc in range(8):
                        mm(pL[:, i, :], h2T[:, kc, i * 128:(i + 1) * 128], wr_[:, kc, :], kc == 0, kc == 7, rd=["wr_"], wr=["pL"])
                    l_ = lg[b2]
                    lk = "lg%d" % b2
                    tt("dve", l_[:], pL[:, i, :], brt[:], ALU.add, rd=["pL", "brt"], wr=[lk])
                    m_ = m8[b2]
                    mk = "m8r%d" % b2
                    S.op("dve", (lambda m_=m_, l_=l_: lambda e: e.max(out=m_[:], in_=l_[:]))(), [lk], [mk])
                    k4 = mk4[b2]
                    k4k = "mk4%d" % b2
                    ts("dve", k4[:], l_[:], m_[:, 3:4], None, ALU.is_ge, rd=[lk, mk], wr=[k4k])
                    s_ = s4[b2]
                    sk = "s4r%d" % b2
                    ts("dve", s_[:, 0:1], m_[:, 0:1], -1.0, None, ALU.mult, rd=[mk], wr=[sk])
                    act(l_[:], l_[:], AF.Exp, rd=[lk, sk, k4k], wr=[lk], bias=s_[:, 0:1])
                    tt("dve", l_[:], l_[:], k4[:], ALU.mult, rd=[lk, k4k], wr=[lk])
                    S.op("dve", (lambda s_=s_, l_=l_: lambda e: e.tensor_reduce(out=s_[:, 1:2], in_=l_[:], axis=AX.X, op=ALU.add))(),
                         [lk, sk], [sk])
                    rcp(s_[:, 2:3], s_[:, 1:2], rd=[sk], wr=[sk])
                    ts("dve", gate[:, i, :], l_[:], s_[:, 2:3], None, ALU.mult, rd=[lk, sk], wr=["gate%d" % i])
                    tr(pG[b2], gate[:, i, :], identf[:], rd=["gate%d" % i], wr=["pG%d" % b2])
                    cp("dve", gateT[:, i * 128:(i + 1) * 128], pG[b2], rd=["pG%d" % b2], wr=["gateT%d" % i])
                    for db in range(2):
                        mm(pB[db][:], gateT[:, i * 128:(i + 1) * 128], bdn[:, db * 512:(db + 1) * 512], True, True,
                           rd=["gateT%d" % i, "bdn"], wr=["pB%d" % db])
                        act(yacc[:, i, db * 512:(db + 1) * 512], pB[db][:], AF.Copy, rd=["pB%d" % db], wr=[])
                S.flush()

            with ExitStack() as es:
                wup = [hT[:, :, TO + i * 1024:TO + (i + 1) * 1024] for i in range(2)]
                wdn = [SB(es, "wdn%d" % i, [128, 4, D], BF16) for i in range(2)]
                gg = [SB(es, "gg%d" % i, [128, 512], F32) for i in range(2)]
                sg = [SB(es, "sg%d" % i, [128, 512], F32) for i in range(2)]
                ll = [SB(es, "ll%d" % i, [128, 512], F32) for i in range(2)]
                actT = [SB(es, "actT%d" % i, [128, 4, 512], BF16) for i in range(2)]
                pU = [PS(es, "pU%d" % i, [128, 512], F32) for i in range(4)]
                pD = [PS(es, "pD%d" % i, [128, 512], F32) for i in range(4)]
                nU = 0
                nD = 0
                nA = 0
                w_up_v = w_up.rearrange("e (k p) f -> e p k f", p=128)
                w_dn_v = w_down.rearrange("e (c p) d -> e p c d", p=128)
                units = [(e_, u) for e_ in range(NE) for u in range(2)]

                def load_unit(n):
                    e_, u = units[n]
                    wb = n % 2
                    for kc in range(8):
                        dma("pool", wup[wb][:, kc, 0:512], w_up_v[e_, :, kc, 512 * u:512 * u + 512], wr=["wup%d" % wb])
                        dma("pool", wup[wb][:, kc, 512:1024], w_up_v[e_, :, kc, 1024 + 512 * u:1024 + 512 * u + 512], wr=["wup%d" % wb])
                    for fc in range(4):
                        dma("pool", wdn[wb][:, fc, :], w_dn_v[e_, :, 4 * u + fc, :], wr=["wdn%d" % wb])

                load_unit(0)
                for n, (e_, u) in enumerate(units):
                    if n + 1 < len(units):
                        load_unit(n + 1)
                    wb = n % 2
                    wuk = "wup%d" % wb
                    wdk = "wdn%d" % wb
                    for tb in range(4):
                        ab = nA % 2
                        nA += 1
                        ak = "actT%d" % ab
                        for fc in range(4):
                            pg = pU[nU % 4]
                            pgk = "pU%d" % (nU % 4)
                            nU += 1
                            pl = pU[nU % 4]
                            plk = "pU%d" % (nU % 4)
                            nU += 1
                            for kc in range(8):
                                mm(pg[:], wup[wb][:, kc, fc * 128:(fc + 1) * 128], h2T[:, kc, tb * 512:(tb + 1) * 512],
                                   kc == 0, kc == 7, rd=[wuk], wr=[pgk])
                            for kc in range(8):
                                mm(pl[:], wup[wb][:, kc, 512 + fc * 128:512 + (fc + 1) * 128], h2T[:, kc, tb * 512:(tb + 1) * 512],
                                   kc == 0, kc == 7, rd=[wuk], wr=[plk])
                            b2 = fc % 2
                            jg = 4 * u + fc
                            ts("dve", gg[b2][:], pg[:], bup[:, e_, jg:jg + 1], 7.0, ALU.add, ALU.min, rd=[pgk], wr=["gg%d" % b2])
                            act(sg[b2][:], gg[b2][:], AF.Sigmoid, rd=["gg%d" % b2], wr=["sg%d" % b2], scale=1.702)
                            ts("dve", ll[b2][:], pl[:], bup[:, e_, 8 + jg:8 + jg + 1], 7.0, ALU.add, ALU.min, rd=[plk], wr=["ll%d" % b2])
                            ts("dve", ll[b2][:], ll[b2][:], -7.0, 1.0, ALU.max, ALU.add, rd=["ll%d" % b2], wr=["ll%d" % b2])
                            tt("dve", gg[b2][:], gg[b2][:], sg[b2][:], ALU.mult, rd=["gg%d" % b2, "sg%d" % b2], wr=["gg%d" % b2])
                            tt("dve", actT[ab][:, fc, :], gg[b2][:], ll[b2][:], ALU.mult, rd=["gg%d" % b2, "ll%d" % b2],
                               wr=[ak + "_%d" % fc])
                        for tt_ in range(4):
                            ti = tb * 4 + tt_
                            for db in range(2):
                                pd = pD[nD % 4]
                                pdk = "pD%d" % (nD % 4)
                                nD += 1
                                for fc in range(4):
                                    mm(pd[:], actT[ab][:, fc, tt_ * 128:(tt_ + 1) * 128], wdn[wb][:, fc, db * 512:(db + 1) * 512],
                                       fc == 0, fc == 3, rd=[ak + "_%d" % fc, wdk], wr=[pdk])
                                ya = yacc[:, ti, db * 512:(db + 1) * 512]
                                stt("dve", ya, pd[:], gate[:, ti, e_:e_ + 1], ya, ALU.mult, ALU.add, rd=[pdk, "ya%d_%d" % (ti, db)],
                                    wr=["ya%d_%d" % (ti, db)])
                S.flush()

            with ExitStack() as fs:
                xr = [SB(fs, "xr%d" % i, [128, D], F32) for i in range(2)]
                ob = [SB(fs, "ob%d" % i, [128, D], F32) for i in range(2)]
                s5 = [SB(fs, "s6%d" % i, [128, 4], F32) for i in range(2)]
                for i in range(NTO):
                    b2 = i % 2
                    dma("sp", xr[b2][:], out_t[i], wr=["xr%d" % b2])
                    s_ = s5[b2]
                    sk = "s6%d" % b2
                    sumsq(yacc[:, i, :], [], s_[:, 0:1], sk)
                    act(s_[:, 1:2], s_[:, 0:1], AF.Sqrt, rd=[sk], wr=[sk], scale=1.0 / D, bias=epsc[:, 0:1])
                    rcp(s_[:, 2:3], s_[:, 1:2], rd=[sk], wr=[sk])
                    stt("dve", ob[b2][:], yacc[:, i, :], s_[:, 2:3], GT2[:], ALU.mult, ALU.mult, rd=[sk], wr=["ob%d" % b2])
                    tt("pool", ob[b2][:], ob[b2][:], xr[b2][:], ALU.add, rd=["ob%d" % b2, "xr%d" % b2], wr=["ob%d" % b2])
                    dma("sp", out_t[i], ob[b2][:], rd=["ob%d" % b2])
                S.flush()
    return nc


_TABLE_CACHE = {}


def _tables(rel_bias, s):
    rb = np.asarray(rel_bias, np.float32)
    lo = 0 if s == 1 else 2048
    jl = np.arange(128)[:, None]
    tl = np.arange(128)[None, :]
    t_sb = np.empty((2, 2, 128, 4, 128), np.float32)
    t_wb = np.empty((2, 128, 10, 4, 128), np.float32)
    t_cst = np.empty((2, 1, 4, 128), np.float32)
    for g in range(2):
        for h in range(4):
            col = rb[:, 4 * g + h]
            t_cst[g, 0, h, :] = col[31]
            for dl in range(5):
                dist = dl * 128 + tl - jl
                bias = col[_t5_bucket(dist)]
                if dl < 2:
                    t_sb[g, dl, :, h, :] = np.where(dist >= 0, bias, np.float32(NEG))
                wv = np.where((dist >= 0) & (dist < 512), bias, np.float32(NEG))
                t_wb[g, :, dl, h, :] = wv
                t_wb[g, :, 5 + dl, h, :] = wv if s == 1 else np.float32(NEG)
    t_cb = np.empty((2, 16, 128, 2, 4, 128), np.float32)
    m = (np.arange(2)[None, :] * 128 + np.arange(128)[:, None])
    end = 16 * m + 31
    for i in range(16):
        tw = 2048 + 128 * i + np.arange(128)
        dist = tw[None, None, :] - end[:, :, None]
        valid = (16 * m[:, :, None] >= lo) & (dist >= 0) & (m[:, :, None] <= 254)
        bk = _t5_bucket(dist)
        for g in range(2):
            for h in range(4):
                t_cb[g, i, :, :, h, :] = np.where(valid, rb[:, 4 * g + h][bk], np.float32(NEG))
    t_add = np.empty((128, 16, 64), np.float32)
    blk = np.arange(64)[None, :]
    for i in range(16):
        tw = (2048 + 128 * i + np.arange(128))[:, None]
        cur = tw // 64
        first = lo // 64
        valid = (64 * blk >= lo) & (64 * blk <= tw)
        forced = (blk == first) | (blk == cur) | (blk == cur - 1)
        t_add[:, i, :] = np.where(valid, np.where(forced, np.float32(1000.0), np.float32(0.0)), np.float32(-1e6))
    return dict(t_sb=t_sb.reshape(2, 2, 128, 512), t_cst=t_cst.reshape(2, 1, 512), t_wb=t_wb.reshape(2, 128, 10, 512),
                t_cb=t_cb.reshape(2, 16, 128, 2, 512), t_add=t_add)


def _consts():
    E = np.zeros((65, 32, 128), np.float32)
    for j in range(32):
        for k in range(128):
            E[2 * j + k // 64, j, k] = 1.0
    E[64] = 1.0
    ovl = np.zeros((256, 64), np.float32)
    for m in range(255):
        for p in range(16 * m, 16 * m + 32):
            ovl[m, p // 64] += 1.0 / 32
    ovl = ovl.reshape(2, 128, 64).transpose(1, 0, 2).copy()
    tri = (np.arange(128)[:, None] <= np.arange(128)[None, :]).astype(np.float32)
    return dict(t_E=E, t_ovl=ovl, t_tri=tri)


_NC_CACHE = {}


def make_in_maps(inputs):
    f = lambda a: np.ascontiguousarray(np.asarray(a, np.float32))
    x = f(inputs["x"])
    c = f(inputs["c"])
    col = lambda v, n: f(np.asarray(v, np.float32).reshape(n, 128).T)
    perm = _perm()
    shared = dict(
        w_ada=f(inputs["w_ada"][0]),
        b_ada_col=col(inputs["b_ada"][0], 48),
        b_ada_row=f(inputs["b_ada"][0][None, :]),
        gpm_col=col(inputs["g_pre_mix"][0], 8),
        gpf_col=col(inputs["g_pre_ffn"][0], 8),
        gpostm=f(inputs["g_post_mix"][0][None, :]),
        gpostf=f(inputs["g_post_ffn"][0][None, :]),
        w_in=f(np.asarray(inputs["w_in"][0])[:, perm]),
        b_gates=f(inputs["b_gates"][0][None, :]),
        conv_col=f(np.asarray(inputs["conv_qk"][0]).reshape(4, 8, 128).transpose(2, 1, 0)),
        wck=f(np.asarray(inputs["w_cmp_k"][0]).transpose(1, 0, 2)),
        wcv=f(np.asarray(inputs["w_cmp_v"][0]).transpose(1, 0, 2)),
        posk=f(np.asarray(inputs["pos_cmp_k"][0]).T),
        posv=f(np.asarray(inputs["pos_cmp_v"][0]).T),
        w_out=f(inputs["w_out"][0]),
        w_router=f(inputs["w_router"][0]),
        b_router=f(inputs["b_router"][0][None, :]),
        w_up=f(inputs["w_up"][0]),
        b_up_col=f(np.asarray(inputs["b_up"][0]).reshape(NE, 16, 128).transpose(2, 0, 1)),
        w_down=f(inputs["w_down"][0]),
        b_down=f(inputs["b_down"][0]),
    )
    shared.update(_consts())
    tabs = [_tables(inputs["rel_bias"], s) for s in range(2)]
    in_maps = []
    for core in range(8):
        b, s = core // 2, core % 2
        m = dict(shared)
        m.update(tabs[s])
        if s == 1:
            m["xw"] = x[b]
        else:
            m["xw"] = np.ascontiguousarray(np.concatenate([x[b, TO:], x[b, :TO]], axis=0))
        m["c_col"] = col(c[b], 8)
        m["flag"] = np.full((128, 1), float(s), np.float32)
        in_maps.append(m)
    return in_maps


def kernel(**inputs):
    if "nc" not in _NC_CACHE:
        _NC_CACHE["nc"] = build_nc()
    nc = _NC_CACHE["nc"]
    in_maps = make_in_maps(inputs)
    res = run_bass_kernel_spmd(nc, in_maps, core_ids=list(range(8)))
    out = np.empty((4, 4096, D), np.float32)
    for core in range(8):
        b, s = core // 2, core % 2
        out[b, s * TO:(s + 1) * TO] = res.results[core]["out"]
    return out
```

```python
import numpy as np
from contextlib import ExitStack
import concourse.bass as bass
import concourse.mybir as mybir
from concourse.bass_utils import run_bass_kernel_spmd

F32 = mybir.dt.float32
BF16 = mybir.dt.bfloat16
AF = mybir.ActivationFunctionType
ALU = mybir.AluOpType
AX = mybir.AxisListType

COMPUTE = ("pe", "act", "dve", "pool")
NDMASEM = 24
NEG = -30000.0


class _Op:
    __slots__ = ("id", "eng", "fn", "deps", "dma", "needs_inc", "inc_idx", "dsem", "dval", "dprev")


class Sched:
    def __init__(self, nc, stack, same_engine_sync=True):
        self.nc = nc
        self.same = same_engine_sync
        self.sem = {e: stack.enter_context(nc.semaphore("sem_" + e)) for e in COMPUTE}
        self.dsem = [stack.enter_context(nc.semaphore("dsem%d" % i)) for i in range(NDMASEM)]
        self.bar = stack.enter_context(nc.semaphore("sem_bar"))
        self.cnt = {e: 0 for e in COMPUTE}
        self.ndma = 0
        self.dfinal = {}
        self.phase = 0
        self.scr = stack.enter_context(nc.sbuf_tensor("sched_scr", [128, 8], F32))
        self._reset()

    def _reset(self):
        self.ops = []
        self.lastw = {}
        self.readers = {}
        self.dma_hist = {}

    def _add(self, eng, fn, reads, writes, dma):
        deps = {}

        def add_dep(p):
            o = self.ops[p]
            if o.dma:
                deps[("d", p)] = p
            else:
                k = o.eng
                if k not in deps or deps[k] < p:
                    deps[k] = p

        for k in reads:
            for p in self.lastw.get(k, ()):
                add_dep(p)
        for k in writes:
            lw = self.lastw.get(k, ())
            if not (dma and lw and all(self.ops[p].dma for p in lw) and not self.readers.get(k)):
                for p in lw:
                    add_dep(p)
            for r in self.readers.get(k, ()):
                add_dep(r)
        op = _Op()
        op.id = len(self.ops)
        op.eng = eng
        op.fn = fn
        op.deps = sorted(set(deps.values()))
        op.dma = dma
        op.needs_inc = False
        op.inc_idx = 0
        op.dsem = None
        op.dval = 0
        op.dprev = None
        if dma:
            op.dsem = self.ndma % NDMASEM
            op.dval = 16 * (self.ndma // NDMASEM + 1)
            op.dprev = self.dma_hist.get(op.dsem)
            self.dma_hist[op.dsem] = op.id
            self.dfinal[op.dsem] = op.dval
            self.ndma += 1
        self.ops.append(op)
        for k in writes:
            lw = self.lastw.get(k, ())
            if dma and lw and all(self.ops[p].dma for p in lw) and not self.readers.get(k):
                self.lastw[k] = list(lw) + [op.id]
            else:
                self.lastw[k] = [op.id]
            self.readers[k] = []
        for k in reads:
            self.readers.setdefault(k, []).append(op.id)
        return op.id

    def op(self, eng, fn, reads=(), writes=()):
        return self._add(eng, fn, tuple(reads), tuple(writes), False)

    def dma(self, eng, fn, reads=(), writes=()):
        return self._add(eng, fn, tuple(reads), tuple(writes), True)

    def flush(self):
        nc = self.nc
        ops = self.ops
        for o in ops:
            for p in o.deps:
                po = ops[p]
                if po.dma:
                    continue
                if po.eng == o.eng and (po.eng == "pe" or not self.same):
                    continue
                po.needs_inc = True
        for o in ops:
            if not o.dma and o.needs_inc:
                self.cnt[o.eng] += 1
                o.inc_idx = self.cnt[o.eng]
        per = {e: [] for e in ("pe", "act", "dve", "pool", "sp")}
        for o in ops:
            per[o.eng].append(o)
        self.phase += 1
        phase = self.phase
        dfinal = dict(self.dfinal)
        sem, dsem, bar, scr = self.sem, self.dsem, self.bar, self.scr
        same = self.same

        def run(ename, e):
            waited_e = {f: 0 for f in COMPUTE}
            waited_d = {}
            for o in per[ename]:
                for p in o.deps:
                    po = ops[p]
                    if po.dma:
                        if waited_d.get(po.dsem, 0) < po.dval:
                            e.wait_ge(dsem[po.dsem], po.dval)
                            waited_d[po.dsem] = po.dval
                    else:
                        if not po.needs_inc:
                            continue
                        if po.eng == ename and (ename == "pe" or not same):
                            continue
                        if waited_e[po.eng] < po.inc_idx:
                            e.wait_ge(sem[po.eng], po.inc_idx)
                            waited_e[po.eng] = po.inc_idx
                if o.dma and o.dprev is not None:
                    po = ops[o.dprev]
                    if waited_d.get(po.dsem, 0) < po.dval:
                        e.wait_ge(dsem[po.dsem], po.dval)
                        waited_d[po.dsem] = po.dval
                inst = o.fn(e)
                if o.dma:
                    inst.then_inc(dsem[o.dsem], 16)
                elif o.needs_inc:
                    inst.then_inc(sem[o.eng], 1)
            if ename == "act":
                e.memzero(scr[0:1, 0:1]).then_inc(bar, 1)
            elif ename == "dve":
                e.memset(scr[0:1, 1:2], 0.0).then_inc(bar, 1)
            elif ename == "pool":
                e.memset(scr[0:1, 2:3], 0.0).then_inc(bar, 1)
            e.wait_ge(bar, 3 * phase)
            for s, v in dfinal.items():
                e.wait_ge(dsem[s], v)

        with nc.Block() as block:
            @block.tensor
            def _(e):
                run("pe", e)

            @block.scalar
            def _(e):
                run("act", e)

            @block.vector
            def _(e):
                run("dve", e)

            @block.gpsimd
            def _(e):
                run("pool", e)

            @block.sync
            def _(e):
                run("sp", e)
        self._reset()


D = 1024
T = 4096
TO = 2048
NT = 32
NTO = 16
D_IN = 3360
NE = 32
def KM(h): return 512 + 128 * h
def QM(h): return 128 * h
def VM(h): return 1024 + 128 * h
def OM(h): return 1536 + 128 * h
GIF = 2048
def QA(h8): return 2056 + 64 * h8
def KC(g): return 2568 + 64 * g
def VC(g): return 2696 + 64 * g
def KS(g): return 2824 + 64 * g
def KW(g): return 2952 + 64 * g
def VSW(g): return 3080 + 128 * g
GA = 3336


def _perm():
    p = list(range(D_IN))
    new = list(range(2568, 2952))
    new += list(range(3080, 3208))
    for g in range(2):
        new += list(range(2952 + 64 * g, 2952 + 64 * g + 64))
        new += list(range(3208 + 64 * g, 3208 + 64 * g + 64))
    p[2568:3336] = new
    return np.array(p)


def _t5_bucket(dist):
    n = np.maximum(dist, 0)
    nf = np.maximum(n, 1).astype(np.float32)
    large = 16 + (np.log(nf / np.float32(16)) / np.float32(np.log(128 / 16)) * np.float32(16)).astype(np.int32)
    large = np.minimum(large, 31)
    return np.where(n < 16, n, large)


def build_nc(stage=99, same=True):
    nc = bass.Bass("TRN2", target_bir_lowering=False)
    din = lambda name, shape: nc.dram_tensor(name, list(shape), F32, kind="ExternalInput").ap()
    xw = din("xw", [T, D])
    c_col = din("c_col", [128, 8])
    w_ada = din("w_ada", [D, 6 * D])
    b_ada_col = din("b_ada_col", [128, 48])
    b_ada_row = din("b_ada_row", [1, 6 * D])
    gpm_col = din("gpm_col", [128, 8])
    gpf_col = din("gpf_col", [128, 8])
    gpostm = din("gpostm", [1, D])
    gpostf = din("gpostf", [1, D])
    w_in = din("w_in", [D, D_IN])
    b_gates = din("b_gates", [1, 8])
    conv_col = din("conv_col", [128, 8, 4])
    wck = din("wck", [64, 32, 64])
    wcv = din("wcv", [64, 32, 64])
    posk = din("posk", [64, 32])
    posv = din("posv", [64, 32])
    w_out = din("w_out", [D, D])
    w_router = din("w_router", [D, NE])
    b_router = din("b_router", [1, NE])
    w_up = din("w_up", [NE, D, 2 * D])
    b_up_col = din("b_up_col", [128, NE, 16])
    w_down = din("w_down", [NE, D, D])
    b_down = din("b_down", [NE, D])
    flag = din("flag", [128, 1])
    t_sb = din("t_sb", [2, 2, 128, 512])
    t_cst = din("t_cst", [2, 1, 512])
    t_wb = din("t_wb", [2, 128, 10, 512])
    t_cb = din("t_cb", [2, 16, 128, 2, 512])
    t_add = din("t_add", [128, 16, 64])
    t_E = din("t_E", [65, 32, 128])
    t_ovl = din("t_ovl", [128, 2, 64])
    t_tri = din("t_tri", [128, 128])
    out = nc.dram_tensor("out", [TO, D], F32, kind="ExternalOutput").ap()
    dbg = None
    if stage < 99:
        dbg = nc.dram_tensor("dbg", [128, 8 * TO], F32, kind="ExternalOutput").ap()

    xw_t = xw.rearrange("(n p) d -> n p d", p=128)
    out_t = out.rearrange("(n p) d -> n p d", p=128)

    with ExitStack() as gs:
        S = Sched(nc, gs, same_engine_sync=same)
        _uid = [0]

        def _nm(name):
            _uid[0] += 1
            return "%s_u%d" % (name, _uid[0])

        SB = lambda st, name, shape, dt: st.enter_context(nc.sbuf_tensor(_nm(name), list(shape), dt))
        PS = lambda st, name, shape, dt: st.enter_context(nc.psum_tensor(_nm(name), list(shape), dt))

        def mm(o, l, r, start, stop, rd=(), wr=(), skip=False):
            if skip:
                S.op("pe", lambda e: e.matmul(o, lhsT=l, rhs=r, start=start, stop=stop, skip_group_check=True), rd, wr)
            else:
                S.op("pe", lambda e: e.matmul(o, lhsT=l, rhs=r, start=start, stop=stop), rd, wr)

        def tr(o, i, idn, rd=(), wr=()):
            S.op("pe", lambda e: e.transpose(o, i, idn), rd, wr)

        def act(o, i, func, rd=(), wr=(), **kw):
            S.op("act", lambda e: e.activation(out=o, in_=i, func=func, **kw), rd, wr)

        def ts(eng, o, i, s1, s2, op0, op1=None, rd=(), wr=()):
            if op1 is None:
                S.op(eng, lambda e: e.tensor_scalar(out=o, in0=i, scalar1=s1, scalar2=None, op0=op0), rd, wr)
            else:
                S.op(eng, lambda e: e.tensor_scalar(out=o, in0=i, scalar1=s1, scalar2=s2, op0=op0, op1=op1), rd, wr)

        def tt(eng, o, a, b, op, rd=(), wr=()):
            S.op(eng, lambda e: e.tensor_tensor(out=o, in0=a, in1=b, op=op), rd, wr)

        def stt(eng, o, a, sc, b, op0, op1, rd=(), wr=()):
            S.op(eng, lambda e: e.scalar_tensor_tensor(out=o, in0=a, scalar=sc, in1=b, op0=op0, op1=op1), rd, wr)

        def cp(eng, o, i, rd=(), wr=()):
            S.op(eng, lambda e: e.tensor_copy(out=o, in_=i), rd, wr)

        def ms(eng, o, v, rd=(), wr=()):
            S.op(eng, lambda e: e.memset(o, v), rd, wr)

        def dma(q, o, i, rd=(), wr=()):
            S.dma(q, lambda e: e.dma_start(out=o, in_=i), rd, wr)

        def rcp(o, i, rd=(), wr=()):
            S.op("dve", lambda e: e.reciprocal(out=o, in_=i), rd, wr)

        identb = SB(gs, "identb", [128, 128], BF16)
        identf = SB(gs, "identf", [128, 128], F32)
        tri_f = SB(gs, "tri_f", [128, 128], F32)
        tri_b = SB(gs, "tri_b", [128, 128], BF16)
        ones_f = SB(gs, "ones_f", [128, 128], F32)
        ones_b = SB(gs, "ones_b", [128, 128], BF16)
        A1 = SB(gs, "A1", [128, 8], F32)
        B1 = SB(gs, "B1", [128, 8], F32)
        A2 = SB(gs, "A2", [128, 8], F32)
        B2 = SB(gs, "B2", [128, 8], F32)
        GT1 = SB(gs, "GT1", [128, D], F32)
        GT2 = SB(gs, "GT2", [128, D], F32)
        flg = SB(gs, "flg", [128, 1], F32)
        junk = SB(gs, "junk", [128, D], BF16)
        epsc = SB(gs, "epsc", [128, 1], F32)
        xn = [SB(gs, "xng%d" % i, [128, D], BF16) for i in range(2)]
        junkf = SB(gs, "junkf", [128, D], F32)

        def sumsq(src, srckeys, dst, dkey):
            S.op("act", lambda e: e.activation(out=junkf[:], in_=src, func=AF.Square), list(srckeys), ["junkf"])
            S.op("dve", lambda e: e.tensor_reduce(out=dst, in_=junkf[:], axis=AX.X, op=ALU.add), ["junkf", dkey], [dkey])
        hT = SB(gs, "hT", [128, 8, T], BF16)
        h2T = hT

        with ExitStack() as st:
            wada = [SB(st, "wada%d" % i, [128, 8, 1536], BF16) for i in range(2)]
            ccol = SB(st, "ccol", [128, 8], F32)
            scb = SB(st, "scb", [128, 8], BF16)
            scB = SB(st, "scB", [128, 8, 128], BF16)
            bcol = SB(st, "bcol", [128, 48], F32)
            modc = SB(st, "modc", [128, 48], F32)
            gpm = SB(st, "gpm", [128, 8], F32)
            gpf = SB(st, "gpf", [128, 8], F32)
            brow = SB(st, "brow", [128, 2, D], F32)
            grow = SB(st, "grow", [128, 2, D], F32)
            psmod = PS(st, "psmod", [128, 48], F32)
            psg = [PS(st, "psg%d" % i, [128, 512], F32) for i in range(4)]

            ms("pool", identf[:], 1.0, rd=[], wr=["tri_tmp"])
            S.op("pool", lambda e: e.affine_select(out=identf[:], in_=identf[:], pattern=[[-1, 128]],
                                                   compare_op=ALU.is_equal, fill=0.0, base=0, channel_multiplier=1),
                 ["tri_tmp"], ["tri_tmp"])
            cp("dve", identb[:], identf[:], rd=["tri_tmp"], wr=["identb"])
            dma("sp", tri_f[:], t_tri, wr=["tri_f"])
            cp("dve", tri_b[:], tri_f[:], rd=["tri_f"], wr=["tri_b"])
            ms("pool", epsc[:], 1e-6, wr=["epsc"])
            ms("pool", ones_f[:], 1.0, wr=["ones_f"])
            ms("pool", ones_b[:], 1.0, wr=["ones_b"])
            dma("sp", flg[:], flag, wr=["flg"])
            dma("sp", ccol[:], c_col, wr=["ccol"])
            dma("sp", bcol[:], b_ada_col, wr=["bcol"])
            dma("sp", gpm[:], gpm_col, wr=["gpm"])
            dma("sp", gpf[:], gpf_col, wr=["gpf"])
            dma("sp", brow[:, 0, :], b_ada_row[:, 2048:3072].partition_broadcast(128), wr=["brow0"])
            dma("sp", brow[:, 1, :], b_ada_row[:, 5120:6144].partition_broadcast(128), wr=["brow1"])
            dma("sp", grow[:, 0, :], gpostm.partition_broadcast(128), wr=["grow0"])
            dma("sp", grow[:, 1, :], gpostf.partition_broadcast(128), wr=["grow1"])
            act(scb[:], ccol[:], AF.Silu, rd=["ccol"], wr=["scb"])
            for kc in range(8):
                cp("dve", scB[:, kc, :], scb[:, kc:kc + 1].to_broadcast([128, 128]), rd=["scb"], wr=["scB"])
            wada_v = w_ada.rearrange("(k p) f -> p k f", p=128)
            for pc in range(4):
                wb = wada[pc % 2]
                key = "wada%d" % (pc % 2)
                for kc in range(8):
                    dma("pool", wb[:, kc, :], wada_v[:, kc, pc * 1536:(pc + 1) * 1536], wr=[key])
                for fl in range(12):
                    fc = pc * 12 + fl
                    for kc in range(8):
                        mm(psmod[:, fc:fc + 1], wb[:, kc, fl * 128:(fl + 1) * 128], scb[:, kc:kc + 1],
                           kc == 0, kc == 7, rd=[key, "scb"], wr=["psmod"])
                if pc in (1, 3):
                    j = 0 if pc == 1 else 1
                    for hb in range(2):
                        pt = psg[j * 2 + hb]
                        for kc in range(8):
                            mm(pt[:], scB[:, kc, :], wb[:, kc, 512 + hb * 512:512 + (hb + 1) * 512],
                               kc == 0, kc == 7, rd=[key, "scB"], wr=["psg%d" % (j * 2 + hb)])
                        dst = (GT1 if j == 0 else GT2)
                        tt("dve", dst[:, hb * 512:(hb + 1) * 512], pt[:], brow[:, j, hb * 512:(hb + 1) * 512], ALU.add,
                           rd=["psg%d" % (j * 2 + hb), "brow%d" % j], wr=["GT%d%d" % (j, hb)])
                        tt("dve", dst[:, hb * 512:(hb + 1) * 512], dst[:, hb * 512:(hb + 1) * 512],
                           grow[:, j, hb * 512:(hb + 1) * 512], ALU.mult,
                           rd=["GT%d%d" % (j, hb), "grow%d" % j], wr=["GT%d%d" % (j, hb)])
            tt("dve", modc[:], psmod[:], bcol[:], ALU.add, rd=["psmod", "bcol"], wr=["modc"])
            stt("dve", A1[:], modc[:, 8:16], 1.0, gpm[:], ALU.add, ALU.mult, rd=["modc", "gpm"], wr=["A1"])
            cp("dve", B1[:], modc[:, 0:8], rd=["modc"], wr=["B1"])
            stt("dve", A2[:], modc[:, 32:40], 1.0, gpf[:], ALU.add, ALU.mult, rd=["modc", "gpf"], wr=["A2"])
            cp("dve", B2[:], modc[:, 24:32], rd=["modc"], wr=["B2"])
            S.flush()

        def norm_transpose(src_f32, srckey, dstT, col0, Acol, Bcol, xn, pT, ss, idx, tag):
            import os
            STEP = int(os.environ.get("DBG_STEP", 99))
            if os.environ.get("DBG_XNJ"):
                xn = junk
            k = "%s%d" % (tag, idx % 2)
            if STEP < 2:
                return
            sumsq(src_f32, [srckey], ss[:, 0:1], "ss" + k)
            if STEP < 3:
                return
            act(ss[:, 1:2], ss[:, 0:1], AF.Sqrt, rd=["ss" + k], wr=["ss" + k], scale=1.0 / D, bias=epsc[:, 0:1])
            rcp(ss[:, 2:3], ss[:, 1:2], rd=["ss" + k], wr=["ss" + k])
            if STEP < 4:
                return
            act(xn[:], src_f32, AF.Identity, rd=[srckey, "ss" + k], wr=["xn" + k], scale=ss[:, 2:3])
            if STEP < 5:
                return
            for kc in range(8):
                tr(pT[:, kc * 128:(kc + 1) * 128], xn[:, kc * 128:(kc + 1) * 128], identb[:], rd=["xn" + k], wr=["pT" + k])
            if STEP < 6:
                return
            for kc in range(8):
                o = dstT[:, kc, col0:col0 + 128]
                i = pT[:, kc * 128:(kc + 1) * 128]
                if idx % 2 == 0:
                    act(o, i, AF.Identity, rd=["pT" + k], wr=[], scale=Acol[:, kc:kc + 1], bias=Bcol[:, kc:kc + 1])
                else:
                    ts("dve", o, i, Acol[:, kc:kc + 1], Bcol[:, kc:kc + 1], ALU.mult, ALU.add, rd=["pT" + k], wr=[])

        if stage == 0:
            dma("sp", dbg[:, 0:1024], GT1[:])
            dma("sp", dbg[:, 1024:2048], GT2[:])
            dma("sp", dbg[:, 2048:2056], A1[:])
            dma("sp", dbg[:, 2056:2064], B1[:])
            dma("sp", dbg[:, 2064:2072], A2[:])
            dma("sp", dbg[:, 2072:2080], B2[:])
            S.flush()
            return nc
        with ExitStack() as mix:
            yT = SB(mix, "yT", [128, 8, TO], BF16)
            with ExitStack() as st:
                xt = [SB(st, "xt%d" % i, [128, D], F32) for i in range(3)]
                ssb = [SB(st, "ssb%d" % i, [128, 4], F32) for i in range(2)]
                pT = [PS(st, "pT%d" % i, [128, D], BF16) for i in range(2)]
                import os
                for wt in range(int(os.environ.get("DBG_NT", NT))):
                    dma("sp", xt[wt % 3][:], xw_t[wt], wr=["xt%d" % (wt % 3)])
                    norm_transpose(xt[wt % 3][:], "xt%d" % (wt % 3), hT, wt * 128, A1, B1, xn[wt % 2], pT[wt % 2],
                                   ssb[wt % 2], wt, "b")
                S.flush()
            if stage == 1:
                with ExitStack() as st:
                    tmp = SB(st, "dbgt", [128, 8, TO], F32)
                    for kc in range(8):
                        cp("dve", tmp[:, kc, :], hT[:, kc, TO:T], wr=["t%d" % kc])
                        dma("sp", dbg[:, kc * TO:(kc + 1) * TO], tmp[:, kc, :], rd=["t%d" % kc])
                    S.flush()

            if stage >= 2:
              with ExitStack() as st:
                wing = SB(st, "win_g", [128, 8, 8], BF16)
                w_in_v = w_in.rearrange("(k p) f -> p k f", p=128)
                dma("pool", wing[:], w_in_v[:, :, GIF:GIF + 8], wr=["win"])
                winh = [SB(st, "win_h%d" % h_, [128, 8, 512], BF16) for h_ in range(2)]

                def load_winh(h_):
                    for jj, c0 in enumerate((QM(h_), KM(h_), VM(h_), OM(h_))):
                        dma("pool", winh[h_ % 2][:, :, jj * 128:(jj + 1) * 128], w_in_v[:, :, c0:c0 + 128], wr=["winh%d" % (h_ % 2)])

                load_winh(0)
                bg = SB(st, "bg", [128, 8], F32)
                dma("sp", bg[:], b_gates.partition_broadcast(128), wr=["bg"])
                convw = SB(st, "convw", [128, 8, 4], F32)
                dma("sp", convw[:], conv_col, wr=["convw"])
                gpre = SB(st, "gpre", [128, NT, 8], F32)
                lf = SB(st, "lf", [128, NT, 4], F32)
                gcs = SB(st, "gcs", [128, NT, 4], F32)
                ea = SB(st, "ea", [128, NT, 4], F32)
                qs = SB(st, "qs", [128, NT, 4], F32)
                eG = SB(st, "eG", [128, NT, 4], F32)
                psgt = PS(st, "psgt", [128, NT, 8], F32)
                for wt in range(NT):
                    for kc in range(8):
                        mm(psgt[:, wt, :], hT[:, kc, wt * 128:(wt + 1) * 128], wing[:, kc, :], kc == 0, kc == 7,
                           rd=["win"], wr=["psgt"])
                tt("dve", gpre[:], psgt[:], bg[:].unsqueeze(1).to_broadcast([128, NT, 8]), ALU.add, rd=["psgt", "bg"], wr=["gpre"])
                act(lf[:], gpre[:, :, 4:8], AF.Exp, rd=["gpre"], wr=["lf"], scale=-1.0)
                act(lf[:], lf[:], AF.Ln, rd=["lf"], wr=["lf"], bias=1.0)
                ts("dve", lf[:], lf[:], -1.0, None, ALU.mult, rd=["lf"], wr=["lf"])
                lf2 = lf[:].rearrange("p n h -> p (n h)")
                mm(psgt[:].rearrange("p n h -> p (n h)")[:, 0:128], tri_f[:], lf2, True, True, rd=["lf", "tri_f", "gpre"], wr=["psgt"])
                mm(psgt[:].rearrange("p n h -> p (n h)")[:, 128:256], ones_f[:], lf2, True, True, rd=["lf", "ones_f"], wr=["psgt"])
                psv = psgt[:].rearrange("p n h -> p (n h)")
                act(gcs[:].rearrange("p n h -> p (n h)"), psv[:, 0:128], AF.Copy, rd=["psgt"], wr=["gcs"])
                act(eG[:].rearrange("p n h -> p (n h)"), psv[:, 128:256], AF.Exp, rd=["psgt"], wr=["eG"])
                tt("dve", ea[:], gpre[:, :, 0:4], gcs[:], ALU.subtract, rd=["gpre", "gcs"], wr=["ea"])
                act(ea[:], ea[:], AF.Exp, rd=["ea"], wr=["ea"])
                act(qs[:], gcs[:], AF.Exp, rd=["gcs"], wr=["qs"])
                ts("dve", qs[:], qs[:], float(128 ** -0.5), None, ALU.mult, rd=["qs"], wr=["qs"])
                ts("dve", eG[:, 15, :], eG[:, 15, :], flg[:, 0:1], None, ALU.mult, rd=["eG", "flg"], wr=["eG"])
                S.flush()

                for hd in range(4):
                    with ExitStack() as hs:
                        win = winh[hd % 2]
                        if hd + 1 < 4:
                            load_winh(hd + 1)
                        kraw = SB(hs, "kraw", [128, T + 3], BF16)
                        qraw = SB(hs, "qraw", [128, TO + 3], BF16)
                        ctmp = [SB(hs, "ctmp%d" % i, [128, 1024], F32) for i in range(2)]
                        kT = SB(hs, "kT", [128, T], BF16)
                        qT = SB(hs, "qT", [128, TO], BF16)
                        vt = SB(hs, "vt", [128, NT, 129], BF16)
                        ktm = SB(hs, "ktm", [128, NT, 128], BF16)
                        og = SB(hs, "og", [128, NTO, 128], BF16)
                        Cst = SB(hs, "Cst", [128, 129], F32)
                        Ct = SB(hs, "Ct", [128, 129], F32)
                        Cbf = [SB(hs, "Cbf%d" % i, [128, 129], BF16) for i in range(2)]
                        Sm = [SB(hs, "Sm%d" % i, [128, 128], BF16) for i in range(2)]
                        sm4 = [SB(hs, "sm4%d" % i, [128, 4], F32) for i in range(2)]
                        ytm = [SB(hs, "ytm%d" % i, [128, 128], BF16) for i in range(2)]
                        pp = [PS(hs, "pp%d" % i, [128, 512], F32) for i in range(3)]
                        pCf = [PS(hs, "pCf%d" % i, [128, 512], F32) for i in range(2)]
                        pC = [t_[:, 0:129] for t_ in pCf]
                        pYf = [PS(hs, "pYf%d" % i, [128, 1024], BF16) for i in range(2)]
                        pY = [t_[:, 0:128] for t_ in pYf]
                        npp = [0]

                        def nextpp():
                            i = npp[0] % 3
                            npp[0] += 1
                            return pp[i], "pp%d" % i

                        ms("dve", kraw[:, 0:3], 0.0, wr=["kraw_h"])
                        for blk in range(8):
                            p, pk = nextpp()
                            for kc in range(8):
                                mm(p[:], win[:, kc, 128:256], hT[:, kc, blk * 512:(blk + 1) * 512], kc == 0, kc == 7, rd=["win"], wr=[pk])
                            act(kraw[:, 3 + blk * 512:3 + (blk + 1) * 512], p[:], AF.Copy, rd=[pk], wr=["kraw%d" % blk])
                        ts("dve", kraw[:, 3 + 2045:3 + 2048], kraw[:, 3 + 2045:3 + 2048], flg[:, 0:1], None, ALU.mult,
                           rd=["kraw3"], wr=["kraw3"])
                        p, pk = nextpp()
                        for kc in range(8):
                            mm(p[:, 0:128], win[:, kc, 0:128], hT[:, kc, 1920:2048], kc == 0, kc == 7, rd=["win"], wr=[pk])
                        ts("dve", qraw[:, 0:3], p[:, 125:128], flg[:, 0:1], None, ALU.mult, rd=[pk], wr=["qraw_h"])
                        for blk in range(4):
                            p, pk = nextpp()
                            for kc in range(8):
                                mm(p[:], win[:, kc, 0:128], hT[:, kc, TO + blk * 512:TO + (blk + 1) * 512],
                                   kc == 0, kc == 7, rd=["win"], wr=[pk])
                            act(qraw[:, 3 + blk * 512:3 + (blk + 1) * 512], p[:], AF.Copy, rd=[pk], wr=["qraw%d" % blk])
                        nct = [0]

                        def conv(raw, rawkeys, dstT, nblk, wch, eng):
                            for b in range(nblk):
                                ci = nct[0] % 2
                                nct[0] += 1
                                ck = "ctmp%d" % ci
                                c_ = ctmp[ci]
                                ts(eng, c_[:], raw[:, b * 1024:b * 1024 + 1024], convw[:, wch, 0:1], None, ALU.mult,
                                   rd=rawkeys, wr=[ck])
                                for j in range(1, 4):
                                    stt(eng, c_[:], raw[:, b * 1024 + j:b * 1024 + j + 1024], convw[:, wch, j:j + 1], c_[:],
                                        ALU.mult, ALU.add, rd=rawkeys + [ck], wr=[ck])
                                act(dstT[:, b * 1024:(b + 1) * 1024], c_[:], AF.Silu, rd=[ck], wr=["cv%d_%d" % (wch, b)])

                        conv(kraw, ["kraw%d" % b for b in range(8)] + ["kraw_h"], kT, 4, 4 + hd, "dve")
                        conv(qraw, ["qraw%d" % b for b in range(4)] + ["qraw_h"], qT, 2, hd, "dve")
                        kTkeys = ["cv%d_%d" % (4 + hd, b) for b in range(4)]
                        qTkeys = ["cv%d_%d" % (hd, b) for b in range(2)]
                        for wt in range(NT):
                            if wt % 4 == 0:
                                p, pk = nextpp()
                            o = p[:, (wt % 4) * 128:(wt % 4 + 1) * 128]
                            for kc in range(8):
                                mm(o, hT[:, kc, wt * 128:(wt + 1) * 128], win[:, kc, 256:384], kc == 0, kc == 7, rd=["win"], wr=[pk])
                            ts("dve", vt[:, wt, 0:128], o, ea[:, wt, hd:hd + 1], None, ALU.mult, rd=[pk], wr=["vt%d" % wt])
                            cp("pool", vt[:, wt, 128:129], ea[:, wt, hd:hd + 1], wr=["vt1_%d" % wt])
                        for i in range(NTO):
                            if i % 4 == 0:
                                p, pk = nextpp()
                            o = p[:, (i % 4) * 128:(i % 4 + 1) * 128]
                            for kc in range(8):
                                mm(o, hT[:, kc, TO + i * 128:TO + (i + 1) * 128], win[:, kc, 384:512], kc == 0, kc == 7, rd=["win"], wr=[pk])
                            act(og[:, i, :], o, AF.Sigmoid, rd=[pk], wr=["og%d" % i])
                        for wt in range(NT - 1):
                            py = pY[wt % 2]
                            tr(py, kT[:, wt * 128:(wt + 1) * 128], identb[:], rd=[kTkeys[wt // 8]], wr=["pY%d" % (wt % 2)])
                            act(ktm[:, wt, :], py, AF.Copy, rd=["pY%d" % (wt % 2)], wr=["ktm%d" % wt])
                        ms("dve", Cst[:], 0.0, wr=["Cst"])
                        ms("dve", Cbf[0][:], 0.0, wr=["Cbf0"])
                        for c in range(NT):
                            cb = Cbf[c % 2]
                            cbk = "Cbf%d" % (c % 2)
                            if c >= 16:
                                i = c - 16
                                p, pk = nextpp()
                                mm(p[:, 0:128], kT[:, c * 128:(c + 1) * 128], qT[:, i * 128:(i + 1) * 128], True, True,
                                   rd=[kTkeys[c // 8], qTkeys[i // 8]], wr=[pk])
                                sm = Sm[i % 2]
                                smk = "Sm%d" % (i % 2)
                                tt("dve", sm[:], p[:, 0:128], tri_f[:], ALU.mult, rd=[pk], wr=[smk])
                                pn = p[:, 256:385]
                                mm(pn, sm[:], vt[:, c, :], True, False, rd=[smk, "vt%d" % c, "vt1_%d" % c], wr=[pk])
                                mm(pn, qT[:, i * 128:(i + 1) * 128], cb[:], False, True, rd=[cbk, qTkeys[i // 8]], wr=[pk])
                                s4 = sm4[i % 2]
                                s4k = "sm4%d" % (i % 2)
                                ts("dve", s4[:, 0:1], p[:, 384:385], qs[:, c, hd:hd + 1], None, ALU.mult, rd=[pk], wr=[s4k])
                                ts("dve", s4[:, 3:4], s4[:, 0:1], -1.0, None, ALU.mult, rd=[s4k], wr=[s4k])
                                tt("dve", s4[:, 0:1], s4[:, 0:1], s4[:, 3:4], ALU.max, rd=[s4k], wr=[s4k])
                                ts("dve", s4[:, 0:1], s4[:, 0:1], 1.0, None, ALU.max, rd=[s4k], wr=[s4k])
                                rcp(s4[:, 1:2], s4[:, 0:1], rd=[s4k], wr=[s4k])
                                tt("dve", s4[:, 2:3], s4[:, 1:2], qs[:, c, hd:hd + 1], ALU.mult, rd=[s4k], wr=[s4k])
                                yt_ = ytm[i % 2]
                                ytk = "ytm%d" % (i % 2)
                                stt("dve", yt_[:], p[:, 256:384], s4[:, 2:3], og[:, i, :], ALU.mult, ALU.mult,
                                    rd=[pk, s4k, "og%d" % i], wr=[ytk])
                            if c < NT - 1:
                                pc_ = pC[c % 2]
                                pck = "pC%d" % (c % 2)
                                mm(pc_, ktm[:, c, :], vt[:, c, :], True, True, rd=["ktm%d" % c, "vt%d" % c, "vt1_%d" % c], wr=[pck])
                                tt("dve", Ct[:], pc_, Cst[:], ALU.add, rd=[pck, "Cst"], wr=["Ct"])
                                ts("dve", Cst[:], Ct[:], eG[:, c, hd:hd + 1], None, ALU.mult, rd=["Ct"], wr=["Cst"])
                                nb = Cbf[(c + 1) % 2]
                                act(nb[:], Ct[:], AF.Identity, rd=["Ct"], wr=["Cbf%d" % ((c + 1) % 2)], scale=eG[:, c, hd:hd + 1])
                            if c >= 16:
                                i = c - 16
                                py = pY[i % 2]
                                tr(py, ytm[i % 2][:], identb[:], rd=["ytm%d" % (i % 2)], wr=["pY%d" % (i % 2)])
                                act(yT[:, hd, i * 128:(i + 1) * 128], py, AF.Copy, rd=["pY%d" % (i % 2)], wr=[])
                        S.flush()
            if stage == 2:
                with ExitStack() as st:
                    tmp = SB(st, "dbgt", [128, 8, TO], F32)
                    for kc in range(8):
                        if kc < 4:
                            cp("dve", tmp[:, kc, :], yT[:, kc, :], wr=["t%d" % kc])
                        else:
                            ms("dve", tmp[:, kc, :], 0.0, wr=["t%d" % kc])
                        dma("sp", dbg[:, kc * TO:(kc + 1) * TO], tmp[:, kc, :], rd=["t%d" % kc])
                    S.flush()

            if stage >= 3:
              w_in_v = w_in.rearrange("(k p) f -> p k f", p=128)
              for g in range(2):
                with ExitStack() as gsx:
                    qaT = SB(gsx, "qaT", [64, 4, TO], BF16)
                    ksT = SB(gsx, "ksT", [64, T], BF16)
                    kwT = SB(gsx, "kwT", [64, T], BF16)
                    vsa = SB(gsx, "vsa", [128, NT, 65], BF16)
                    vwa = SB(gsx, "vwa", [128, NT, 65], BF16)
                    kcT = SB(gsx, "kcT", [64, 256], BF16)
                    vca = SB(gsx, "vca", [128, 2, 65], BF16)
                    gat = SB(gsx, "gat", [128, NTO, 12], F32)
                    with ExitStack() as st:
                        win = SB(st, "win_a", [128, 8, 652], BF16)
                        dma("pool", win[:, :, 0:256], w_in_v[:, :, QA(4 * g):QA(4 * g) + 256], wr=["win"])
                        dma("pool", win[:, :, 256:320], w_in_v[:, :, KC(g):KC(g) + 64], wr=["win"])
                        dma("pool", win[:, :, 320:384], w_in_v[:, :, VC(g):VC(g) + 64], wr=["win"])
                        dma("pool", win[:, :, 384:448], w_in_v[:, :, KS(g):KS(g) + 64], wr=["win"])
                        dma("pool", win[:, :, 448:512], w_in_v[:, :, KW(g):KW(g) + 64], wr=["win"])
                        dma("pool", win[:, :, 512:640], w_in_v[:, :, VSW(g):VSW(g) + 128], wr=["win"])
                        dma("pool", win[:, :, 640:652], w_in_v[:, :, GA + 12 * g:GA + 12 * g + 12], wr=["win"])
                        kcr = SB(st, "kcr", [64, T + 32], BF16)
                        vcr = SB(st, "vcr", [64, T + 32], BF16)
                        wk = SB(st, "wk", [64, 32, 64], BF16)
                        wv = SB(st, "wv", [64, 32, 64], BF16)
                        pk_ = SB(st, "pk_", [64, 32], BF16)
                        pv_ = SB(st, "pv_", [64, 32], BF16)
                        kcb = SB(st, "kcb", [64, 1], F32)
                        cvr = SB(st, "cvr", [1, 64], BF16)
                        dma("pool", wk[:], wck, wr=["wk"])
                        dma("pool", wv[:], wcv, wr=["wv"])
                        dma("pool", pk_[:], posk, wr=["pk_"])
                        dma("pool", pv_[:], posv, wr=["pv_"])
                        ms("dve", kcr[:, T:T + 32], 0.0, wr=["kcr_t"])
                        ms("dve", vcr[:, T:T + 32], 0.0, wr=["vcr_t"])
                        ms("pool", vsa[:, :, 64:65], 1.0, wr=["vsa1"])
                        ms("pool", vwa[:, :, 64:65], 1.0, wr=["vwa1"])
                        ms("pool", vca[:, :, 64:65], 1.0, wr=["vca1"])
                        ms("pool", kcT[:, 255:256], 0.0, wr=["kcT1"])
                        pp = [PS(st, "pp%d" % i, [128, 512], F32) for i in range(6)]
                        npp = [0]

                        def nextpp():
                            i = npp[0] % 6
                            npp[0] += 1
                            return pp[i], "pp%d" % i

                        ne = [0]

                        def evac(o, i, rd, wr, scale=None):
                            if int(rd[0][2:]) % 2 == 0:
                                if scale is None:
                                    act(o, i, AF.Copy, rd=rd, wr=wr)
                                else:
                                    act(o, i, AF.Identity, rd=rd, wr=wr, scale=scale)
                            else:
                                if scale is None:
                                    cp("dve", o, i, rd=rd, wr=wr)
                                else:
                                    ts("dve", o, i, scale, None, ALU.mult, rd=rd, wr=wr)

                        for h in range(4):
                            for blk in range(4):
                                p, pk = nextpp()
                                for kc in range(8):
                                    mm(p[0:64, :], win[:, kc, h * 64:(h + 1) * 64], hT[:, kc, TO + blk * 512:TO + (blk + 1) * 512],
                                       kc == 0, kc == 7, rd=["win"], wr=[pk])
                                evac(qaT[:, h, blk * 512:(blk + 1) * 512], p[0:64, :], [pk], [], scale=0.125)
                        for (dst, c0, nm) in ((kcr, 256, "kcr"), (vcr, 320, "vcr"), (ksT, 384, "ksT"), (kwT, 448, "kwT")):
                            for blk in range(8):
                                p, pk = nextpp()
                                for kc in range(8):
                                    mm(p[0:64, :], win[:, kc, c0:c0 + 64], hT[:, kc, blk * 512:(blk + 1) * 512], kc == 0, kc == 7,
                                       rd=["win"], wr=[pk])
                                evac(dst[:, blk * 512:(blk + 1) * 512], p[0:64, :], [pk], [nm])
                        for wt in range(NT):
                            if wt % 4 == 0:
                                p, pk = nextpp()
                            o = p[:, (wt % 4) * 128:(wt % 4 + 1) * 128]
                            for kc in range(8):
                                mm(o, hT[:, kc, wt * 128:(wt + 1) * 128], win[:, kc, 512:640], kc == 0, kc == 7, rd=["win"], wr=[pk])
                            evac(vsa[:, wt, 0:64], o[:, 0:64], [pk], [])
                            evac(vwa[:, wt, 0:64], o[:, 64:128], [pk], [])
                        for i in range(NTO):
                            if i % 4 == 0:
                                p, pk = nextpp()
                            o = p[:, (i % 4) * 16:(i % 4) * 16 + 12]
                            for kc in range(8):
                                mm(o, hT[:, kc, TO + i * 128:TO + (i + 1) * 128], win[:, kc, 640:652], kc == 0, kc == 7, rd=["win"], wr=[pk])
                            act(gat[:, i, :], o, AF.Sigmoid, rd=[pk], wr=[])
                        p, pk = nextpp()
                        for l in range(32):
                            mm(p[0:64, 0:255], wk[:, l, :], kcr[:, l:l + 4065:16], l == 0, l == 31,
                               rd=["wk", "kcr", "kcr_t"], wr=[pk])
                        p2, pk2 = nextpp()
                        for l in range(32):
                            mm(p2[0:64, 0:1], wk[:, l, :], pk_[:, l:l + 1], l == 0, l == 31, rd=["wk", "pk_"], wr=[pk2])
                        cp("dve", kcb[:], p2[0:64, 0:1], rd=[pk2], wr=["kcb"])
                        ts("dve", kcT[:, 0:255], p[0:64, 0:255], kcb[:, 0:1], None, ALU.add, rd=[pk, "kcb"], wr=[])
                        p3, pk3 = nextpp()
                        for l in range(32):
                            mm(p3[0:1, 0:64], pv_[:, l:l + 1], wv[:, l, :], l == 0, l == 31, rd=["wv", "pv_"], wr=[pk3])
                        cp("dve", cvr[:], p3[0:1, 0:64], rd=[pk3], wr=["cvr"])
                        for it in range(2):
                            p, pk = nextpp()
                            for l in range(32):
                                mm(p[:, 0:64], vcr[:, it * 2048 + l:it * 2048 + l + 2033:16], wv[:, l, :], l == 0, False,
                                   rd=["wv", "vcr", "vcr_t"], wr=[pk])
                            mm(p[:, 0:64], ones_b[0:1, :], cvr[:], False, True, rd=["cvr"], wr=[pk])
                            cp("dve", vca[:, it, 0:64], p[:, 0:64], rd=[pk], wr=[])
                        S.flush()

                    with ExitStack() as st:
                        E = SB(st, "E", [65, NT, 128], BF16)
                        dma("pool", E[:], t_E, wr=["E"])
                        ovl = SB(st, "ovl", [128, 2, 64], BF16)
                        dma("pool", ovl[:], t_ovl, wr=["ovl"])
                        wbt = SB(st, "wbt", [128, 10, 512], BF16)
                        dma("pool", wbt[:], t_wb[g], wr=["wbt"])
                        sbt = SB(st, "sbt", [128, 2, 512], BF16)
                        for dl in range(2):
                            dma("pool", sbt[:, dl, :], t_sb[g, dl], wr=["sbt"])
                        cst = SB(st, "cst", [1, 512], BF16)
                        dma("pool", cst[:], t_cst[g], wr=["cst"])
                        addt = SB(st, "addt", [128, NTO, 64], F32)
                        dma("sp", addt[:], t_add, wr=["addt"])
                        cbt = [SB(st, "cbt%d" % i, [128, 2, 512], BF16) for i in range(2)]
                        Pc = [SB(st, "Pc%d" % i, [128, 2, 512], BF16) for i in range(2)]
                        Pb = [SB(st, "Pb%d" % i, [128, 512], BF16) for i in range(4)]
                        snT = [SB(st, "snT%d" % i, [65, 4, 128], BF16) for i in range(2)]
                        for i_ in range(2):
                            dma("pool", snT[i_][64:65, :, :].rearrange("p h t -> p (h t)"), t_cst[g], wr=["snT%d" % i_])
                        sc = [SB(st, "sc%d" % i, [128, 64], F32) for i in range(2)]
                        sc2 = [SB(st, "sc2%d" % i, [128, 64], F32) for i in range(2)]
                        m8 = [SB(st, "m8%d" % i, [128, 8], F32) for i in range(2)]
                        nm = [SB(st, "nm%d" % i, [128, 64], BF16) for i in range(2)]
                        rc = [SB(st, "rc%d" % i, [128, 3, 4], F32) for i in range(2)]
                        oacc = [SB(st, "oacc%d" % i, [128, 4, 64], F32) for i in range(2)]
                        yab = [SB(st, "yab%d" % i, [128, 256], BF16) for i in range(2)]
                        osb = [SB(st, "osb%d" % i, [128, 260], F32) for i in range(2)]
                        pS = [PS(st, "pS%d" % i, [128, 512], F32) for i in range(4)]
                        pCR = PS(st, "pCR", [128, 512], F32)
                        pOf = [None] + [PS(st, "pO%d" % i, [128, 512], F32) for i in (1, 2)]
                        pO = [pCR[:, 0:256].rearrange("p (h d) -> p h d", h=4)] + \
                             [t_[:, 0:260].rearrange("p (h d) -> p h d", h=4) for t_ in pOf[1:]]
                        pR = pCR[:, 256:512].rearrange("p (h d) -> p h d", h=4)
                        pM = PS(st, "pM", [128, 1024], BF16)
                        nS = [0]

                        def nextS():
                            i = nS[0] % 4
                            nS[0] += 1
                            return pS[i], "pS%d" % i, Pb[i], "Pb%d" % i

                        def attn_tile(i):
                            c = 16 + i
                            b2 = i % 2
                            qslice = qaT[:, :, i * 128:(i + 1) * 128]
                            r_ = rc[b2]
                            rk = "rc%d" % b2
                            sn = snT[b2]
                            snk = "snT%d" % b2
                            v3 = lambda p: p[:].rearrange("p (h t) -> p h t", h=4)
                            gv = gat[:, i, :].rearrange("p (h b) -> p b h", b=3)
                            oa = oacc[b2]
                            ok = "oacc%d" % b2

                            def cmp_S(it):
                                def f():
                                    p, pk, _, _ = nextS()
                                    mm(v3(p), kcT[:, it * 128:(it + 1) * 128], qslice, True, False, wr=[pk])
                                    mm(p[:], identb[:], cbt[b2][:, it, :], False, True, rd=["cbt%d" % b2], wr=[pk])
                                    act(Pc[b2][:, it, :], p[:], AF.Exp, rd=[pk], wr=["Pc%d_%d" % (b2, it)])
                                return f

                            def cmp_PV():
                                for h in range(4):
                                    for it in range(2):
                                        mm(pO[0][:, h, :], Pc[b2][:, it, h * 128:(h + 1) * 128], vca[:, it, 0:64], it == 0, it == 1,
                                           rd=["Pc%d_%d" % (b2, it)], wr=["pO0"])
                                for h in range(4):
                                    for it in range(2):
                                        mm(pR[:, h, :], Pc[b2][:, it, h * 128:(h + 1) * 128], ovl[:, it, :], it == 0, it == 1,
                                           rd=["Pc%d_%d" % (b2, it), "ovl"], wr=["pO0"])
                                S.op("dve", lambda e: e.tensor_reduce(out=r_[:, 0, :], in_=pR, axis=AX.X, op=ALU.add), ["pO0"], [rk])
                                ts("dve", r_[:, 0, :], r_[:, 0, :], 1e-30, None, ALU.max, rd=[rk], wr=[rk])
                                rcp(r_[:, 0, :], r_[:, 0, :], rd=[rk], wr=[rk])
                                s_ = sc[b2]
                                sk = "sc%d" % b2
                                cp("dve", s_[:], addt[:, i, :], rd=["addt"], wr=[sk])
                                for h in range(4):
                                    stt("dve", s_[:], pR[:, h, :], r_[:, 0, h:h + 1], s_[:], ALU.mult, ALU.add, rd=["pO0", rk, sk], wr=[sk])
                                s2_ = sc2[b2]
                                s2k = "sc2%d" % b2
                                m_ = m8[b2]
                                mk = "m8%d" % b2
                                S.op("dve", lambda e: e.max(out=m_[:], in_=s_[:]), [sk], [mk])
                                S.op("dve", lambda e: e.match_replace(out=s2_[:], in_to_replace=m_[:], in_values=s_[:], imm_value=-1e30),
                                     [sk, mk], [s2k])
                                S.op("dve", lambda e: e.max(out=m_[:], in_=s2_[:]), [s2k], [mk])
                                ts("dve", s2_[:], s_[:], m_[:, 7:8], None, ALU.is_ge, rd=[sk, mk, s2k], wr=[s2k])
                                ts("dve", s_[:], s_[:], -1e5, None, ALU.is_ge, rd=[sk, s2k], wr=[sk])
                                tt("dve", s2_[:], s2_[:], s_[:], ALU.mult, rd=[sk, s2k], wr=[s2k])
                                n_ = nm[b2]
                                nk = "nm%d" % b2
                                ts("dve", n_[:], s2_[:], -1.0, -NEG, ALU.add, ALU.mult, rd=[s2k], wr=[nk])
                                tt("dve", r_[:, 0, :], r_[:, 0, :], gv[:, 0, :], ALU.mult, rd=[rk], wr=[rk])
                                for h in range(4):
                                    ts("dve", oa[:, h, :], pO[0][:, h, :], r_[:, 0, h:h + 1], None, ALU.mult, rd=["pO0", rk], wr=[ok])

                            def cmpB():
                                n_ = nm[b2]
                                tr(pM[0:64, 0:128], n_[:], identb[:], rd=["nm%d" % b2], wr=["pM"])
                                cp("dve", sn[0:64, :, :], pM[0:64, 0:128].unsqueeze(1).to_broadcast([64, 4, 128]), rd=["pM"], wr=[snk])

                            cmp_stages = [(cmp_S(0), None), (cmp_S(1), cmp_PV)]
                            win_stages = []
                            sel_stages = []

                            def pair(kT_, j, bias_fn, V_, vkey, acc, acck, first, last, use_sel):
                                st_ = {}

                                def fS():
                                    p, pk, pb, pbk = nextS()
                                    st_["x"] = (pb, pbk)
                                    mm(v3(p), kT_[:, j * 128:(j + 1) * 128], qslice, True, False, wr=[pk])
                                    if use_sel == 2:
                                        mm(p[:], E[:, j, :], sn[:].rearrange("p h t -> p (h t)"), False, True, rd=["E", snk], wr=[pk])
                                    else:
                                        if use_sel == 1:
                                            mm(p[:], E[0:64, j, :], sn[0:64, :, :].rearrange("p h t -> p (h t)"), False, False,
                                               rd=["E", snk], wr=[pk])
                                        bias_fn(p, pk)
                                    act(pb[:], p[:], AF.Exp, rd=[pk], wr=[pbk])

                                def fPV():
                                    pb, pbk = st_["x"]
                                    for h in range(4):
                                        mm(acc[:, h, :], pb[:, h * 128:(h + 1) * 128], V_[:, j, :], (first and h == 0), last,
                                           rd=[pbk, vkey], wr=[acck], skip=True)
                                return fS, fPV

                            for dl in range(4, -1, -1):
                                j = c - dl
                                var = dl if j >= 16 else 5 + dl
                                bf = (lambda var: lambda p, pk: mm(p[:], identb[:], wbt[:, var, :], False, True, rd=["wbt"], wr=[pk]))(var)
                                win_stages.append(pair(kwT, j, bf, vwa, "vwa1", pO[2], "pO2", dl == 4, dl == 0, 0))
                            for j in range(c + 1):
                                dl = c - j
                                if dl <= 1:
                                    bf = (lambda dl: lambda p, pk: mm(p[:], identb[:], sbt[:, dl, :], False, True, rd=["sbt"], wr=[pk]))(dl)
                                else:
                                    bf = lambda p, pk: mm(p[:], ones_b[0:1, :], cst[:], False, True, rd=["cst"], wr=[pk])
                                sel_stages.append(pair(ksT, j, bf, vsa, "vsa1", pO[1], "pO1", j == 0, j == c, 1 if dl <= 1 else 2))
                            def merge():
                                for br in (1, 2):
                                    cp("dve", osb[br - 1][:], pOf[br][:, 0:260], rd=["pO%d" % br], wr=["osb%d" % (br - 1)])
                                for br in (1, 2):
                                    ov = osb[br - 1][:, 0:260].rearrange("p (h d) -> p h d", h=4)
                                    ts("dve", r_[:, br, :], ov[:, :, 64], 1e-30, None, ALU.max, rd=["osb%d" % (br - 1), rk], wr=[rk])
                                    rcp(r_[:, br, :], r_[:, br, :], rd=[rk], wr=[rk])
                                    tt("dve", r_[:, br, :], r_[:, br, :], gv[:, br, :], ALU.mult, rd=[rk], wr=[rk])
                                ov1 = osb[0][:, 0:260].rearrange("p (h d) -> p h d", h=4)
                                ov2 = osb[1][:, 0:260].rearrange("p (h d) -> p h d", h=4)
                                for h in range(4):
                                    stt("dve", oa[:, h, :], ov1[:, h, 0:64], r_[:, 1, h:h + 1], oa[:, h, :], ALU.mult, ALU.add,
                                        rd=["osb0", rk, ok], wr=[ok])
                                    stt("dve", yab[b2][:, h * 64:(h + 1) * 64], ov2[:, h, 0:64], r_[:, 2, h:h + 1], oa[:, h, :],
                                        ALU.mult, ALU.add, rd=["osb1", rk, ok], wr=["yab%d" % b2])

                            def mergeB():
                                for hp in range(2):
                                    tr(pM[:, 128 + hp * 128:256 + hp * 128], yab[b2][:, hp * 128:(hp + 1) * 128], identb[:],
                                       rd=["yab%d" % b2], wr=["pM"])
                                    cp("dve", yT[:, 4 + 2 * g + hp, i * 128:(i + 1) * 128], pM[:, 128 + hp * 128:256 + hp * 128],
                                       rd=["pM"], wr=[])

                            return cmp_stages, win_stages, sel_stages, merge, mergeB, cmpB

                        tiles = []
                        allst = []
                        dma("pool", cbt[0][:], t_cb[g, 0], wr=["cbt0"])
                        tiles.append(attn_tile(0))
                        allst += tiles[0][0]
                        allst.append((None, tiles[0][5]))
                        for i in range(NTO):
                            cs, ws, ss_, mg, mgB, cB = tiles[i]
                            allst += ws
                            if i > 0:
                                allst.append((None, tiles[i - 1][4]))
                            if i + 1 < NTO:
                                allst.append(((lambda i=i: lambda: dma("pool", cbt[(i + 1) % 2][:], t_cb[g, i + 1],
                                                                       wr=["cbt%d" % ((i + 1) % 2)]))(), None))
                                tiles.append(attn_tile(i + 1))
                                allst += tiles[i + 1][0]
                            allst += ss_
                            allst.append((None, mg))
                            if i + 1 < NTO:
                                allst.append((None, tiles[i + 1][5]))
                        allst.append((None, tiles[NTO - 1][4]))
                        pend = []
                        for (fS, fPV) in allst:
                            if fS is not None:
                                fS()
                            if len(pend) >= 2:
                                f_ = pend.pop(0)
                                if f_ is not None:
                                    f_()
                            pend.append(fPV)
                        for f_ in pend:
                            if f_ is not None:
                                f_()
                        S.flush()
            if stage == 3:
                with ExitStack() as st:
                    tmp = SB(st, "dbgt", [128, 8, TO], F32)
                    for kc in range(8):
                        cp("dve", tmp[:, kc, :], yT[:, kc, :], wr=["t%d" % kc])
                        dma("sp", dbg[:, kc * TO:(kc + 1) * TO], tmp[:, kc, :], rd=["t%d" % kc])
                    S.flush()

            if stage >= 4:
              with ExitStack() as st:
                wo = SB(st, "wo", [128, 8, D], BF16)
                w_out_v = w_out.rearrange("(k p) f -> p k f", p=128)
                for kc in range(8):
                    dma("pool", wo[:, kc, :], w_out_v[:, kc, :], wr=["wo"])
                xo = [SB(st, "xo%d" % i, [128, D], F32) for i in range(2)]
                x1 = [SB(st, "x1%d" % i, [128, D], F32) for i in range(2)]
                ssb = [SB(st, "ssb%d" % i, [128, 4], F32) for i in range(2)]
                s5 = [SB(st, "s5%d" % i, [128, 4], F32) for i in range(2)]
                pT = [PS(st, "pT%d" % i, [128, D], BF16) for i in range(2)]
                pY = [PS(st, "pYe%d" % i, [128, D], F32) for i in range(2)]
                for i in range(NTO):
                    b2 = i % 2
                    dma("sp", xo[b2][:], xw_t[16 + i], wr=["xo%d" % b2])
                    py = pY[b2]
                    pyk = "pYe%d" % b2
                    for hb in range(2):
                        for kc in range(8):
                            mm(py[:, hb * 512:(hb + 1) * 512], yT[:, kc, i * 128:(i + 1) * 128], wo[:, kc, hb * 512:(hb + 1) * 512],
                               kc == 0, kc == 7, rd=["wo"], wr=[pyk])
                    s_ = s5[b2]
                    sk = "s5%d" % b2
                    S.op("act", (lambda py=py: lambda e: e.activation(out=junkf[:, 0:512], in_=py[:, 0:512], func=AF.Square))(), [pyk], ["junkf"])
                    S.op("act", (lambda py=py: lambda e: e.activation(out=junkf[:, 512:1024], in_=py[:, 512:1024], func=AF.Square))(), [pyk], ["junkf"])
                    S.op("dve", (lambda s_=s_: lambda e: e.tensor_reduce(out=s_[:, 2:3], in_=junkf[:], axis=AX.X, op=ALU.add))(), ["junkf", sk], [sk])
                    act(s_[:, 2:3], s_[:, 2:3], AF.Sqrt, rd=[sk], wr=[sk], scale=1.0 / D, bias=epsc[:, 0:1])
                    rcp(s_[:, 3:4], s_[:, 2:3], rd=[sk], wr=[sk])
                    xk = "x1%d" % b2
                    for hb in range(2):
                        stt("dve", x1[b2][:, hb * 512:(hb + 1) * 512], py[:, hb * 512:(hb + 1) * 512], s_[:, 3:4],
                            GT1[:, hb * 512:(hb + 1) * 512], ALU.mult, ALU.mult, rd=[pyk, sk], wr=[xk])
                    tt("pool", x1[b2][:], x1[b2][:], xo[b2][:], ALU.add, rd=[xk, "xo%d" % b2], wr=[xk])
                    import os
                    _de = int(os.environ.get("DBG_E", 0))
                    if _de != 1:
                        dma("sp", out_t[i], x1[b2][:], rd=[xk])
                    if _de != 2:
                        norm_transpose(x1[b2][:], xk, h2T, i * 128, A2, B2, xn[b2], pT[b2], ssb[b2], i, "e")
                S.flush()
        if stage == 4:
            with ExitStack() as st:
                tmp = SB(st, "dbgt", [128, 8, TO], F32)
                for kc in range(8):
                    cp("dve", tmp[:, kc, :], h2T[:, kc, 0:TO], wr=["t%d" % kc])
                    dma("sp", dbg[:, kc * TO:(kc + 1) * TO], tmp[:, kc, :], rd=["t%d" % kc])
                S.flush()

        if stage >= 5:
          with ExitStack() as st:
            yacc = SB(st, "yacc", [128, NTO, D], F32)
            gate = SB(st, "gate", [128, NTO, NE], F32)
            gateT = SB(st, "gateT", [NE, TO], BF16)
            bup = SB(st, "bup", [128, NE, 16], F32)
            dma("sp", bup[:], b_up_col, wr=["bup"])
            with ExitStack() as rs:
                wr_ = SB(rs, "wr_", [128, 8, NE], BF16)
                dma("pool", wr_[:], w_router.rearrange("(k p) e -> p k e", p=128), wr=["wr_"])
                brt = SB(rs, "brt", [128, NE], F32)
                dma("sp", brt[:], b_router.partition_broadcast(128), wr=["brt"])
                bdn = SB(rs, "bdn", [NE, D], BF16)
                dma("pool", bdn[:], b_down, wr=["bdn"])
                lg = [SB(rs, "lg%d" % i, [128, NE], F32) for i in range(2)]
                mk4 = [SB(rs, "mk4%d" % i, [128, NE], F32) for i in range(2)]
                m8 = [SB(rs, "m8r%d" % i, [128, 8], F32) for i in range(2)]
                s4 = [SB(rs, "s4r%d" % i, [128, 4], F32) for i in range(2)]
                pL = PS(rs, "pL", [128, NTO, NE], F32)
                pGf = [PS(rs, "pG%d" % i, [128, 512], F32) for i in range(2)]
                pG = [t_[0:NE, 0:128] for t_ in pGf]
                pB = [PS(rs, "pB%d" % i, [128, 512], F32) for i in range(2)]
                for i in range(NTO):
                    b2 = i % 2
                    for kc in range(8):
                        mm(pL[:, i, :], h2T[:, kc, i * 128:(i + 1) * 128], wr_[:, kc, :], kc == 0, kc == 7, rd=["wr_"], wr=["pL"])
                    l_ = lg[b2]
                    lk = "lg%d" % b2
                    tt("dve", l_[:], pL[:, i, :], brt[:], ALU.add, rd=["pL", "brt"], wr=[lk])
                    m_ = m8[b2]
                    mk = "m8r%d" % b2
                    S.op("dve", (lambda m_=m_, l_=l_: lambda e: e.max(out=m_[:], in_=l_[:]))(), [lk], [mk])
                    k4 = mk4[b2]
                    k4k = "mk4%d" % b2
                    ts("dve", k4[:], l_[:], m_[:, 3:4], None, ALU.is_ge, rd=[lk, mk], wr=[k4k])
                    s_ = s4[b2]
                    sk = "s4r%d" % b2
                    ts("dve", s_[:, 0:1], m_[:, 0:1], -1.0, None, ALU.mult, rd=[mk], wr=[sk])
                    act(l_[:], l_[:], AF.Exp, rd=[lk, sk, k4k], wr=[lk], bias=s_[:, 0:1])
                    tt("dve", l_[:], l_[:], k4[:], ALU.mult, rd=[lk, k4k], wr=[lk])
                    S.op("dve", (lambda s_=s_, l_=l_: lambda e: e.tensor_reduce(out=s_[:, 1:2], in_=l_[:], axis=AX.X, op=ALU.add))(),
                         [lk, sk], [sk])
                    rcp(s_[:, 2:3], s_[:, 1:2], rd=[sk], wr=[sk])
                    ts("dve", gate[:, i, :], l_[:], s_[:, 2:3], None, ALU.mult, rd=[lk, sk], wr=["gate%d" % i])
                    tr(pG[b2], gate[:, i, :], identf[:], rd=["gate%d" % i], wr=["pG%d" % b2])
                    cp("dve", gateT[:, i * 128:(i + 1) * 128], pG[b2], rd=["pG%d" % b2], wr=["gateT%d" % i])
                    for db in range(2):
                        mm(pB[db][:], gateT[:, i * 128:(i + 1) * 128], bdn[:, db * 512:(db + 1) * 512], True, True,
                           rd=["gateT%d" % i, "bdn"], wr=["pB%d" % db])
                        act(yacc[:, i, db * 512:(db + 1) * 512], pB[db][:], AF.Copy, rd=["pB%d" % db], wr=[])
                S.flush()

            with ExitStack() as es:
                wup = [hT[:, :, TO + i * 1024:TO + (i + 1) * 1024] for i in range(2)]
                wdn = [SB(es, "wdn%d" % i, [128, 4, D], BF16) for i in range(2)]
                gg = [SB(es, "gg%d" % i, [128, 512], F32) for i in range(2)]
                sg = [SB(es, "sg%d" % i, [128, 512], F32) for i in range(2)]
                ll = [SB(es, "ll%d" % i, [128, 512], F32) for i in range(2)]
                actT = [SB(es, "actT%d" % i, [128, 4, 512], BF16) for i in range(2)]
                pU = [PS(es, "pU%d" % i, [128, 512], F32) for i in range(4)]
                pD = [PS(es, "pD%d" % i, [128, 512], F32) for i in range(4)]
                nU = 0
                nD = 0
                nA = 0
                w_up_v = w_up.rearrange("e (k p) f -> e p k f", p=128)
                w_dn_v = w_down.rearrange("e (c p) d -> e p c d", p=128)
                units = [(e_, u) for e_ in range(NE) for u in range(2)]

                def load_unit(n):
                    e_, u = units[n]
                    wb = n % 2
                    for kc in range(8):
                        dma("pool", wup[wb][:, kc, 0:512], w_up_v[e_, :, kc, 512 * u:512 * u + 512], wr=["wup%d" % wb])
                        dma("pool", wup[wb][:, kc, 512:1024], w_up_v[e_, :, kc, 1024 + 512 * u:1024 + 512 * u + 512], wr=["wup%d" % wb])
                    for fc in range(4):
                        dma("pool", wdn[wb][:, fc, :], w_dn_v[e_, :, 4 * u + fc, :], wr=["wdn%d" % wb])

                load_unit(0)
                for n, (e_, u) in enumerate(units):
                    if n + 1 < len(units):
                        load_unit(n + 1)
                    wb = n % 2
                    wuk = "wup%d" % wb
                    wdk = "wdn%d" % wb
                    for tb in range(4):
                        ab = nA % 2
                        nA += 1
                        ak = "actT%d" % ab
                        for fc in range(4):
                            pg = pU[nU % 4]
                            pgk = "pU%d" % (nU % 4)
                            nU += 1
                            pl = pU[nU % 4]
                            plk = "pU%d" % (nU % 4)
                            nU += 1
                            for kc in range(8):
                                mm(pg[:], wup[wb][:, kc, fc * 128:(fc + 1) * 128], h2T[:, kc, tb * 512:(tb + 1) * 512],
                                   kc == 0, kc == 7, rd=[wuk], wr=[pgk])
                            for kc in range(8):
                                mm(pl[:], wup[wb][:, kc, 512 + fc * 128:512 + (fc + 1) * 128], h2T[:, kc, tb * 512:(tb + 1) * 512],
                                   kc == 0, kc == 7, rd=[wuk], wr=[plk])
                            b2 = fc % 2
                            jg = 4 * u + fc
                            ts("dve", gg[b2][:], pg[:], bup[:, e_, jg:jg + 1], 7.0, ALU.add, ALU.min, rd=[pgk], wr=["gg%d" % b2])
                            act(sg[b2][:], gg[b2][:], AF.Sigmoid, rd=["gg%d" % b2], wr=["sg%d" % b2], scale=1.702)
                            ts("dve", ll[b2][:], pl[:], bup[:, e_, 8 + jg:8 + jg + 1], 7.0, ALU.add, ALU.min, rd=[plk], wr=["ll%d" % b2])
                            ts("dve", ll[b2][:], ll[b2][:], -7.0, 1.0, ALU.max, ALU.add, rd=["ll%d" % b2], wr=["ll%d" % b2])
                            tt("dve", gg[b2][:], gg[b2][:], sg[b2][:], ALU.mult, rd=["gg%d" % b2, "sg%d" % b2], wr=["gg%d" % b2])
                            tt("dve", actT[ab][:, fc, :], gg[b2][:], ll[b2][:], ALU.mult, rd=["gg%d" % b2, "ll%d" % b2],
                               wr=[ak + "_%d" % fc])
                        for tt_ in range(4):
                            ti = tb * 4 + tt_
                            for db in range(2):
                                pd = pD[nD % 4]
                                pdk = "pD%d" % (nD % 4)
                                nD += 1
                                for fc in range(4):
                                    mm(pd[:], actT[ab][:, fc, tt_ * 128:(tt_ + 1) * 128], wdn[wb][:, fc, db * 512:(db + 1) * 512],
                                       fc == 0, fc == 3, rd=[ak + "_%d" % fc, wdk], wr=[pdk])
                                ya = yacc[:, ti, db * 512:(db + 1) * 512]
                                stt("dve", ya, pd[:], gate[:, ti, e_:e_ + 1], ya, ALU.mult, ALU.add, rd=[pdk, "ya%d_%d" % (ti, db)],
                                    wr=["ya%d_%d" % (ti, db)])
                S.flush()

            with ExitStack() as fs:
                xr = [SB(fs, "xr%d" % i, [128, D], F32) for i in range(2)]
                ob = [SB(fs, "ob%d" % i, [128, D], F32) for i in range(2)]
                s5 = [SB(fs, "s6%d" % i, [128, 4], F32) for i in range(2)]
                for i in range(NTO):
                    b2 = i % 2
                    dma("sp", xr[b2][:], out_t[i], wr=["xr%d" % b2])
                    s_ = s5[b2]
                    sk = "s6%d" % b2
                    sumsq(yacc[:, i, :], [], s_[:, 0:1], sk)
                    act(s_[:, 1:2], s_[:, 0:1], AF.Sqrt, rd=[sk], wr=[sk], scale=1.0 / D, bias=epsc[:, 0:1])
                    rcp(s_[:, 2:3], s_[:, 1:2], rd=[sk], wr=[sk])
                    stt("dve", ob[b2][:], yacc[:, i, :], s_[:, 2:3], GT2[:], ALU.mult, ALU.mult, rd=[sk], wr=["ob%d" % b2])
                    tt("pool", ob[b2][:], ob[b2][:], xr[b2][:], ALU.add, rd=["ob%d" % b2, "xr%d" % b2], wr=["ob%d" % b2])
                    dma("sp", out_t[i], ob[b2][:], rd=["ob%d" % b2])
                S.flush()
    return nc


_TABLE_CACHE = {}


def _tables(rel_bias, s):
    rb = np.asarray(rel_bias, np.float32)
    lo = 0 if s == 1 else 2048
    jl = np.arange(128)[:, None]
    tl = np.arange(128)[None, :]
    t_sb = np.empty((2, 2, 128, 4, 128), np.float32)
    t_wb = np.empty((2, 128, 10, 4, 128), np.float32)
    t_cst = np.empty((2, 1, 4, 128), np.float32)
    for g in range(2):
        for h in range(4):
            col = rb[:, 4 * g + h]
            t_cst[g, 0, h, :] = col[31]
            for dl in range(5):
                dist = dl * 128 + tl - jl
                bias = col[_t5_bucket(dist)]
                if dl < 2:
                    t_sb[g, dl, :, h, :] = np.where(dist >= 0, bias, np.float32(NEG))
                wv = np.where((dist >= 0) & (dist < 512), bias, np.float32(NEG))
                t_wb[g, :, dl, h, :] = wv
                t_wb[g, :, 5 + dl, h, :] = wv if s == 1 else np.float32(NEG)
    t_cb = np.empty((2, 16, 128, 2, 4, 128), np.float32)
    m = (np.arange(2)[None, :] * 128 + np.arange(128)[:, None])
    end = 16 * m + 31
    for i in range(16):
        tw = 2048 + 128 * i + np.arange(128)
        dist = tw[None, None, :] - end[:, :, None]
        valid = (16 * m[:, :, None] >= lo) & (dist >= 0) & (m[:, :, None] <= 254)
        bk = _t5_bucket(dist)
        for g in range(2):
            for h in range(4):
                t_cb[g, i, :, :, h, :] = np.where(valid, rb[:, 4 * g + h][bk], np.float32(NEG))
    t_add = np.empty((128, 16, 64), np.float32)
    blk = np.arange(64)[None, :]
    for i in range(16):
        tw = (2048 + 128 * i + np.arange(128))[:, None]
        cur = tw // 64
        first = lo // 64
        valid = (64 * blk >= lo) & (64 * blk <= tw)
        forced = (blk == first) | (blk == cur) | (blk == cur - 1)
        t_add[:, i, :] = np.where(valid, np.where(forced, np.float32(1000.0), np.float32(0.0)), np.float32(-1e6))
    return dict(t_sb=t_sb.reshape(2, 2, 128, 512), t_cst=t_cst.reshape(2, 1, 512), t_wb=t_wb.reshape(2, 128, 10, 512),
                t_cb=t_cb.reshape(2, 16, 128, 2, 512), t_add=t_add)


def _consts():
    E = np.zeros((65, 32, 128), np.float32)
    for j in range(32):
        for k in range(128):
            E[2 * j + k // 64, j, k] = 1.0
    E[64] = 1.0
    ovl = np.zeros((256, 64), np.float32)
    for m in range(255):
        for p in range(16 * m, 16 * m + 32):
            ovl[m, p // 64] += 1.0 / 32
    ovl = ovl.reshape(2, 128, 64).transpose(1, 0, 2).copy()
    tri = (np.arange(128)[:, None] <= np.arange(128)[None, :]).astype(np.float32)
    return dict(t_E=E, t_ovl=ovl, t_tri=tri)


_NC_CACHE = {}


def make_in_maps(inputs):
    f = lambda a: np.ascontiguousarray(np.asarray(a, np.float32))
    x = f(inputs["x"])
    c = f(inputs["c"])
    col = lambda v, n: f(np.asarray(v, np.float32).reshape(n, 128).T)
    perm = _perm()
    shared = dict(
        w_ada=f(inputs["w_ada"][0]),
        b_ada_col=col(inputs["b_ada"][0], 48),
        b_ada_row=f(inputs["b_ada"][0][None, :]),
        gpm_col=col(inputs["g_pre_mix"][0], 8),
        gpf_col=col(inputs["g_pre_ffn"][0], 8),
        gpostm=f(inputs["g_post_mix"][0][None, :]),
        gpostf=f(inputs["g_post_ffn"][0][None, :]),
        w_in=f(np.asarray(inputs["w_in"][0])[:, perm]),
        b_gates=f(inputs["b_gates"][0][None, :]),
        conv_col=f(np.asarray(inputs["conv_qk"][0]).reshape(4, 8, 128).transpose(2, 1, 0)),
        wck=f(np.asarray(inputs["w_cmp_k"][0]).transpose(1, 0, 2)),
        wcv=f(np.asarray(inputs["w_cmp_v"][0]).transpose(1, 0, 2)),
        posk=f(np.asarray(inputs["pos_cmp_k"][0]).T),
        posv=f(np.asarray(inputs["pos_cmp_v"][0]).T),
        w_out=f(inputs["w_out"][0]),
        w_router=f(inputs["w_router"][0]),
        b_router=f(inputs["b_router"][0][None, :]),
        w_up=f(inputs["w_up"][0]),
        b_up_col=f(np.asarray(inputs["b_up"][0]).reshape(NE, 16, 128).transpose(2, 0, 1)),
        w_down=f(inputs["w_down"][0]),
        b_down=f(inputs["b_down"][0]),
    )
    shared.update(_consts())
    tabs = [_tables(inputs["rel_bias"], s) for s in range(2)]
    in_maps = []
    for core in range(8):
        b, s = core // 2, core % 2
        m = dict(shared)
        m.update(tabs[s])
        if s == 1:
            m["xw"] = x[b]
        else:
            m["xw"] = np.ascontiguousarray(np.concatenate([x[b, TO:], x[b, :TO]], axis=0))
        m["c_col"] = col(c[b], 8)
        m["flag"] = np.full((128, 1), float(s), np.float32)
        in_maps.append(m)
    return in_maps


def kernel(**inputs):
    if "nc" not in _NC_CACHE:
        _NC_CACHE["nc"] = build_nc()
    nc = _NC_CACHE["nc"]
    in_maps = make_in_maps(inputs)
    res = run_bass_kernel_spmd(nc, in_maps, core_ids=list(range(8)))
    out = np.empty((4, 4096, D), np.float32)
    for core in range(8):
        b, s = core // 2, core % 2
        out[b, s * TO:(s + 1) * TO] = res.results[core]["out"]
    return out
```

```python
import numpy as np
from contextlib import ExitStack
import concourse.bass as bass
import concourse.mybir as mybir
from concourse.bass_utils import run_bass_kernel_spmd

F32 = mybir.dt.float32
BF16 = mybir.dt.bfloat16
AF = mybir.ActivationFunctionType
ALU = mybir.AluOpType
AX = mybir.AxisListType

COMPUTE = ("pe", "act", "dve", "pool")
NDMASEM = 24
NEG = -30000.0


class _Op:
    __slots__ = ("id", "eng", "fn", "deps", "dma", "needs_inc", "inc_idx", "dsem", "dval", "dprev")


class Sched:
    def __init__(self, nc, stack, same_engine_sync=True):
        self.nc = nc
        self.same = same_engine_sync
        self.sem = {e: stack.enter_context(nc.semaphore("sem_" + e)) for e in COMPUTE}
        self.dsem = [stack.enter_context(nc.semaphore("dsem%d" % i)) for i in range(NDMASEM)]
        self.bar = stack.enter_context(nc.semaphore("sem_bar"))
        self.cnt = {e: 0 for e in COMPUTE}
        self.ndma = 0
        self.dfinal = {}
        self.phase = 0
        self.scr = stack.enter_context(nc.sbuf_tensor("sched_scr", [128, 8], F32))
        self._reset()

    def _reset(self):
        self.ops = []
        self.lastw = {}
        self.readers = {}
        self.dma_hist = {}

    def _add(self, eng, fn, reads, writes, dma):
        deps = {}

        def add_dep(p):
            o = self.ops[p]
            if o.dma:
                deps[("d", p)] = p
            else:
                k = o.eng
                if k not in deps or deps[k] < p:
                    deps[k] = p

        for k in reads:
            for p in self.lastw.get(k, ()):
                add_dep(p)
        for k in writes:
            lw = self.lastw.get(k, ())
            if not (dma and lw and all(self.ops[p].dma for p in lw) and not self.readers.get(k)):
                for p in lw:
                    add_dep(p)
            for r in self.readers.get(k, ()):
                add_dep(r)
        op = _Op()
        op.id = len(self.ops)
        op.eng = eng
        op.fn = fn
        op.deps = sorted(set(deps.values()))
        op.dma = dma
        op.needs_inc = False
        op.inc_idx = 0
        op.dsem = None
        op.dval = 0
        op.dprev = None
        if dma:
            op.dsem = self.ndma % NDMASEM
            op.dval = 16 * (self.ndma // NDMASEM + 1)
            op.dprev = self.dma_hist.get(op.dsem)
            self.dma_hist[op.dsem] = op.id
            self.dfinal[op.dsem] = op.dval
            self.ndma += 1
        self.ops.append(op)
        for k in writes:
            lw = self.lastw.get(k, ())
            if dma and lw and all(self.ops[p].dma for p in lw) and not self.readers.get(k):
                self.lastw[k] = list(lw) + [op.id]
            else:
                self.lastw[k] = [op.id]
            self.readers[k] = []
        for k in reads:
            self.readers.setdefault(k, []).append(op.id)
        return op.id

    def op(self, eng, fn, reads=(), writes=()):
        return self._add(eng, fn, tuple(reads), tuple(writes), False)

    def dma(self, eng, fn, reads=(), writes=()):
        return self._add(eng, fn, tuple(reads), tuple(writes), True)

    def flush(self):
        nc = self.nc
        ops = self.ops
        for o in ops:
            for p in o.deps:
                po = ops[p]
                if po.dma:
                    continue
                if po.eng == o.eng and (po.eng == "pe" or not self.same):
                    continue
                po.needs_inc = True
        for o in ops:
            if not o.dma and o.needs_inc:
                self.cnt[o.eng] += 1
                o.inc_idx = self.cnt[o.eng]
        per = {e: [] for e in ("pe", "act", "dve", "pool", "sp")}
        for o in ops:
            per[o.eng].append(o)
        self.phase += 1
        phase = self.phase
        dfinal = dict(self.dfinal)
        sem, dsem, bar, scr = self.sem, self.dsem, self.bar, self.scr
        same = self.same

        def run(ename, e):
            waited_e = {f: 0 for f in COMPUTE}
            waited_d = {}
            for o in per[ename]:
                for p in o.deps:
                    po = ops[p]
                    if po.dma:
                        if waited_d.get(po.dsem, 0) < po.dval:
                            e.wait_ge(dsem[po.dsem], po.dval)
                            waited_d[po.dsem] = po.dval
                    else:
                        if not po.needs_inc:
                            continue
                        if po.eng == ename and (ename == "pe" or not same):
                            continue
                        if waited_e[po.eng] < po.inc_idx:
                            e.wait_ge(sem[po.eng], po.inc_idx)
                            waited_e[po.eng] = po.inc_idx
                if o.dma and o.dprev is not None:
                    po = ops[o.dprev]
                    if waited_d.get(po.dsem, 0) < po.dval:
                        e.wait_ge(dsem[po.dsem], po.dval)
                        waited_d[po.dsem] = po.dval
                inst = o.fn(e)
                if o.dma:
                    inst.then_inc(dsem[o.dsem], 16)
                elif o.needs_inc:
                    inst.then_inc(sem[o.eng], 1)
            if ename == "act":
                e.memzero(scr[0:1, 0:1]).then_inc(bar, 1)
            elif ename == "dve":
                e.memset(scr[0:1, 1:2], 0.0).then_inc(bar, 1)
            elif ename == "pool":
                e.memset(scr[0:1, 2:3], 0.0).then_inc(bar, 1)
            e.wait_ge(bar, 3 * phase)
            for s, v in dfinal.items():
                e.wait_ge(dsem[s], v)

        with nc.Block() as block:
            @block.tensor
            def _(e):
                run("pe", e)

            @block.scalar
            def _(e):
                run("act", e)

            @block.vector
            def _(e):
                run("dve", e)

            @block.gpsimd
            def _(e):
                run("pool", e)

            @block.sync
            def _(e):
                run("sp", e)
        self._reset()


D = 1024
T = 4096
TO = 2048
NT = 32
NTO = 16
D_IN = 3360
NE = 32
def KM(h): return 512 + 128 * h
def QM(h): return 128 * h
def VM(h): return 1024 + 128 * h
def OM(h): return 1536 + 128 * h
GIF = 2048
def QA(h8): return 2056 + 64 * h8
def KC(g): return 2568 + 64 * g
def VC(g): return 2696 + 64 * g
def KS(g): return 2824 + 64 * g
def KW(g): return 2952 + 64 * g
def VSW(g): return 3080 + 128 * g
GA = 3336


def _perm():
    p = list(range(D_IN))
    new = list(range(2568, 2952))
    new += list(range(3080, 3208))
    for g in range(2):
        new += list(range(2952 + 64 * g, 2952 + 64 * g + 64))
        new += list(range(3208 + 64 * g, 3208 + 64 * g + 64))
    p[2568:3336] = new
    return np.array(p)


def _t5_bucket(dist):
    n = np.maximum(dist, 0)
    nf = np.maximum(n, 1).astype(np.float32)
    large = 16 + (np.log(nf / np.float32(16)) / np.float32(np.log(128 / 16)) * np.float32(16)).astype(np.int32)
    large = np.minimum(large, 31)
    return np.where(n < 16, n, large)


def build_nc(stage=99, same=True):
    nc = bass.Bass("TRN2", target_bir_lowering=False)
    din = lambda name, shape: nc.dram_tensor(name, list(shape), F32, kind="ExternalInput").ap()
    xw = din("xw", [T, D])
    c_col = din("c_col", [128, 8])
    w_ada = din("w_ada", [D, 6 * D])
    b_ada_col = din("b_ada_col", [128, 48])
    b_ada_row = din("b_ada_row", [1, 6 * D])
    gpm_col = din("gpm_col", [128, 8])
    gpf_col = din("gpf_col", [128, 8])
    gpostm = din("gpostm", [1, D])
    gpostf = din("gpostf", [1, D])
    w_in = din("w_in", [D, D_IN])
    b_gates = din("b_gates", [1, 8])
    conv_col = din("conv_col", [128, 8, 4])
    wck = din("wck", [64, 32, 64])
    wcv = din("wcv", [64, 32, 64])
    posk = din("posk", [64, 32])
    posv = din("posv", [64, 32])
    w_out = din("w_out", [D, D])
    w_router = din("w_router", [D, NE])
    b_router = din("b_router", [1, NE])
    w_up = din("w_up", [NE, D, 2 * D])
    b_up_col = din("b_up_col", [128, NE, 16])
    w_down = din("w_down", [NE, D, D])
    b_down = din("b_down", [NE, D])
    flag = din("flag", [128, 1])
    t_sb = din("t_sb", [2, 2, 128, 512])
    t_cst = din("t_cst", [2, 1, 512])
    t_wb = din("t_wb", [2, 128, 10, 512])
    t_cb = din("t_cb", [2, 16, 128, 2, 512])
    t_add = din("t_add", [128, 16, 64])
    t_E = din("t_E", [65, 32, 128])
    t_ovl = din("t_ovl", [128, 2, 64])
    t_tri = din("t_tri", [128, 128])
    out = nc.dram_tensor("out", [TO, D], F32, kind="ExternalOutput").ap()
    dbg = None
    if stage < 99:
        dbg = nc.dram_tensor("dbg", [128, 8 * TO], F32, kind="ExternalOutput").ap()

    xw_t = xw.rearrange("(n p) d -> n p d", p=128)
    out_t = out.rearrange("(n p) d -> n p d", p=128)

    with ExitStack() as gs:
        S = Sched(nc, gs, same_engine_sync=same)
        _uid = [0]

        def _nm(name):
            _uid[0] += 1
            return "%s_u%d" % (name, _uid[0])

        SB = lambda st, name, shape, dt: st.enter_context(nc.sbuf_tensor(_nm(name), list(shape), dt))
        PS = lambda st, name, shape, dt: st.enter_context(nc.psum_tensor(_nm(name), list(shape), dt))

        def mm(o, l, r, start, stop, rd=(), wr=(), skip=False):
            if skip:
                S.op("pe", lambda e: e.matmul(o, lhsT=l, rhs=r, start=start, stop=stop, skip_group_check=True), rd, wr)
            else:
                S.op("pe", lambda e: e.matmul(o, lhsT=l, rhs=r, start=start, stop=stop), rd, wr)

        def tr(o, i, idn, rd=(), wr=()):
            S.op("pe", lambda e: e.transpose(o, i, idn), rd, wr)

        def act(o, i, func, rd=(), wr=(), **kw):
            S.op("act", lambda e: e.activation(out=o, in_=i, func=func, **kw), rd, wr)

        def ts(eng, o, i, s1, s2, op0, op1=None, rd=(), wr=()):
            if op1 is None:
                S.op(eng, lambda e: e.tensor_scalar(out=o, in0=i, scalar1=s1, scalar2=None, op0=op0), rd, wr)
            else:
                S.op(eng, lambda e: e.tensor_scalar(out=o, in0=i, scalar1=s1, scalar2=s2, op0=op0, op1=op1), rd, wr)

        def tt(eng, o, a, b, op, rd=(), wr=()):
            S.op(eng, lambda e: e.tensor_tensor(out=o, in0=a, in1=b, op=op), rd, wr)

        def stt(eng, o, a, sc, b, op0, op1, rd=(), wr=()):
            S.op(eng, lambda e: e.scalar_tensor_tensor(out=o, in0=a, scalar=sc, in1=b, op0=op0, op1=op1), rd, wr)

        def cp(eng, o, i, rd=(), wr=()):
            S.op(eng, lambda e: e.tensor_copy(out=o, in_=i), rd, wr)

        def ms(eng, o, v, rd=(), wr=()):
            S.op(eng, lambda e: e.memset(o, v), rd, wr)

        def dma(q, o, i, rd=(), wr=()):
            S.dma(q, lambda e: e.dma_start(out=o, in_=i), rd, wr)

        def rcp(o, i, rd=(), wr=()):
            S.op("dve", lambda e: e.reciprocal(out=o, in_=i), rd, wr)

        identb = SB(gs, "identb", [128, 128], BF16)
        identf = SB(gs, "identf", [128, 128], F32)
        tri_f = SB(gs, "tri_f", [128, 128], F32)
        tri_b = SB(gs, "tri_b", [128, 128], BF16)
        ones_f = SB(gs, "ones_f", [128, 128], F32)
        ones_b = SB(gs, "ones_b", [128, 128], BF16)
        A1 = SB(gs, "A1", [128, 8], F32)
        B1 = SB(gs, "B1", [128, 8], F32)
        A2 = SB(gs, "A2", [128, 8], F32)
        B2 = SB(gs, "B2", [128, 8], F32)
        GT1 = SB(gs, "GT1", [128, D], F32)
        GT2 = SB(gs, "GT2", [128, D], F32)
        flg = SB(gs, "flg", [128, 1], F32)
        junk = SB(gs, "junk", [128, D], BF16)
        epsc = SB(gs, "epsc", [128, 1], F32)
        xn = [SB(gs, "xng%d" % i, [128, D], BF16) for i in range(2)]
        junkf = SB(gs, "junkf", [128, D], F32)

        def sumsq(src, srckeys, dst, dkey):
            S.op("act", lambda e: e.activation(out=junkf[:], in_=src, func=AF.Square), list(srckeys), ["junkf"])
            S.op("dve", lambda e: e.tensor_reduce(out=dst, in_=junkf[:], axis=AX.X, op=ALU.add), ["junkf", dkey], [dkey])
        hT = SB(gs, "hT", [128, 8, T], BF16)
        h2T = hT

        with ExitStack() as st:
            wada = [SB(st, "wada%d" % i, [128, 8, 1536], BF16) for i in range(2)]
            ccol = SB(st, "ccol", [128, 8], F32)
            scb = SB(st, "scb", [128, 8], BF16)
            scB = SB(st, "scB", [128, 8, 128], BF16)
            bcol = SB(st, "bcol", [128, 48], F32)
            modc = SB(st, "modc", [128, 48], F32)
            gpm = SB(st, "gpm", [128, 8], F32)
            gpf = SB(st, "gpf", [128, 8], F32)
            brow = SB(st, "brow", [128, 2, D], F32)
            grow = SB(st, "grow", [128, 2, D], F32)
            psmod = PS(st, "psmod", [128, 48], F32)
            psg = [PS(st, "psg%d" % i, [128, 512], F32) for i in range(4)]

            ms("pool", identf[:], 1.0, rd=[], wr=["tri_tmp"])
            S.op("pool", lambda e: e.affine_select(out=identf[:], in_=identf[:], pattern=[[-1, 128]],
                                                   compare_op=ALU.is_equal, fill=0.0, base=0, channel_multiplier=1),
                 ["tri_tmp"], ["tri_tmp"])
            cp("dve", identb[:], identf[:], rd=["tri_tmp"], wr=["identb"])
            dma("sp", tri_f[:], t_tri, wr=["tri_f"])
            cp("dve", tri_b[:], tri_f[:], rd=["tri_f"], wr=["tri_b"])
            ms("pool", epsc[:], 1e-6, wr=["epsc"])
            ms("pool", ones_f[:], 1.0, wr=["ones_f"])
            ms("pool", ones_b[:], 1.0, wr=["ones_b"])
            dma("sp", flg[:], flag, wr=["flg"])
            dma("sp", ccol[:], c_col, wr=["ccol"])
            dma("sp", bcol[:], b_ada_col, wr=["bcol"])
            dma("sp", gpm[:], gpm_col, wr=["gpm"])
            dma("sp", gpf[:], gpf_col, wr=["gpf"])
            dma("sp", brow[:, 0, :], b_ada_row[:, 2048:3072].partition_broadcast(128), wr=["brow0"])
            dma("sp", brow[:, 1, :], b_ada_row[:, 5120:6144].partition_broadcast(128), wr=["brow1"])
            dma("sp", grow[:, 0, :], gpostm.partition_broadcast(128), wr=["grow0"])
            dma("sp", grow[:, 1, :], gpostf.partition_broadcast(128), wr=["grow1"])
            act(scb[:], ccol[:], AF.Silu, rd=["ccol"], wr=["scb"])
            for kc in range(8):
                cp("dve", scB[:, kc, :], scb[:, kc:kc + 1].to_broadcast([128, 128]), rd=["scb"], wr=["scB"])
            wada_v = w_ada.rearrange("(k p) f -> p k f", p=128)
            for pc in range(4):
                wb = wada[pc % 2]
                key = "wada%d" % (pc % 2)
                for kc in range(8):
                    dma("pool", wb[:, kc, :], wada_v[:, kc, pc * 1536:(pc + 1) * 1536], wr=[key])
                for fl in range(12):
                    fc = pc * 12 + fl
                    for kc in range(8):
                        mm(psmod[:, fc:fc + 1], wb[:, kc, fl * 128:(fl + 1) * 128], scb[:, kc:kc + 1],
                           kc == 0, kc == 7, rd=[key, "scb"], wr=["psmod"])
                if pc in (1, 3):
                    j = 0 if pc == 1 else 1
                    for hb in range(2):
                        pt = psg[j * 2 + hb]
                        for kc in range(8):
                            mm(pt[:], scB[:, kc, :], wb[:, kc, 512 + hb * 512:512 + (hb + 1) * 512],
                               kc == 0, kc == 7, rd=[key, "scB"], wr=["psg%d" % (j * 2 + hb)])
                        dst = (GT1 if j == 0 else GT2)
                        tt("dve", dst[:, hb * 512:(hb + 1) * 512], pt[:], brow[:, j, hb * 512:(hb + 1) * 512], ALU.add,
                           rd=["psg%d" % (j * 2 + hb), "brow%d" % j], wr=["GT%d%d" % (j, hb)])
                        tt("dve", dst[:, hb * 512:(hb + 1) * 512], dst[:, hb * 512:(hb + 1) * 512],
                           grow[:, j, hb * 512:(hb + 1) * 512], ALU.mult,
                           rd=["GT%d%d" % (j, hb), "grow%d" % j], wr=["GT%d%d" % (j, hb)])
            tt("dve", modc[:], psmod[:], bcol[:], ALU.add, rd=["psmod", "bcol"], wr=["modc"])
            stt("dve", A1[:], modc[:, 8:16], 1.0, gpm[:], ALU.add, ALU.mult, rd=["modc", "gpm"], wr=["A1"])
            cp("dve", B1[:], modc[:, 0:8], rd=["modc"], wr=["B1"])
            stt("dve", A2[:], modc[:, 32:40], 1.0, gpf[:], ALU.add, ALU.mult, rd=["modc", "gpf"], wr=["A2"])
            cp("dve", B2[:], modc[:, 24:32], rd=["modc"], wr=["B2"])
            S.flush()

        def norm_transpose(src_f32, srckey, dstT, col0, Acol, Bcol, xn, pT, ss, idx, tag, hook=None):
            import os
            STEP = int(os.environ.get("DBG_STEP", 99))
            if os.environ.get("DBG_XNJ"):
                xn = junk
            k = "%s%d" % (tag, idx % 2)
            if STEP < 2:
                return
            sumsq(src_f32, [srckey], ss[:, 0:1], "ss" + k)
            if STEP < 3:
                return
            act(ss[:, 1:2], ss[:, 0:1], AF.Sqrt, rd=["ss" + k], wr=["ss" + k], scale=1.0 / D, bias=epsc[:, 0:1])
            rcp(ss[:, 2:3], ss[:, 1:2], rd=["ss" + k], wr=["ss" + k])
            if STEP < 4:
                return
            act(xn[:], src_f32, AF.Identity, rd=[srckey, "ss" + k], wr=["xn" + k], scale=ss[:, 2:3])
            if STEP < 5:
                return
            if hook is not None:
                hook()
            for kc in range(8):
                tr(pT[:, kc * 128:(kc + 1) * 128], xn[:, kc * 128:(kc + 1) * 128], identb[:], rd=["xn" + k], wr=["pT" + k])
            if STEP < 6:
                return
            for kc in range(8):
                o = dstT[:, kc, col0:col0 + 128]
                i = pT[:, kc * 128:(kc + 1) * 128]
                if idx % 2 == 0:
                    act(o, i, AF.Identity, rd=["pT" + k], wr=[], scale=Acol[:, kc:kc + 1], bias=Bcol[:, kc:kc + 1])
                else:
                    ts("dve", o, i, Acol[:, kc:kc + 1], Bcol[:, kc:kc + 1], ALU.mult, ALU.add, rd=["pT" + k], wr=[])

        if stage == 0:
            dma("sp", dbg[:, 0:1024], GT1[:])
            dma("sp", dbg[:, 1024:2048], GT2[:])
            dma("sp", dbg[:, 2048:2056], A1[:])
            dma("sp", dbg[:, 2056:2064], B1[:])
            dma("sp", dbg[:, 2064:2072], A2[:])
            dma("sp", dbg[:, 2072:2080], B2[:])
            S.flush()
            return nc
        with ExitStack() as mix:
            yT = SB(mix, "yT", [128, 8, TO], BF16)
            with ExitStack() as st:
                xt = [SB(st, "xt%d" % i, [128, D], F32) for i in range(3)]
                ssb = [SB(st, "ssb%d" % i, [128, 4], F32) for i in range(2)]
                pT = [PS(st, "pT%d" % i, [128, D], BF16) for i in range(2)]
                import os
                for wt in range(int(os.environ.get("DBG_NT", NT))):
                    dma("sp", xt[wt % 3][:], xw_t[wt], wr=["xt%d" % (wt % 3)])
                    norm_transpose(xt[wt % 3][:], "xt%d" % (wt % 3), hT, wt * 128, A1, B1, xn[wt % 2], pT[wt % 2],
                                   ssb[wt % 2], wt, "b")
                S.flush()
            if stage == 1:
                with ExitStack() as st:
                    tmp = SB(st, "dbgt", [128, 8, TO], F32)
                    for kc in range(8):
                        cp("dve", tmp[:, kc, :], hT[:, kc, TO:T], wr=["t%d" % kc])
                        dma("sp", dbg[:, kc * TO:(kc + 1) * TO], tmp[:, kc, :], rd=["t%d" % kc])
                    S.flush()

            if stage >= 2:
              with ExitStack() as st:
                wing = SB(st, "win_g", [128, 8, 8], BF16)
                w_in_v = w_in.rearrange("(k p) f -> p k f", p=128)
                dma("pool", wing[:], w_in_v[:, :, GIF:GIF + 8], wr=["win"])
                winh = [SB(st, "win_h%d" % h_, [128, 8, 512], BF16) for h_ in range(2)]

                def load_winh(h_):
                    for jj, c0 in enumerate((QM(h_), KM(h_), VM(h_), OM(h_))):
                        dma("pool", winh[h_ % 2][:, :, jj * 128:(jj + 1) * 128], w_in_v[:, :, c0:c0 + 128], wr=["winh%d" % (h_ % 2)])

                load_winh(0)
                bg = SB(st, "bg", [128, 8], F32)
                dma("sp", bg[:], b_gates.partition_broadcast(128), wr=["bg"])
                convw = SB(st, "convw", [128, 8, 4], F32)
                dma("sp", convw[:], conv_col, wr=["convw"])
                gpre = SB(st, "gpre", [128, NT, 8], F32)
                lf = SB(st, "lf", [128, NT, 4], F32)
                gcs = SB(st, "gcs", [128, NT, 4], F32)
                ea = SB(st, "ea", [128, NT, 4], F32)
                qs = SB(st, "qs", [128, NT, 4], F32)
                eG = SB(st, "eG", [128, NT, 4], F32)
                psgt = PS(st, "psgt", [128, NT, 8], F32)
                for wt in range(NT):
                    for kc in range(8):
                        mm(psgt[:, wt, :], hT[:, kc, wt * 128:(wt + 1) * 128], wing[:, kc, :], kc == 0, kc == 7,
                           rd=["win"], wr=["psgt"])
                tt("dve", gpre[:], psgt[:], bg[:].unsqueeze(1).to_broadcast([128, NT, 8]), ALU.add, rd=["psgt", "bg"], wr=["gpre"])
                act(lf[:], gpre[:, :, 4:8], AF.Exp, rd=["gpre"], wr=["lf"], scale=-1.0)
                act(lf[:], lf[:], AF.Ln, rd=["lf"], wr=["lf"], bias=1.0)
                ts("dve", lf[:], lf[:], -1.0, None, ALU.mult, rd=["lf"], wr=["lf"])
                lf2 = lf[:].rearrange("p n h -> p (n h)")
                mm(psgt[:].rearrange("p n h -> p (n h)")[:, 0:128], tri_f[:], lf2, True, True, rd=["lf", "tri_f", "gpre"], wr=["psgt"])
                mm(psgt[:].rearrange("p n h -> p (n h)")[:, 128:256], ones_f[:], lf2, True, True, rd=["lf", "ones_f"], wr=["psgt"])
                psv = psgt[:].rearrange("p n h -> p (n h)")
                act(gcs[:].rearrange("p n h -> p (n h)"), psv[:, 0:128], AF.Copy, rd=["psgt"], wr=["gcs"])
                act(eG[:].rearrange("p n h -> p (n h)"), psv[:, 128:256], AF.Exp, rd=["psgt"], wr=["eG"])
                tt("dve", ea[:], gpre[:, :, 0:4], gcs[:], ALU.subtract, rd=["gpre", "gcs"], wr=["ea"])
                act(ea[:], ea[:], AF.Exp, rd=["ea"], wr=["ea"])
                act(qs[:], gcs[:], AF.Exp, rd=["gcs"], wr=["qs"])
                ts("dve", qs[:], qs[:], float(128 ** -0.5), None, ALU.mult, rd=["qs"], wr=["qs"])
                ts("dve", eG[:, 15, :], eG[:, 15, :], flg[:, 0:1], None, ALU.mult, rd=["eG", "flg"], wr=["eG"])
                S.flush()

                for hd in range(4):
                    with ExitStack() as hs:
                        win = winh[hd % 2]
                        if hd + 1 < 4:
                            load_winh(hd + 1)
                        kraw = SB(hs, "kraw", [128, T + 3], BF16)
                        qraw = SB(hs, "qraw", [128, TO + 3], BF16)
                        ctmp = [SB(hs, "ctmp%d" % i, [128, 1024], F32) for i in range(2)]
                        kT = SB(hs, "kT", [128, T], BF16)
                        qT = SB(hs, "qT", [128, TO], BF16)
                        vt = SB(hs, "vt", [128, NT, 129], BF16)
                        ktm = SB(hs, "ktm", [128, NT, 128], BF16)
                        og = SB(hs, "og", [128, NTO, 128], BF16)
                        Cst = SB(hs, "Cst", [128, 129], F32)
                        Ct = SB(hs, "Ct", [128, 129], F32)
                        Cbf = [SB(hs, "Cbf%d" % i, [128, 129], BF16) for i in range(2)]
                        Sm = [SB(hs, "Sm%d" % i, [128, 128], BF16) for i in range(2)]
                        sm4 = [SB(hs, "sm4%d" % i, [128, 4], F32) for i in range(2)]
                        ytm = [SB(hs, "ytm%d" % i, [128, 128], BF16) for i in range(2)]
                        pp = [PS(hs, "pp%d" % i, [128, 512], F32) for i in range(3)]
                        pCf = [PS(hs, "pCf%d" % i, [128, 512], F32) for i in range(2)]
                        pC = [t_[:, 0:129] for t_ in pCf]
                        pYf = [PS(hs, "pYf%d" % i, [128, 1024], BF16) for i in range(2)]
                        pY = [t_[:, 0:128] for t_ in pYf]
                        npp = [0]

                        def nextpp():
                            i = npp[0] % 3
                            npp[0] += 1
                            return pp[i], "pp%d" % i

                        ms("dve", kraw[:, 0:3], 0.0, wr=["kraw_h"])
                        for blk in range(8):
                            p, pk = nextpp()
                            for kc in range(8):
                                mm(p[:], win[:, kc, 128:256], hT[:, kc, blk * 512:(blk + 1) * 512], kc == 0, kc == 7, rd=["win"], wr=[pk])
                            act(kraw[:, 3 + blk * 512:3 + (blk + 1) * 512], p[:], AF.Copy, rd=[pk], wr=["kraw%d" % blk])
                        ts("dve", kraw[:, 3 + 2045:3 + 2048], kraw[:, 3 + 2045:3 + 2048], flg[:, 0:1], None, ALU.mult,
                           rd=["kraw3"], wr=["kraw3"])
                        p, pk = nextpp()
                        for kc in range(8):
                            mm(p[:, 0:128], win[:, kc, 0:128], hT[:, kc, 1920:2048], kc == 0, kc == 7, rd=["win"], wr=[pk])
                        ts("dve", qraw[:, 0:3], p[:, 125:128], flg[:, 0:1], None, ALU.mult, rd=[pk], wr=["qraw_h"])
                        for blk in range(4):
                            p, pk = nextpp()
                            for kc in range(8):
                                mm(p[:], win[:, kc, 0:128], hT[:, kc, TO + blk * 512:TO + (blk + 1) * 512],
                                   kc == 0, kc == 7, rd=["win"], wr=[pk])
                            act(qraw[:, 3 + blk * 512:3 + (blk + 1) * 512], p[:], AF.Copy, rd=[pk], wr=["qraw%d" % blk])
                        nct = [0]

                        def conv(raw, rawkeys, dstT, nblk, wch, eng):
                            for b in range(nblk):
                                ci = nct[0] % 2
                                nct[0] += 1
                                ck = "ctmp%d" % ci
                                c_ = ctmp[ci]
                                ts(eng, c_[:], raw[:, b * 1024:b * 1024 + 1024], convw[:, wch, 0:1], None, ALU.mult,
                                   rd=rawkeys, wr=[ck])
                                for j in range(1, 4):
                                    stt(eng, c_[:], raw[:, b * 1024 + j:b * 1024 + j + 1024], convw[:, wch, j:j + 1], c_[:],
                                        ALU.mult, ALU.add, rd=rawkeys + [ck], wr=[ck])
                                act(dstT[:, b * 1024:(b + 1) * 1024], c_[:], AF.Silu, rd=[ck], wr=["cv%d_%d" % (wch, b)])

                        conv(kraw, ["kraw%d" % b for b in range(8)] + ["kraw_h"], kT, 4, 4 + hd, "dve")
                        conv(qraw, ["qraw%d" % b for b in range(4)] + ["qraw_h"], qT, 2, hd, "dve")
                        kTkeys = ["cv%d_%d" % (4 + hd, b) for b in range(4)]
                        qTkeys = ["cv%d_%d" % (hd, b) for b in range(2)]
                        for wt in range(NT):
                            if wt % 4 == 0:
                                p, pk = nextpp()
                            o = p[:, (wt % 4) * 128:(wt % 4 + 1) * 128]
                            for kc in range(8):
                                mm(o, hT[:, kc, wt * 128:(wt + 1) * 128], win[:, kc, 256:384], kc == 0, kc == 7, rd=["win"], wr=[pk])
                            ts("dve", vt[:, wt, 0:128], o, ea[:, wt, hd:hd + 1], None, ALU.mult, rd=[pk], wr=["vt%d" % wt])
                            cp("pool", vt[:, wt, 128:129], ea[:, wt, hd:hd + 1], wr=["vt1_%d" % wt])
                        for i in range(NTO):
                            if i % 4 == 0:
                                p, pk = nextpp()
                            o = p[:, (i % 4) * 128:(i % 4 + 1) * 128]
                            for kc in range(8):
                                mm(o, hT[:, kc, TO + i * 128:TO + (i + 1) * 128], win[:, kc, 384:512], kc == 0, kc == 7, rd=["win"], wr=[pk])
                            act(og[:, i, :], o, AF.Sigmoid, rd=[pk], wr=["og%d" % i])
                        for wt in range(NT - 1):
                            py = pY[wt % 2]
                            tr(py, kT[:, wt * 128:(wt + 1) * 128], identb[:], rd=[kTkeys[wt // 8]], wr=["pY%d" % (wt % 2)])
                            act(ktm[:, wt, :], py, AF.Copy, rd=["pY%d" % (wt % 2)], wr=["ktm%d" % wt])
                        ms("dve", Cst[:], 0.0, wr=["Cst"])
                        ms("dve", Cbf[0][:], 0.0, wr=["Cbf0"])
                        for c in range(NT):
                            cb = Cbf[c % 2]
                            cbk = "Cbf%d" % (c % 2)
                            if c >= 16:
                                i = c - 16
                                p, pk = nextpp()
                                mm(p[:, 0:128], kT[:, c * 128:(c + 1) * 128], qT[:, i * 128:(i + 1) * 128], True, True,
                                   rd=[kTkeys[c // 8], qTkeys[i // 8]], wr=[pk])
                                sm = Sm[i % 2]
                                smk = "Sm%d" % (i % 2)
                                tt("dve", sm[:], p[:, 0:128], tri_f[:], ALU.mult, rd=[pk], wr=[smk])
                                pn = p[:, 256:385]
                                mm(pn, sm[:], vt[:, c, :], True, False, rd=[smk, "vt%d" % c, "vt1_%d" % c], wr=[pk])
                                mm(pn, qT[:, i * 128:(i + 1) * 128], cb[:], False, True, rd=[cbk, qTkeys[i // 8]], wr=[pk])
                                s4 = sm4[i % 2]
                                s4k = "sm4%d" % (i % 2)
                                ts("dve", s4[:, 0:1], p[:, 384:385], qs[:, c, hd:hd + 1], None, ALU.mult, rd=[pk], wr=[s4k])
                                ts("dve", s4[:, 3:4], s4[:, 0:1], -1.0, None, ALU.mult, rd=[s4k], wr=[s4k])
                                tt("dve", s4[:, 0:1], s4[:, 0:1], s4[:, 3:4], ALU.max, rd=[s4k], wr=[s4k])
                                ts("dve", s4[:, 0:1], s4[:, 0:1], 1.0, None, ALU.max, rd=[s4k], wr=[s4k])
                                rcp(s4[:, 1:2], s4[:, 0:1], rd=[s4k], wr=[s4k])
                                tt("dve", s4[:, 2:3], s4[:, 1:2], qs[:, c, hd:hd + 1], ALU.mult, rd=[s4k], wr=[s4k])
                                yt_ = ytm[i % 2]
                                ytk = "ytm%d" % (i % 2)
                                stt("dve", yt_[:], p[:, 256:384], s4[:, 2:3], og[:, i, :], ALU.mult, ALU.mult,
                                    rd=[pk, s4k, "og%d" % i], wr=[ytk])
                            if c < NT - 1:
                                pc_ = pC[c % 2]
                                pck = "pC%d" % (c % 2)
                                mm(pc_, ktm[:, c, :], vt[:, c, :], True, True, rd=["ktm%d" % c, "vt%d" % c, "vt1_%d" % c], wr=[pck])
                                tt("dve", Ct[:], pc_, Cst[:], ALU.add, rd=[pck, "Cst"], wr=["Ct"])
                                ts("dve", Cst[:], Ct[:], eG[:, c, hd:hd + 1], None, ALU.mult, rd=["Ct"], wr=["Cst"])
                                nb = Cbf[(c + 1) % 2]
                                act(nb[:], Ct[:], AF.Identity, rd=["Ct"], wr=["Cbf%d" % ((c + 1) % 2)], scale=eG[:, c, hd:hd + 1])
                            if c >= 16:
                                i = c - 16
                                py = pY[i % 2]
                                tr(py, ytm[i % 2][:], identb[:], rd=["ytm%d" % (i % 2)], wr=["pY%d" % (i % 2)])
                                act(yT[:, hd, i * 128:(i + 1) * 128], py, AF.Copy, rd=["pY%d" % (i % 2)], wr=[])
                        S.flush()
            if stage == 2:
                with ExitStack() as st:
                    tmp = SB(st, "dbgt", [128, 8, TO], F32)
                    for kc in range(8):
                        if kc < 4:
                            cp("dve", tmp[:, kc, :], yT[:, kc, :], wr=["t%d" % kc])
                        else:
                            ms("dve", tmp[:, kc, :], 0.0, wr=["t%d" % kc])
                        dma("sp", dbg[:, kc * TO:(kc + 1) * TO], tmp[:, kc, :], rd=["t%d" % kc])
                    S.flush()

            if stage >= 3:
              w_in_v = w_in.rearrange("(k p) f -> p k f", p=128)
              for g in range(2):
                with ExitStack() as gsx:
                    qaT = SB(gsx, "qaT", [64, 4, TO], BF16)
                    ksT = SB(gsx, "ksT", [64, T], BF16)
                    kwT = SB(gsx, "kwT", [64, T], BF16)
                    vsa = SB(gsx, "vsa", [128, NT, 65], BF16)
                    vwa = SB(gsx, "vwa", [128, NT, 65], BF16)
                    kcT = SB(gsx, "kcT", [64, 256], BF16)
                    vca = SB(gsx, "vca", [128, 2, 65], BF16)
                    gat = SB(gsx, "gat", [128, NTO, 12], F32)
                    with ExitStack() as st:
                        win = SB(st, "win_a", [128, 8, 652], BF16)
                        dma("pool", win[:, :, 0:256], w_in_v[:, :, QA(4 * g):QA(4 * g) + 256], wr=["win"])
                        dma("pool", win[:, :, 256:320], w_in_v[:, :, KC(g):KC(g) + 64], wr=["win"])
                        dma("pool", win[:, :, 320:384], w_in_v[:, :, VC(g):VC(g) + 64], wr=["win"])
                        dma("pool", win[:, :, 384:448], w_in_v[:, :, KS(g):KS(g) + 64], wr=["win"])
                        dma("pool", win[:, :, 448:512], w_in_v[:, :, KW(g):KW(g) + 64], wr=["win"])
                        dma("pool", win[:, :, 512:640], w_in_v[:, :, VSW(g):VSW(g) + 128], wr=["win"])
                        dma("pool", win[:, :, 640:652], w_in_v[:, :, GA + 12 * g:GA + 12 * g + 12], wr=["win"])
                        kcr = SB(st, "kcr", [64, T + 32], BF16)
                        vcr = SB(st, "vcr", [64, T + 32], BF16)
                        wk = SB(st, "wk", [64, 32, 64], BF16)
                        wv = SB(st, "wv", [64, 32, 64], BF16)
                        pk_ = SB(st, "pk_", [64, 32], BF16)
                        pv_ = SB(st, "pv_", [64, 32], BF16)
                        kcb = SB(st, "kcb", [64, 1], F32)
                        cvr = SB(st, "cvr", [1, 64], BF16)
                        dma("pool", wk[:], wck, wr=["wk"])
                        dma("pool", wv[:], wcv, wr=["wv"])
                        dma("pool", pk_[:], posk, wr=["pk_"])
                        dma("pool", pv_[:], posv, wr=["pv_"])
                        ms("dve", kcr[:, T:T + 32], 0.0, wr=["kcr_t"])
                        ms("dve", vcr[:, T:T + 32], 0.0, wr=["vcr_t"])
                        ms("pool", vsa[:, :, 64:65], 1.0, wr=["vsa1"])
                        ms("pool", vwa[:, :, 64:65], 1.0, wr=["vwa1"])
                        ms("pool", vca[:, :, 64:65], 1.0, wr=["vca1"])
                        ms("pool", kcT[:, 255:256], 0.0, wr=["kcT1"])
                        pp = [PS(st, "pp%d" % i, [128, 512], F32) for i in range(6)]
                        npp = [0]

                        def nextpp():
                            i = npp[0] % 6
                            npp[0] += 1
                            return pp[i], "pp%d" % i

                        ne = [0]

                        def evac(o, i, rd, wr, scale=None):
                            if int(rd[0][2:]) % 2 == 0:
                                if scale is None:
                                    act(o, i, AF.Copy, rd=rd, wr=wr)
                                else:
                                    act(o, i, AF.Identity, rd=rd, wr=wr, scale=scale)
                            else:
                                if scale is None:
                                    cp("dve", o, i, rd=rd, wr=wr)
                                else:
                                    ts("dve", o, i, scale, None, ALU.mult, rd=rd, wr=wr)

                        for h in range(4):
                            for blk in range(4):
                                p, pk = nextpp()
                                for kc in range(8):
                                    mm(p[0:64, :], win[:, kc, h * 64:(h + 1) * 64], hT[:, kc, TO + blk * 512:TO + (blk + 1) * 512],
                                       kc == 0, kc == 7, rd=["win"], wr=[pk])
                                evac(qaT[:, h, blk * 512:(blk + 1) * 512], p[0:64, :], [pk], [], scale=0.125)
                        for (dst, c0, nm) in ((kcr, 256, "kcr"), (vcr, 320, "vcr"), (ksT, 384, "ksT"), (kwT, 448, "kwT")):
                            for blk in range(8):
                                p, pk = nextpp()
                                for kc in range(8):
                                    mm(p[0:64, :], win[:, kc, c0:c0 + 64], hT[:, kc, blk * 512:(blk + 1) * 512], kc == 0, kc == 7,
                                       rd=["win"], wr=[pk])
                                evac(dst[:, blk * 512:(blk + 1) * 512], p[0:64, :], [pk], [nm])
                        for wt in range(NT):
                            if wt % 4 == 0:
                                p, pk = nextpp()
                            o = p[:, (wt % 4) * 128:(wt % 4 + 1) * 128]
                            for kc in range(8):
                                mm(o, hT[:, kc, wt * 128:(wt + 1) * 128], win[:, kc, 512:640], kc == 0, kc == 7, rd=["win"], wr=[pk])
                            evac(vsa[:, wt, 0:64], o[:, 0:64], [pk], [])
                            evac(vwa[:, wt, 0:64], o[:, 64:128], [pk], [])
                        for i in range(NTO):
                            if i % 4 == 0:
                                p, pk = nextpp()
                            o = p[:, (i % 4) * 16:(i % 4) * 16 + 12]
                            for kc in range(8):
                                mm(o, hT[:, kc, TO + i * 128:TO + (i + 1) * 128], win[:, kc, 640:652], kc == 0, kc == 7, rd=["win"], wr=[pk])
                            act(gat[:, i, :], o, AF.Sigmoid, rd=[pk], wr=[])
                        p, pk = nextpp()
                        for l in range(32):
                            mm(p[0:64, 0:255], wk[:, l, :], kcr[:, l:l + 4065:16], l == 0, l == 31,
                               rd=["wk", "kcr", "kcr_t"], wr=[pk])
                        p2, pk2 = nextpp()
                        for l in range(32):
                            mm(p2[0:64, 0:1], wk[:, l, :], pk_[:, l:l + 1], l == 0, l == 31, rd=["wk", "pk_"], wr=[pk2])
                        cp("dve", kcb[:], p2[0:64, 0:1], rd=[pk2], wr=["kcb"])
                        ts("dve", kcT[:, 0:255], p[0:64, 0:255], kcb[:, 0:1], None, ALU.add, rd=[pk, "kcb"], wr=[])
                        p3, pk3 = nextpp()
                        for l in range(32):
                            mm(p3[0:1, 0:64], pv_[:, l:l + 1], wv[:, l, :], l == 0, l == 31, rd=["wv", "pv_"], wr=[pk3])
                        cp("dve", cvr[:], p3[0:1, 0:64], rd=[pk3], wr=["cvr"])
                        for it in range(2):
                            p, pk = nextpp()
                            for l in range(32):
                                mm(p[:, 0:64], vcr[:, it * 2048 + l:it * 2048 + l + 2033:16], wv[:, l, :], l == 0, False,
                                   rd=["wv", "vcr", "vcr_t"], wr=[pk])
                            mm(p[:, 0:64], ones_b[0:1, :], cvr[:], False, True, rd=["cvr"], wr=[pk])
                            cp("dve", vca[:, it, 0:64], p[:, 0:64], rd=[pk], wr=[])
                        S.flush()

                    with ExitStack() as st:
                        E = SB(st, "E", [65, NT, 128], BF16)
                        dma("pool", E[:], t_E, wr=["E"])
                        ovl = SB(st, "ovl", [128, 2, 64], BF16)
                        dma("pool", ovl[:], t_ovl, wr=["ovl"])
                        wbt = SB(st, "wbt", [128, 10, 512], BF16)
                        dma("pool", wbt[:], t_wb[g], wr=["wbt"])
                        sbt = SB(st, "sbt", [128, 2, 512], BF16)
                        for dl in range(2):
                            dma("pool", sbt[:, dl, :], t_sb[g, dl], wr=["sbt"])
                        cst = SB(st, "cst", [1, 512], BF16)
                        dma("pool", cst[:], t_cst[g], wr=["cst"])
                        addt = SB(st, "addt", [128, NTO, 64], F32)
                        dma("sp", addt[:], t_add, wr=["addt"])
                        cbt = [SB(st, "cbt%d" % i, [128, 2, 512], BF16) for i in range(2)]
                        Pc = [SB(st, "Pc%d" % i, [128, 2, 512], BF16) for i in range(2)]
                        Pb = [SB(st, "Pb%d" % i, [128, 512], BF16) for i in range(4)]
                        snT = [SB(st, "snT%d" % i, [65, 4, 128], BF16) for i in range(2)]
                        for i_ in range(2):
                            dma("pool", snT[i_][64:65, :, :].rearrange("p h t -> p (h t)"), t_cst[g], wr=["snT%d" % i_])
                        sc = [SB(st, "sc%d" % i, [128, 64], F32) for i in range(2)]
                        sc2 = [SB(st, "sc2%d" % i, [128, 64], F32) for i in range(2)]
                        m8 = [SB(st, "m8%d" % i, [128, 8], F32) for i in range(2)]
                        nm = [SB(st, "nm%d" % i, [128, 64], BF16) for i in range(2)]
                        rc = [SB(st, "rc%d" % i, [128, 3, 4], F32) for i in range(2)]
                        oacc = [SB(st, "oacc%d" % i, [128, 4, 64], F32) for i in range(2)]
                        yab = [SB(st, "yab%d" % i, [128, 256], BF16) for i in range(2)]
                        osb = [SB(st, "osb%d" % i, [128, 260], F32) for i in range(2)]
                        pS = [PS(st, "pS%d" % i, [128, 512], F32) for i in range(4)]
                        pCR = PS(st, "pCR", [128, 512], F32)
                        pOf = [None] + [PS(st, "pO%d" % i, [128, 512], F32) for i in (1, 2)]
                        pO = [pCR[:, 0:256].rearrange("p (h d) -> p h d", h=4)] + \
                             [t_[:, 0:260].rearrange("p (h d) -> p h d", h=4) for t_ in pOf[1:]]
                        pR = pCR[:, 256:512].rearrange("p (h d) -> p h d", h=4)
                        pM = PS(st, "pM", [128, 1024], BF16)
                        nS = [0]

                        def nextS():
                            i = nS[0] % 4
                            nS[0] += 1
                            return pS[i], "pS%d" % i, Pb[i], "Pb%d" % i

                        def attn_tile(i):
                            c = 16 + i
                            b2 = i % 2
                            qslice = qaT[:, :, i * 128:(i + 1) * 128]
                            r_ = rc[b2]
                            rk = "rc%d" % b2
                            sn = snT[b2]
                            snk = "snT%d" % b2
                            v3 = lambda p: p[:].rearrange("p (h t) -> p h t", h=4)
                            gv = gat[:, i, :].rearrange("p (h b) -> p b h", b=3)
                            oa = oacc[b2]
                            ok = "oacc%d" % b2

                            def cmp_S(it):
                                def f():
                                    p, pk, _, _ = nextS()
                                    mm(v3(p), kcT[:, it * 128:(it + 1) * 128], qslice, True, False, wr=[pk])
                                    mm(p[:], identb[:], cbt[b2][:, it, :], False, True, rd=["cbt%d" % b2], wr=[pk])
                                    act(Pc[b2][:, it, :], p[:], AF.Exp, rd=[pk], wr=["Pc%d_%d" % (b2, it)])
                                return f

                            def cmp_PV():
                                for h in range(4):
                                    for it in range(2):
                                        mm(pO[0][:, h, :], Pc[b2][:, it, h * 128:(h + 1) * 128], vca[:, it, 0:64], it == 0, it == 1,
                                           rd=["Pc%d_%d" % (b2, it)], wr=["pO0"])
                                for h in range(4):
                                    for it in range(2):
                                        mm(pR[:, h, :], Pc[b2][:, it, h * 128:(h + 1) * 128], ovl[:, it, :], it == 0, it == 1,
                                           rd=["Pc%d_%d" % (b2, it), "ovl"], wr=["pO0"])
                                S.op("dve", lambda e: e.tensor_reduce(out=r_[:, 0, :], in_=pR, axis=AX.X, op=ALU.add), ["pO0"], [rk])
                                ts("dve", r_[:, 0, :], r_[:, 0, :], 1e-30, None, ALU.max, rd=[rk], wr=[rk])
                                rcp(r_[:, 0, :], r_[:, 0, :], rd=[rk], wr=[rk])
                                s_ = sc[b2]
                                sk = "sc%d" % b2
                                cp("dve", s_[:], addt[:, i, :], rd=["addt"], wr=[sk])
                                for h in range(4):
                                    stt("dve", s_[:], pR[:, h, :], r_[:, 0, h:h + 1], s_[:], ALU.mult, ALU.add, rd=["pO0", rk, sk], wr=[sk])
                                s2_ = sc2[b2]
                                s2k = "sc2%d" % b2
                                m_ = m8[b2]
                                mk = "m8%d" % b2
                                S.op("dve", lambda e: e.max(out=m_[:], in_=s_[:]), [sk], [mk])
                                S.op("dve", lambda e: e.match_replace(out=s2_[:], in_to_replace=m_[:], in_values=s_[:], imm_value=-1e30),
                                     [sk, mk], [s2k])
                                S.op("dve", lambda e: e.max(out=m_[:], in_=s2_[:]), [s2k], [mk])
                                ts("dve", s2_[:], s_[:], m_[:, 7:8], None, ALU.is_ge, rd=[sk, mk, s2k], wr=[s2k])
                                ts("dve", s_[:], s_[:], -1e5, None, ALU.is_ge, rd=[sk, s2k], wr=[sk])
                                tt("dve", s2_[:], s2_[:], s_[:], ALU.mult, rd=[sk, s2k], wr=[s2k])
                                n_ = nm[b2]
                                nk = "nm%d" % b2
                                ts("dve", n_[:], s2_[:], -1.0, -NEG, ALU.add, ALU.mult, rd=[s2k], wr=[nk])
                                tt("dve", r_[:, 0, :], r_[:, 0, :], gv[:, 0, :], ALU.mult, rd=[rk], wr=[rk])
                                for h in range(4):
                                    ts("dve", oa[:, h, :], pO[0][:, h, :], r_[:, 0, h:h + 1], None, ALU.mult, rd=["pO0", rk], wr=[ok])

                            def cmpB():
                                n_ = nm[b2]
                                tr(pM[0:64, 0:128], n_[:], identb[:], rd=["nm%d" % b2], wr=["pM"])
                                cp("dve", sn[0:64, :, :], pM[0:64, 0:128].unsqueeze(1).to_broadcast([64, 4, 128]), rd=["pM"], wr=[snk])

                            cmp_stages = [(cmp_S(0), None), (cmp_S(1), cmp_PV)]
                            win_stages = []
                            sel_stages = []

                            def pair(kT_, j, bias_fn, V_, vkey, acc, acck, first, last, use_sel):
                                st_ = {}

                                def fS():
                                    p, pk, pb, pbk = nextS()
                                    st_["x"] = (pb, pbk)
                                    mm(v3(p), kT_[:, j * 128:(j + 1) * 128], qslice, True, False, wr=[pk])
                                    if use_sel == 2:
                                        mm(p[:], E[:, j, :], sn[:].rearrange("p h t -> p (h t)"), False, True, rd=["E", snk], wr=[pk])
                                    else:
                                        if use_sel == 1:
                                            mm(p[:], E[0:64, j, :], sn[0:64, :, :].rearrange("p h t -> p (h t)"), False, False,
                                               rd=["E", snk], wr=[pk])
                                        bias_fn(p, pk)
                                    act(pb[:], p[:], AF.Exp, rd=[pk], wr=[pbk])

                                def fPV():
                                    pb, pbk = st_["x"]
                                    for h in range(4):
                                        mm(acc[:, h, :], pb[:, h * 128:(h + 1) * 128], V_[:, j, :], (first and h == 0), last,
                                           rd=[pbk, vkey], wr=[acck], skip=True)
                                return fS, fPV

                            for dl in range(4, -1, -1):
                                j = c - dl
                                var = dl if j >= 16 else 5 + dl
                                bf = (lambda var: lambda p, pk: mm(p[:], identb[:], wbt[:, var, :], False, True, rd=["wbt"], wr=[pk]))(var)
                                win_stages.append(pair(kwT, j, bf, vwa, "vwa1", pO[2], "pO2", dl == 4, dl == 0, 0))
                            for j in range(c + 1):
                                dl = c - j
                                if dl <= 1:
                                    bf = (lambda dl: lambda p, pk: mm(p[:], identb[:], sbt[:, dl, :], False, True, rd=["sbt"], wr=[pk]))(dl)
                                else:
                                    bf = lambda p, pk: mm(p[:], ones_b[0:1, :], cst[:], False, True, rd=["cst"], wr=[pk])
                                sel_stages.append(pair(ksT, j, bf, vsa, "vsa1", pO[1], "pO1", j == 0, j == c, 1 if dl <= 1 else 2))
                            def merge():
                                for br in (1, 2):
                                    cp("dve", osb[br - 1][:], pOf[br][:, 0:260], rd=["pO%d" % br], wr=["osb%d" % (br - 1)])
                                for br in (1, 2):
                                    ov = osb[br - 1][:, 0:260].rearrange("p (h d) -> p h d", h=4)
                                    ts("dve", r_[:, br, :], ov[:, :, 64], 1e-30, None, ALU.max, rd=["osb%d" % (br - 1), rk], wr=[rk])
                                    rcp(r_[:, br, :], r_[:, br, :], rd=[rk], wr=[rk])
                                    tt("dve", r_[:, br, :], r_[:, br, :], gv[:, br, :], ALU.mult, rd=[rk], wr=[rk])
                                ov1 = osb[0][:, 0:260].rearrange("p (h d) -> p h d", h=4)
                                ov2 = osb[1][:, 0:260].rearrange("p (h d) -> p h d", h=4)
                                for h in range(4):
                                    stt("dve", oa[:, h, :], ov1[:, h, 0:64], r_[:, 1, h:h + 1], oa[:, h, :], ALU.mult, ALU.add,
                                        rd=["osb0", rk, ok], wr=[ok])
                                    stt("dve", yab[b2][:, h * 64:(h + 1) * 64], ov2[:, h, 0:64], r_[:, 2, h:h + 1], oa[:, h, :],
                                        ALU.mult, ALU.add, rd=["osb1", rk, ok], wr=["yab%d" % b2])

                            def mergeB():
                                for hp in range(2):
                                    tr(pM[:, 128 + hp * 128:256 + hp * 128], yab[b2][:, hp * 128:(hp + 1) * 128], identb[:],
                                       rd=["yab%d" % b2], wr=["pM"])
                                    cp("dve", yT[:, 4 + 2 * g + hp, i * 128:(i + 1) * 128], pM[:, 128 + hp * 128:256 + hp * 128],
                                       rd=["pM"], wr=[])

                            return cmp_stages, win_stages, sel_stages, merge, mergeB, cmpB

                        tiles = []
                        allst = []
                        dma("pool", cbt[0][:], t_cb[g, 0], wr=["cbt0"])
                        tiles.append(attn_tile(0))
                        allst += tiles[0][0]
                        allst.append((None, tiles[0][5]))
                        for i in range(NTO):
                            cs, ws, ss_, mg, mgB, cB = tiles[i]
                            allst += ws
                            if i > 0:
                                allst.append((None, tiles[i - 1][4]))
                            if i + 1 < NTO:
                                allst.append(((lambda i=i: lambda: dma("pool", cbt[(i + 1) % 2][:], t_cb[g, i + 1],
                                                                       wr=["cbt%d" % ((i + 1) % 2)]))(), None))
                                tiles.append(attn_tile(i + 1))
                                allst += tiles[i + 1][0]
                            allst += ss_
                            allst.append((None, mg))
                            if i + 1 < NTO:
                                allst.append((None, tiles[i + 1][5]))
                        allst.append((None, tiles[NTO - 1][4]))
                        pend = []
                        for (fS, fPV) in allst:
                            if fS is not None:
                                fS()
                            if len(pend) >= 2:
                                f_ = pend.pop(0)
                                if f_ is not None:
                                    f_()
                            pend.append(fPV)
                        for f_ in pend:
                            if f_ is not None:
                                f_()
                        S.flush()
            if stage == 3:
                with ExitStack() as st:
                    tmp = SB(st, "dbgt", [128, 8, TO], F32)
                    for kc in range(8):
                        cp("dve", tmp[:, kc, :], yT[:, kc, :], wr=["t%d" % kc])
                        dma("sp", dbg[:, kc * TO:(kc + 1) * TO], tmp[:, kc, :], rd=["t%d" % kc])
                    S.flush()

            if stage >= 4:
              with ExitStack() as st:
                wo = SB(st, "wo", [128, 8, D], BF16)
                w_out_v = w_out.rearrange("(k p) f -> p k f", p=128)
                for kc in range(8):
                    dma("pool", wo[:, kc, :], w_out_v[:, kc, :], wr=["wo"])
                xo = [SB(st, "xo%d" % i, [128, D], F32) for i in range(2)]
                x1 = [SB(st, "x1%d" % i, [128, D], F32) for i in range(2)]
                ssb = [SB(st, "ssb%d" % i, [128, 4], F32) for i in range(2)]
                s5 = [SB(st, "s5%d" % i, [128, 4], F32) for i in range(2)]
                pT = [PS(st, "pT%d" % i, [128, D], BF16) for i in range(2)]
                pY = [PS(st, "pYe%d" % i, [128, D], F32) for i in range(2)]
                def mm_y(i_):
                    py_ = pY[i_ % 2]
                    for hb in range(2):
                        for kc in range(8):
                            mm(py_[:, hb * 512:(hb + 1) * 512], yT[:, kc, i_ * 128:(i_ + 1) * 128], wo[:, kc, hb * 512:(hb + 1) * 512],
                               kc == 0, kc == 7, rd=["wo"], wr=["pYe%d" % (i_ % 2)])

                mm_y(0)
                for i in range(NTO):
                    b2 = i % 2
                    dma("sp", xo[b2][:], xw_t[16 + i], wr=["xo%d" % b2])
                    py = pY[b2]
                    pyk = "pYe%d" % b2
                    s_ = s5[b2]
                    sk = "s5%d" % b2
                    S.op("act", (lambda py=py: lambda e: e.activation(out=junkf[:, 0:512], in_=py[:, 0:512], func=AF.Square))(), [pyk], ["junkf"])
                    S.op("act", (lambda py=py: lambda e: e.activation(out=junkf[:, 512:1024], in_=py[:, 512:1024], func=AF.Square))(), [pyk], ["junkf"])
                    S.op("dve", (lambda s_=s_: lambda e: e.tensor_reduce(out=s_[:, 2:3], in_=junkf[:], axis=AX.X, op=ALU.add))(), ["junkf", sk], [sk])
                    act(s_[:, 2:3], s_[:, 2:3], AF.Sqrt, rd=[sk], wr=[sk], scale=1.0 / D, bias=epsc[:, 0:1])
                    rcp(s_[:, 3:4], s_[:, 2:3], rd=[sk], wr=[sk])
                    xk = "x1%d" % b2
                    for hb in range(2):
                        stt("dve", x1[b2][:, hb * 512:(hb + 1) * 512], py[:, hb * 512:(hb + 1) * 512], s_[:, 3:4],
                            GT1[:, hb * 512:(hb + 1) * 512], ALU.mult, ALU.mult, rd=[pyk, sk], wr=[xk])
                    tt("pool", x1[b2][:], x1[b2][:], xo[b2][:], ALU.add, rd=[xk, "xo%d" % b2], wr=[xk])
                    import os
                    _de = int(os.environ.get("DBG_E", 0))
                    if _de != 1:
                        dma("sp", out_t[i], x1[b2][:], rd=[xk])
                    if _de != 2:
                        norm_transpose(x1[b2][:], xk, h2T, i * 128, A2, B2, xn[b2], pT[b2], ssb[b2], i, "e",
                                       hook=(lambda i=i: (lambda: mm_y(i + 1)) if i + 1 < NTO else None)())
                S.flush()
        if stage == 4:
            with ExitStack() as st:
                tmp = SB(st, "dbgt", [128, 8, TO], F32)
                for kc in range(8):
                    cp("dve", tmp[:, kc, :], h2T[:, kc, 0:TO], wr=["t%d" % kc])
                    dma("sp", dbg[:, kc * TO:(kc + 1) * TO], tmp[:, kc, :], rd=["t%d" % kc])
                S.flush()

        if stage >= 5:
          with ExitStack() as st:
            yacc = SB(st, "yacc", [128, NTO, D], F32)
            gate = SB(st, "gate", [128, NTO, NE], F32)
            gateT = SB(st, "gateT", [NE, TO], BF16)
            bup = SB(st, "bup", [128, NE, 16], F32)
            dma("sp", bup[:], b_up_col, wr=["bup"])
            with ExitStack() as rs:
                wr_ = SB(rs, "wr_", [128, 8, NE], BF16)
                dma("pool", wr_[:], w_router.rearrange("(k p) e -> p k e", p=128), wr=["wr_"])
                brt = SB(rs, "brt", [128, NE], F32)
                dma("sp", brt[:], b_router.partition_broadcast(128), wr=["brt"])
                bdn = SB(rs, "bdn", [NE, D], BF16)
                dma("pool", bdn[:], b_down, wr=["bdn"])
                lg = [SB(rs, "lg%d" % i, [128, NE], F32) for i in range(2)]
                mk4 = [SB(rs, "mk4%d" % i, [128, NE], F32) for i in range(2)]
                m8 = [SB(rs, "m8r%d" % i, [128, 8], F32) for i in range(2)]
                s4 = [SB(rs, "s4r%d" % i, [128, 4], F32) for i in range(2)]
                pL = PS(rs, "pL", [128, NTO, NE], F32)
                pGf = [PS(rs, "pG%d" % i, [128, 512], F32) for i in range(2)]
                pG = [t_[0:NE, 0:128] for t_ in pGf]
                pB = [PS(rs, "pB%d" % i, [128, 512], F32) for i in range(2)]
                for i in range(NTO):
                    b2 = i % 2
                    for kc in range(8):
                        mm(pL[:, i, :], h2T[:, kc, i * 128:(i + 1) * 128], wr_[:, kc, :], kc == 0, kc == 7, rd=["wr_"], wr=["pL"])
                    l_ = lg[b2]
                    lk = "lg%d" % b2
                    tt("dve", l_[:], pL[:, i, :], brt[:], ALU.add, rd=["pL", "brt"], wr=[lk])
                    m_ = m8[b2]
                    mk = "m8r%d" % b2
                    S.op("dve", (lambda m_=m_, l_=l_: lambda e: e.max(out=m_[:], in_=l_[:]))(), [lk], [mk])
                    k4 = mk4[b2]
                    k4k = "mk4%d" % b2
                    ts("dve", k4[:], l_[:], m_[:, 3:4], None, ALU.is_ge, rd=[lk, mk], wr=[k4k])
                    s_ = s4[b2]
                    sk = "s4r%d" % b2
                    ts("dve", s_[:, 0:1], m_[:, 0:1], -1.0, None, ALU.mult, rd=[mk], wr=[sk])
                    act(l_[:], l_[:], AF.Exp, rd=[lk, sk, k4k], wr=[lk], bias=s_[:, 0:1])
                    tt("dve", l_[:], l_[:], k4[:], ALU.mult, rd=[lk, k4k], wr=[lk])
                    S.op("dve", (lambda s_=s_, l_=l_: lambda e: e.tensor_reduce(out=s_[:, 1:2], in_=l_[:], axis=AX.X, op=ALU.add))(),
                         [lk, sk], [sk])
                    rcp(s_[:, 2:3], s_[:, 1:2], rd=[sk], wr=[sk])
                    ts("dve", gate[:, i, :], l_[:], s_[:, 2:3], None, ALU.mult, rd=[lk, sk], wr=["gate%d" % i])
                    tr(pG[b2], gate[:, i, :], identf[:], rd=["gate%d" % i], wr=["pG%d" % b2])
                    cp("dve", gateT[:, i * 128:(i + 1) * 128], pG[b2], rd=["pG%d" % b2], wr=["gateT%d" % i])
                    for db in range(2):
                        mm(pB[db][:], gateT[:, i * 128:(i + 1) * 128], bdn[:, db * 512:(db + 1) * 512], True, True,
                           rd=["gateT%d" % i, "bdn"], wr=["pB%d" % db])
                        act(yacc[:, i, db * 512:(db + 1) * 512], pB[db][:], AF.Copy, rd=["pB%d" % db], wr=[])
                S.flush()

            with ExitStack() as es:
                wup = [hT[:, :, TO + i * 1024:TO + (i + 1) * 1024] for i in range(2)]
                wdn = [SB(es, "wdn%d" % i, [128, 4, D], BF16) for i in range(2)]
                gg = [SB(es, "gg%d" % i, [128, 512], F32) for i in range(2)]
                sg = [SB(es, "sg%d" % i, [128, 512], F32) for i in range(2)]
                ll = [SB(es, "ll%d" % i, [128, 512], F32) for i in range(2)]
                actT = [SB(es, "actT%d" % i, [128, 4, 512], BF16) for i in range(2)]
                pU = [PS(es, "pU%d" % i, [128, 512], F32) for i in range(4)]
                pD = [PS(es, "pD%d" % i, [128, 512], F32) for i in range(4)]
                nU = 0
                nD = 0
                nA = 0
                w_up_v = w_up.rearrange("e (k p) f -> e p k f", p=128)
                w_dn_v = w_down.rearrange("e (c p) d -> e p c d", p=128)
                units = [(e_, u) for e_ in range(NE) for u in range(2)]

                def load_unit(n):
                    e_, u = units[n]
                    wb = n % 2
                    for kc in range(8):
                        dma("pool", wup[wb][:, kc, 0:512], w_up_v[e_, :, kc, 512 * u:512 * u + 512], wr=["wup%d" % wb])
                        dma("pool", wup[wb][:, kc, 512:1024], w_up_v[e_, :, kc, 1024 + 512 * u:1024 + 512 * u + 512], wr=["wup%d" % wb])
                    for fc in range(4):
                        dma("pool", wdn[wb][:, fc, :], w_dn_v[e_, :, 4 * u + fc, :], wr=["wdn%d" % wb])

                load_unit(0)
                for n, (e_, u) in enumerate(units):
                    if n + 1 < len(units):
                        load_unit(n + 1)
                    wb = n % 2
                    wuk = "wup%d" % wb
                    wdk = "wdn%d" % wb
                    for tb in range(4):
                        ab = nA % 2
                        nA += 1
                        ak = "actT%d" % ab
                        for fc in range(4):
                            pg = pU[nU % 4]
                            pgk = "pU%d" % (nU % 4)
                            nU += 1
                            pl = pU[nU % 4]
                            plk = "pU%d" % (nU % 4)
                            nU += 1
                            for kc in range(8):
                                mm(pg[:], wup[wb][:, kc, fc * 128:(fc + 1) * 128], h2T[:, kc, tb * 512:(tb + 1) * 512],
                                   kc == 0, kc == 7, rd=[wuk], wr=[pgk])
                            for kc in range(8):
                                mm(pl[:], wup[wb][:, kc, 512 + fc * 128:512 + (fc + 1) * 128], h2T[:, kc, tb * 512:(tb + 1) * 512],
                                   kc == 0, kc == 7, rd=[wuk], wr=[plk])
                            b2 = fc % 2
                            jg = 4 * u + fc
                            ts("dve", gg[b2][:], pg[:], bup[:, e_, jg:jg + 1], 7.0, ALU.add, ALU.min, rd=[pgk], wr=["gg%d" % b2])
                            act(sg[b2][:], gg[b2][:], AF.Sigmoid, rd=["gg%d" % b2], wr=["sg%d" % b2], scale=1.702)
                            ts("dve", ll[b2][:], pl[:], bup[:, e_, 8 + jg:8 + jg + 1], 7.0, ALU.add, ALU.min, rd=[plk], wr=["ll%d" % b2])
                            ts("dve", ll[b2][:], ll[b2][:], -7.0, 1.0, ALU.max, ALU.add, rd=["ll%d" % b2], wr=["ll%d" % b2])
                            tt("dve", gg[b2][:], gg[b2][:], sg[b2][:], ALU.mult, rd=["gg%d" % b2, "sg%d" % b2], wr=["gg%d" % b2])
                            tt("dve", actT[ab][:, fc, :], gg[b2][:], ll[b2][:], ALU.mult, rd=["gg%d" % b2, "ll%d" % b2],
                               wr=[ak + "_%d" % fc])
                        for tt_ in range(4):
                            ti = tb * 4 + tt_
                            for db in range(2):
                                pd = pD[nD % 4]
                                pdk = "pD%d" % (nD % 4)
                                nD += 1
                                for fc in range(4):
                                    mm(pd[:], actT[ab][:, fc, tt_ * 128:(tt_ + 1) * 128], wdn[wb][:, fc, db * 512:(db + 1) * 512],
                                       fc == 0, fc == 3, rd=[ak + "_%d" % fc, wdk], wr=[pdk])
                                ya = yacc[:, ti, db * 512:(db + 1) * 512]
                                stt("dve", ya, pd[:], gate[:, ti, e_:e_ + 1], ya, ALU.mult, ALU.add, rd=[pdk, "ya%d_%d" % (ti, db)],
                                    wr=["ya%d_%d" % (ti, db)])
                S.flush()

            with ExitStack() as fs:
                xr = [SB(fs, "xr%d" % i, [128, D], F32) for i in range(2)]
                ob = [SB(fs, "ob%d" % i, [128, D], F32) for i in range(2)]
                s5 = [SB(fs, "s6%d" % i, [128, 4], F32) for i in range(2)]
                for i in range(NTO):
                    b2 = i % 2
                    dma("sp", xr[b2][:], out_t[i], wr=["xr%d" % b2])
                    s_ = s5[b2]
                    sk = "s6%d" % b2
                    sumsq(yacc[:, i, :], [], s_[:, 0:1], sk)
                    act(s_[:, 1:2], s_[:, 0:1], AF.Sqrt, rd=[sk], wr=[sk], scale=1.0 / D, bias=epsc[:, 0:1])
                    rcp(s_[:, 2:3], s_[:, 1:2], rd=[sk], wr=[sk])
                    stt("dve", ob[b2][:], yacc[:, i, :], s_[:, 2:3], GT2[:], ALU.mult, ALU.mult, rd=[sk], wr=["ob%d" % b2])
                    tt("pool", ob[b2][:], ob[b2][:], xr[b2][:], ALU.add, rd=["ob%d" % b2, "xr%d" % b2], wr=["ob%d" % b2])
                    dma("sp", out_t[i], ob[b2][:], rd=["ob%d" % b2])
                S.flush()
    return nc


_TABLE_CACHE = {}


def _tables(rel_bias, s):
    rb = np.asarray(rel_bias, np.float32)
    lo = 0 if s == 1 else 2048
    jl = np.arange(128)[:, None]
    tl = np.arange(128)[None, :]
    t_sb = np.empty((2, 2, 128, 4, 128), np.float32)
    t_wb = np.empty((2, 128, 10, 4, 128), np.float32)
    t_cst = np.empty((2, 1, 4, 128), np.float32)
    for g in range(2):
        for h in range(4):
            col = rb[:, 4 * g + h]
            t_cst[g, 0, h, :] = col[31]
            for dl in range(5):
                dist = dl * 128 + tl - jl
                bias = col[_t5_bucket(dist)]
                if dl < 2:
                    t_sb[g, dl, :, h, :] = np.where(dist >= 0, bias, np.float32(NEG))
                wv = np.where((dist >= 0) & (dist < 512), bias, np.float32(NEG))
                t_wb[g, :, dl, h, :] = wv
                t_wb[g, :, 5 + dl, h, :] = wv if s == 1 else np.float32(NEG)
    t_cb = np.empty((2, 16, 128, 2, 4, 128), np.float32)
    m = (np.arange(2)[None, :] * 128 + np.arange(128)[:, None])
    end = 16 * m + 31
    for i in range(16):
        tw = 2048 + 128 * i + np.arange(128)
        dist = tw[None, None, :] - end[:, :, None]
        valid = (16 * m[:, :, None] >= lo) & (dist >= 0) & (m[:, :, None] <= 254)
        bk = _t5_bucket(dist)
        for g in range(2):
            for h in range(4):
                t_cb[g, i, :, :, h, :] = np.where(valid, rb[:, 4 * g + h][bk], np.float32(NEG))
    t_add = np.empty((128, 16, 64), np.float32)
    blk = np.arange(64)[None, :]
    for i in range(16):
        tw = (2048 + 128 * i + np.arange(128))[:, None]
        cur = tw // 64
        first = lo // 64
        valid = (64 * blk >= lo) & (64 * blk <= tw)
        forced = (blk == first) | (blk == cur) | (blk == cur - 1)
        t_add[:, i, :] = np.where(valid, np.where(forced, np.float32(1000.0), np.float32(0.0)), np.float32(-1e6))
    return dict(t_sb=t_sb.reshape(2, 2, 128, 512), t_cst=t_cst.reshape(2, 1, 512), t_wb=t_wb.reshape(2, 128, 10, 512),
                t_cb=t_cb.reshape(2, 16, 128, 2, 512), t_add=t_add)


def _consts():
    E = np.zeros((65, 32, 128), np.float32)
    for j in range(32):
        for k in range(128):
            E[2 * j + k // 64, j, k] = 1.0
    E[64] = 1.0
    ovl = np.zeros((256, 64), np.float32)
    for m in range(255):
        for p in range(16 * m, 16 * m + 32):
            ovl[m, p // 64] += 1.0 / 32
    ovl = ovl.reshape(2, 128, 64).transpose(1, 0, 2).copy()
    tri = (np.arange(128)[:, None] <= np.arange(128)[None, :]).astype(np.float32)
    return dict(t_E=E, t_ovl=ovl, t_tri=tri)


_NC_CACHE = {}


def make_in_maps(inputs):
    f = lambda a: np.ascontiguousarray(np.asarray(a, np.float32))
    x = f(inputs["x"])
    c = f(inputs["c"])
    col = lambda v, n: f(np.asarray(v, np.float32).reshape(n, 128).T)
    perm = _perm()
    shared = dict(
        w_ada=f(inputs["w_ada"][0]),
        b_ada_col=col(inputs["b_ada"][0], 48),
        b_ada_row=f(inputs["b_ada"][0][None, :]),
        gpm_col=col(inputs["g_pre_mix"][0], 8),
        gpf_col=col(inputs["g_pre_ffn"][0], 8),
        gpostm=f(inputs["g_post_mix"][0][None, :]),
        gpostf=f(inputs["g_post_ffn"][0][None, :]),
        w_in=f(np.asarray(inputs["w_in"][0])[:, perm]),
        b_gates=f(inputs["b_gates"][0][None, :]),
        conv_col=f(np.asarray(inputs["conv_qk"][0]).reshape(4, 8, 128).transpose(2, 1, 0)),
        wck=f(np.asarray(inputs["w_cmp_k"][0]).transpose(1, 0, 2)),
        wcv=f(np.asarray(inputs["w_cmp_v"][0]).transpose(1, 0, 2)),
        posk=f(np.asarray(inputs["pos_cmp_k"][0]).T),
        posv=f(np.asarray(inputs["pos_cmp_v"][0]).T),
        w_out=f(inputs["w_out"][0]),
        w_router=f(inputs["w_router"][0]),
        b_router=f(inputs["b_router"][0][None, :]),
        w_up=f(inputs["w_up"][0]),
        b_up_col=f(np.asarray(inputs["b_up"][0]).reshape(NE, 16, 128).transpose(2, 0, 1)),
        w_down=f(inputs["w_down"][0]),
        b_down=f(inputs["b_down"][0]),
    )
    shared.update(_consts())
    tabs = [_tables(inputs["rel_bias"], s) for s in range(2)]
    in_maps = []
    for core in range(8):
        b, s = core // 2, core % 2
        m = dict(shared)
        m.update(tabs[s])
        if s == 1:
            m["xw"] = x[b]
        else:
            m["xw"] = np.ascontiguousarray(np.concatenate([x[b, TO:], x[b, :TO]], axis=0))
        m["c_col"] = col(c[b], 8)
        m["flag"] = np.full((128, 1), float(s), np.float32)
        in_maps.append(m)
    return in_maps


def kernel(**inputs):
    if "nc" not in _NC_CACHE:
        _NC_CACHE["nc"] = build_nc()
    nc = _NC_CACHE["nc"]
    in_maps = make_in_maps(inputs)
    res = run_bass_kernel_spmd(nc, in_maps, core_ids=list(range(8)))
    out = np.empty((4, 4096, D), np.float32)
    for core in range(8):
        b, s = core // 2, core % 2
        out[b, s * TO:(s + 1) * TO] = res.results[core]["out"]
    return out
```
